# Optimizing a Trainium2 kernel written in Bass

```python
import jax, jax.numpy as jnp
from jax import lax
import numpy as np

D_MODEL = 2048
BATCH = 8
SEQ = 2048
DEPTH = 1

HEAD_DIM = 128
ATTN_HEADS = D_MODEL // HEAD_DIM
ATTN_WIDTH = ATTN_HEADS * HEAD_DIM
DILATION_PATTERNS = ((128, 1), (512, 4), (2048, 16))
ATTN_BLOCK = 128
ROPE_THETA = 500000.0
ROPE_DIM = HEAD_DIM // 4
POS_OFFSET_MAX = 4096

REC_EXPAND = 128
REC_HEADS = D_MODEL // REC_EXPAND
REC_KEY_DIM = REC_EXPAND
REC_VAL_DIM = D_MODEL // REC_HEADS
REC_WIDTH = REC_HEADS * REC_KEY_DIM
REC_VWIDTH = REC_HEADS * REC_VAL_DIM
REC_CHUNK = 64

IN_SECTIONS = (ATTN_WIDTH, ATTN_WIDTH, ATTN_WIDTH,
               REC_WIDTH, REC_WIDTH, REC_VWIDTH, REC_VWIDTH,
               D_MODEL, D_MODEL)
IN_WIDTH = sum(IN_SECTIONS)

N_GROUPS = 4
EXPERTS_PER_GROUP = 8
N_EXPERTS = N_GROUPS * EXPERTS_PER_GROUP
TOP_K = 2
EXPERT_HIDDEN = D_MODEL // 2
MOE_BLOCK = 128

NORM_EPS = 1e-6

kernel_name = "hybrid_dilated_attn_hgrn2_hiermoe_block"


def rms_norm(x, g):
    xf = x.astype(jnp.float32)
    y = xf * lax.rsqrt(jnp.mean(xf * xf, axis=-1, keepdims=True) + NORM_EPS)
    return (y * g.astype(jnp.float32)).astype(x.dtype)


def modulate(y, shift, scale):
    return y * (1 + scale[:, None, :]) + shift[:, None, :]


def partial_rotary(x, positions):
    half = ROPE_DIM // 2
    inv_freq = ROPE_THETA ** (-jnp.arange(0, ROPE_DIM, 2, dtype=jnp.float32) / ROPE_DIM)
    ang = positions.astype(jnp.float32)[..., None] * inv_freq
    cos, sin = jnp.cos(ang)[:, :, None, :], jnp.sin(ang)[:, :, None, :]
    xf = x.astype(jnp.float32)
    x1, x2, xp = xf[..., :half], xf[..., half:ROPE_DIM], xf[..., ROPE_DIM:]
    out = jnp.concatenate([x1 * cos - x2 * sin, x2 * cos + x1 * sin, xp], axis=-1)
    return out.astype(x.dtype)


def dilated_window_attention(q, k, v, window, dilation):
    B, S, H, Dh = q.shape
    span = window // dilation
    n_sub = S // dilation
    L = min(ATTN_BLOCK, n_sub)
    nb = -(-n_sub // L)
    tail = nb * L - n_sub
    BR = B * dilation

    def to_sub(a):
        a = a.reshape(B, n_sub, dilation, H, Dh)
        return jnp.swapaxes(a, 1, 2).reshape(BR, n_sub, H, Dh)

    def key_blocks(a):
        a = jnp.pad(to_sub(a), ((0, 0), (L, tail), (0, 0), (0, 0)))
        prev = a[:, :nb * L].reshape(BR, nb, L, H, Dh)
        cur = a[:, L:].reshape(BR, nb, L, H, Dh)
        return jnp.concatenate([prev, cur], axis=2)

    qs = jnp.pad(to_sub(q), ((0, 0), (0, tail), (0, 0), (0, 0))).reshape(BR, nb, L, H, Dh)
    kb, vb = key_blocks(k), key_blocks(v)

    qi = jnp.arange(L)[:, None]
    kj = jnp.arange(2 * L)[None, :]
    dist = qi - kj + L
    kpos = jnp.arange(nb)[:, None, None] * L + kj[None] - L
    valid = (dist >= 0)[None] & (dist <= span)[None] & (kpos >= 0)

    s = jnp.einsum('bnqhd,bnkhd->bnhqk', qs, kb).astype(jnp.float32) * (Dh ** -0.5)
    s = jnp.where(valid[None, :, None], s, -jnp.inf)
    m = jnp.max(s, axis=-1, keepdims=True)
    p = jnp.exp(s - m)
    den = jnp.sum(p, axis=-1, keepdims=True)
    o = jnp.einsum('bnhqk,bnkhd->bnqhd', p, vb.astype(jnp.float32)) / jnp.swapaxes(den, 2, 3)
    lse = jnp.swapaxes((m + jnp.log(den))[..., 0], 2, 3)

    def from_sub(a):
        trail = a.shape[3:]
        a = a.reshape((B, dilation, nb * L) + trail)[:, :, :n_sub]
        return jnp.swapaxes(a, 1, 2).reshape((B, S) + trail)

    return from_sub(o), from_sub(lse)


def dilated_mixture_attention(q, k, v):
    outs, lses = [], []
    for window, dilation in DILATION_PATTERNS:
        o, lse = dilated_window_attention(q, k, v, window, dilation)
        outs.append(o)
        lses.append(lse)
    alpha = jax.nn.softmax(jnp.stack(lses), axis=0)
    return jnp.einsum('pbsh,pbshd->bshd', alpha, jnp.stack(outs))


def chunked_gated_recurrence(q, k, v, log_f):
    B, S, H, K = q.shape
    V = v.shape[-1]
    C = min(REC_CHUNK, S)
    nc = S // C

    def to_chunks(a):
        return a.reshape(B, nc, C, H, a.shape[-1]).transpose(1, 0, 3, 2, 4)

    causal = jnp.tril(jnp.ones((C, C), dtype=bool))

    def step(state, blk):
        qb, kb, vb, gb = blk
        b = jnp.cumsum(gb, axis=2)
        rel = jnp.where(causal[:, :, None], b[:, :, :, None, :] - b[:, :, None, :, :], -jnp.inf)
        scores = jnp.einsum('bhtk,bhtsk,bhsk->bhts', qb, jnp.exp(rel), kb)
        intra = jnp.einsum('bhts,bhsv->bhtv', scores, vb)
        inter = jnp.einsum('bhtk,bhkv->bhtv', qb * jnp.exp(b), state)
        b_last = b[:, :, -1, :]
        new_state = jnp.exp(b_last)[..., None] * state + jnp.einsum(
            'bhsk,bhsv->bhkv', kb * jnp.exp(b_last[:, :, None, :] - b), vb)
        return new_state, intra + inter

    state0 = jnp.zeros((B, H, K, V), jnp.float32)
    _, out = lax.scan(step, state0, (to_chunks(q), to_chunks(k), to_chunks(v), to_chunks(log_f)))
    return out.transpose(1, 0, 3, 2, 4).reshape(B, S, H, V)


def hgrn2_branch(q_r, f_r, i_r, g_r, lower_bound, norm_g):
    B, S, _ = q_r.shape
    lb = lower_bound.reshape(REC_HEADS, REC_KEY_DIM)
    fpre = f_r.astype(jnp.float32).reshape(B, S, REC_HEADS, REC_KEY_DIM)
    log_f = jnp.log(lb + (1 - lb) * jax.nn.sigmoid(fpre))
    k = (1 - lb) * jax.nn.sigmoid(-fpre)
    q = jax.nn.silu(q_r.astype(jnp.float32)).reshape(B, S, REC_HEADS, REC_KEY_DIM)
    v = i_r.astype(jnp.float32).reshape(B, S, REC_HEADS, REC_VAL_DIM)
    o = chunked_gated_recurrence(q, k, v, log_f)
    gate = jax.nn.silu(g_r.astype(jnp.float32).reshape(B, S, REC_HEADS, REC_VAL_DIM))
    o = rms_norm(o, norm_g) * gate
    return o.reshape(B, S, REC_VWIDTH).astype(q_r.dtype)


def hybrid_mixer(u, positions, w_in, w_attn_branch, w_rec_branch, w_mix_out, rec_norm_g, lower_bound):
    B, S, _ = u.shape
    proj = u @ w_in
    splits = np.cumsum(IN_SECTIONS)[:-1].tolist()
    q_a, k_a, v_a, q_r, f_r, i_r, g_r, gate_a, gate_r = jnp.split(proj, splits, axis=-1)
    q_a = partial_rotary(q_a.reshape(B, S, ATTN_HEADS, HEAD_DIM), positions)
    k_a = partial_rotary(k_a.reshape(B, S, ATTN_HEADS, HEAD_DIM), positions)
    v_a = v_a.reshape(B, S, ATTN_HEADS, HEAD_DIM)
    y_attn = dilated_mixture_attention(q_a, k_a, v_a).reshape(B, S, ATTN_WIDTH).astype(u.dtype)
    y_rec = hgrn2_branch(q_r, f_r, i_r, g_r, lower_bound, rec_norm_g)
    merged = (jax.nn.sigmoid(gate_a) * (y_attn @ w_attn_branch)
              + jax.nn.sigmoid(gate_r) * (y_rec @ w_rec_branch))
    return merged @ w_mix_out


def hierarchical_route(xt, wg, bg, we, be):
    T = xt.shape[0]
    lg = (xt @ wg).astype(jnp.float32) + bg.astype(jnp.float32)
    pg = jax.nn.softmax(lg, axis=-1)
    g_sel = jnp.argmax(lg, axis=-1)
    p_group = jnp.take_along_axis(pg, g_sel[:, None], axis=-1)
    le = ((xt @ we).astype(jnp.float32) + be.astype(jnp.float32)).reshape(T, N_GROUPS, EXPERTS_PER_GROUP)
    le_sel = jnp.take_along_axis(le, g_sel[:, None, None], axis=1)[:, 0]
    top_logits, top_local = lax.top_k(le_sel, TOP_K)
    weights = p_group * jax.nn.softmax(top_logits, axis=-1)
    expert_idx = (g_sel[:, None] * EXPERTS_PER_GROUP + top_local).astype(jnp.int32)
    return expert_idx, weights


def grouped_expert_ffn(xt, expert_idx, weights, w_gate, w_up, w_down):
    T, D = xt.shape
    n_assign = T * TOP_K
    n_blocks = -(-n_assign // MOE_BLOCK) + N_EXPERTS
    n_slots = n_blocks * MOE_BLOCK
    flat_e = expert_idx.reshape(-1)
    order = jnp.argsort(flat_e)
    sorted_e = flat_e[order]
    counts = jnp.bincount(flat_e, length=N_EXPERTS)
    padded = ((counts + MOE_BLOCK - 1) // MOE_BLOCK) * MOE_BLOCK
    pad_end = jnp.cumsum(padded)
    pad_start = pad_end - padded
    start = jnp.cumsum(counts) - counts
    rank = jnp.arange(n_assign) - start[sorted_e]
    dest = pad_start[sorted_e] + rank
    token_of_slot = jnp.full((n_slots,), T, jnp.int32).at[dest].set((order // TOP_K).astype(jnp.int32))
    weight_of_slot = jnp.zeros((n_slots,), jnp.float32).at[dest].set(weights.reshape(-1)[order])
    block_expert = jnp.minimum(
        jnp.searchsorted(pad_end, jnp.arange(n_blocks) * MOE_BLOCK, side='right'), N_EXPERTS - 1)
    xpad = jnp.concatenate([xt, jnp.zeros((1, D), xt.dtype)], axis=0)

    def expert_block(args):
        tok, e = args
        xb = xpad[tok]
        hdn = jax.nn.silu(xb @ w_gate[e]) * (xb @ w_up[e])
        return hdn @ w_down[e]

    yb = lax.map(expert_block, (token_of_slot.reshape(n_blocks, MOE_BLOCK), block_expert))
    ys = yb.reshape(n_slots, D).astype(jnp.float32) * weight_of_slot[:, None]
    out = jax.ops.segment_sum(ys, token_of_slot, num_segments=T + 1)[:T]
    return out.astype(xt.dtype)


def hierarchical_moe(u, wg, bg, we, be, w_gate, w_up, w_down):
    B, S, D = u.shape
    xt = u.reshape(B * S, D)
    expert_idx, weights = hierarchical_route(xt, wg, bg, we, be)
    return grouped_expert_ffn(xt, expert_idx, weights, w_gate, w_up, w_down).reshape(B, S, D)


def setup_inputs(seed: int = 0) -> dict:
    key = jax.random.key(seed)
    ks = jax.random.split(key, 24)
    D = D_MODEL

    def nrm(k, shape, scale):
        return jax.random.normal(k, shape, jnp.float32) * scale

    x = nrm(ks[0], (BATCH, SEQ, D), 1.0)
    c = nrm(ks[1], (BATCH, D), 1.0)
    positions = (jax.random.randint(ks[2], (BATCH, 1), 0, POS_OFFSET_MAX, dtype=jnp.int32)
                 + jnp.arange(SEQ, dtype=jnp.int32)[None, :])
    return {
        "x": x,
        "c": c,
        "positions": positions,
        "ada_w": nrm(ks[3], (DEPTH, D, 6 * D), 0.5 * D ** -0.5),
        "ada_b": nrm(ks[4], (DEPTH, 6 * D), 0.02),
        "mix_norm_g": 1.0 + nrm(ks[5], (DEPTH, D), 0.02),
        "w_in": nrm(ks[6], (DEPTH, D, IN_WIDTH), D ** -0.5),
        "w_attn_branch": nrm(ks[7], (DEPTH, ATTN_WIDTH, D), ATTN_WIDTH ** -0.5),
        "w_rec_branch": nrm(ks[8], (DEPTH, REC_VWIDTH, D), REC_VWIDTH ** -0.5),
        "w_mix_out": nrm(ks[9], (DEPTH, D, D), D ** -0.5),
        "rec_norm_g": 1.0 + nrm(ks[10], (DEPTH, REC_VAL_DIM), 0.02),
        "rec_lb_logits": nrm(ks[11], (DEPTH + 1, REC_WIDTH), 0.1),
        "ffn_norm_g": 1.0 + nrm(ks[12], (DEPTH, D), 0.02),
        "router_group_w": nrm(ks[13], (DEPTH, D, N_GROUPS), D ** -0.5),
        "router_group_b": nrm(ks[14], (DEPTH, N_GROUPS), 0.01),
        "router_expert_w": nrm(ks[15], (DEPTH, D, N_EXPERTS), D ** -0.5),
        "router_expert_b": nrm(ks[16], (DEPTH, N_EXPERTS), 0.01),
        "expert_w_gate": nrm(ks[17], (DEPTH, N_EXPERTS, D, EXPERT_HIDDEN), D ** -0.5),
        "expert_w_up": nrm(ks[18], (DEPTH, N_EXPERTS, D, EXPERT_HIDDEN), D ** -0.5),
        "expert_w_down": nrm(ks[19], (DEPTH, N_EXPERTS, EXPERT_HIDDEN, D), EXPERT_HIDDEN ** -0.5),
        "final_norm_g": 1.0 + nrm(ks[20], (D,), 0.02),
    }


def reference(x, c, positions, ada_w, ada_b, mix_norm_g, w_in, w_attn_branch, w_rec_branch,
              w_mix_out, rec_norm_g, rec_lb_logits, ffn_norm_g, router_group_w, router_group_b,
              router_expert_w, router_expert_b, expert_w_gate, expert_w_up, expert_w_down,
              final_norm_g):
    cond = jax.nn.silu(c)
    lower_bounds = jnp.cumsum(jax.nn.softmax(rec_lb_logits.astype(jnp.float32), axis=0), axis=0)
    h = x
    for layer in range(DEPTH):
        mod = cond @ ada_w[layer] + ada_b[layer]
        sh_m, sc_m, gt_m, sh_f, sc_f, gt_f = jnp.split(mod, 6, axis=-1)
        u = modulate(rms_norm(h, mix_norm_g[layer]), sh_m, sc_m)
        mix = hybrid_mixer(u, positions, w_in[layer], w_attn_branch[layer], w_rec_branch[layer],
                           w_mix_out[layer], rec_norm_g[layer], lower_bounds[layer])
        h = h + gt_m[:, None, :] * mix
        u = modulate(rms_norm(h, ffn_norm_g[layer]), sh_f, sc_f)
        ffn = hierarchical_moe(u, router_group_w[layer], router_group_b[layer],
                               router_expert_w[layer], router_expert_b[layer],
                               expert_w_gate[layer], expert_w_up[layer], expert_w_down[layer])
        h = h + gt_f[:, None, :] * ffn
    return rms_norm(h, final_norm_g)
```

```python
import numpy as np
from contextlib import ExitStack
import concourse.bass as bass
import concourse.mybir as mybir
from concourse.bass_utils import run_bass_kernel_spmd

F32 = mybir.dt.float32
BF16 = mybir.dt.bfloat16
I32 = mybir.dt.int32
U32 = mybir.dt.uint32
AF = mybir.ActivationFunctionType
ALU = mybir.AluOpType
AX = mybir.AxisListType

D = 2048
S = 2048
NH = 16
DH = 128
INW = 18432
EPS = 1e-6
NEG = -30000.0
ROPE_THETA = 500000.0


class Prog:
    COMPUTE = ("pe", "act", "dve", "pool")
    ENG = ("pe", "act", "dve", "pool", "sp")

    def __init__(self, nc, es):
        self.nc, self.es = nc, es
        self.streams = {e: [] for e in self.ENG}
        self.esem = {e: es.enter_context(nc.semaphore(f"sem_{e}")) for e in self.COMPUTE}
        self.ecount = {e: 0 for e in self.COMPUTE}
        self.known = {e: {} for e in self.ENG}
        self.state = {}
        self.dsem = {}

    def _need(self, eng, ev):
        if ev is None:
            return
        name, sem, val, owner = ev
        if owner == eng and eng == "pe":
            return
        if self.known[eng].get(name, 0) >= val:
            return
        self.known[eng][name] = val
        self.streams[eng].append(("wait", sem, val))

    def _deps(self, eng, reads, writes):
        for k in reads:
            st = self.state.get(k)
            if st:
                self._need(eng, st["w"])
        for k in writes:
            st = self.state.get(k)
            if st:
                self._need(eng, st["w"])
                for ev in st["r"]:
                    self._need(eng, ev)

    def _commit(self, ev, reads, writes):
        for k in reads:
            st = self.state.setdefault(k, {"w": None, "r": []})
            st["r"].append(ev)
            if len(st["r"]) > 24:
                st["r"] = st["r"][-24:] if False else st["r"]
        for k in writes:
            self.state[k] = {"w": ev, "r": []}

    def op(self, eng, fn, reads=(), writes=()):
        self._deps(eng, reads, writes)
        self.ecount[eng] += 1
        ev = (f"e_{eng}", self.esem[eng], self.ecount[eng], eng)
        self.streams[eng].append(("op", fn, self.esem[eng], 1))
        self._commit(ev, reads, writes)
        return ev

    def dma(self, q, fn, semname, reads=(), writes=()):
        self._deps(q, reads, writes)
        if semname not in self.dsem:
            self.dsem[semname] = [self.es.enter_context(self.nc.semaphore(f"d_{semname}")), 0]
        d = self.dsem[semname]
        d[1] += 16
        ev = (f"d_{semname}", d[0], d[1], "dma")
        self.streams[q].append(("op", fn, d[0], 16))
        self._commit(ev, reads, writes)
        return ev

    def barrier(self):
        evs = []
        for e in self.COMPUTE:
            if self.ecount[e]:
                evs.append((f"e_{e}", self.esem[e], self.ecount[e], e))
        for name, (sem, cnt) in self.dsem.items():
            if cnt:
                evs.append((f"d_{name}", sem, cnt, "dma"))
        for eng in self.ENG:
            for ev in evs:
                self._need(eng, ev)
        self.state = {}

    def emit(self):
        nc = self.nc
        engmap = {"pe": "tensor", "act": "scalar", "dve": "vector", "pool": "gpsimd", "sp": "sync"}
        with nc.Block() as block:
            for e in self.ENG:
                stream = self.streams[e]
                if not stream:
                    continue

                def body(eng, stream=stream):
                    for item in stream:
                        if item[0] == "wait":
                            eng.wait_ge(item[1], item[2])
                        else:
                            ins = item[1](eng)
                            ins.then_inc(item[2], item[3])

                getattr(block, engmap[e])(body)


def host_consts():
    c = {}
    c["ident_bf"] = np.eye(128, dtype=np.float32)
    c["ones_f"] = np.ones((128, 128), np.float32)
    inv = ROPE_THETA ** (-np.arange(0, 32, 2, dtype=np.float32) / np.float32(32))
    c["invf"] = np.concatenate([inv, inv]).astype(np.float32).reshape(32, 1)
    c["sinsign"] = np.concatenate([-np.ones(16), np.ones(16)]).astype(np.float32).reshape(32, 1)
    pm = np.eye(128, dtype=np.float32)
    pm[:32, :32] = 0.0
    for m in range(32):
        pm[(m + 16) % 32, m] = 1.0
    c["perm"] = pm
    kk = np.arange(128)[:, None]
    qq = np.arange(128)[None, :]
    mb = np.zeros((128, 256), np.float32)
    mb[:, :128] = np.where(kk <= qq, 0.0, NEG)
    mb[:, 128:] = np.where(kk >= qq, 0.0, NEG)
    c["maskb"] = mb
    rm = np.ones((128, S), np.float32)
    rm[:, ::64] = 0.0
    c["resetm"] = rm
    ss = np.arange(64)[:, None]
    tt = np.arange(64)[None, :]
    c["causal"] = np.tile((ss <= tt).astype(np.float32), (1, 8))
    c["ltri"] = (np.arange(128)[:, None] < np.arange(128)[None, :]).astype(np.float32)
    c["iota_p"] = np.arange(128, dtype=np.float32).reshape(128, 1)
    c["thrb"] = np.broadcast_to((np.arange(64, dtype=np.float32) * 128.0)[None, :], (128, 64)).copy()
    c["iota_e"] = np.broadcast_to(np.arange(32, dtype=np.float32)[None, :], (128, 32)).copy()
    return c


CONST_SHAPES = {k: v.shape for k, v in host_consts().items()}

IN_SHAPES = {
    "x": ([S, D], F32), "c_t": ([128, 16], F32), "pos": ([S], I32),
    "ada_w": ([D, 6 * D], F32), "ada_b": ([6 * D], F32), "mix_norm_g": ([D], F32),
    "w_in": ([D, INW], F32), "w_a": ([D, D], F32), "w_r": ([D, D], F32), "w_o": ([D, D], F32),
    "rec_norm_g": ([128, 1], F32), "lb_logits_t": ([128, 2, 16], F32), "ffn_norm_g": ([D], F32),
    "w_router": ([D, 36], F32), "b_router": ([36], F32),
    "w_gate": ([32 * D, 1024], F32), "w_up": ([32 * D, 1024], F32), "w_down": ([32 * 1024, D], F32),
    "final_norm_g": ([D], F32),
}


def build(stop_after=99, debug=False, rot=3, trig=True, p2_ct=72, skip=(), nh3=NH, nh4=NH, nb7=64, p3v=3, p3rs=(1, 4, 16)):
    nc = bass.Bass("TRN2", target_bir_lowering=False)
    I = {}
    for k, (shp, dt) in IN_SHAPES.items():
        I[k] = nc.dram_tensor(k, list(shp), dt, kind="ExternalInput").ap()
    C = {}
    for k, shp in CONST_SHAPES.items():
        C[k] = nc.dram_tensor("c_" + k, list(shp), F32, kind="ExternalInput").ap()
    out = nc.dram_tensor("out", [S, D], F32, kind="ExternalOutput").ap()
    skind = "ExternalOutput" if debug else "Internal"

    def scratch(name, shape, dt):
        return nc.dram_tensor(name, list(shape), dt, kind=skind).ap()

    modbc = scratch("s_modbc", [128, 6 * D], F32)
    qT_s = scratch("s_qT", [NH, 128, S], BF16)
    kT_s = scratch("s_kT", [NH, 128, S], BF16)
    v_s = scratch("s_v", [S, D], BF16)
    qr_s = scratch("s_qr", [NH, 128, S], BF16)
    f_s = scratch("s_f", [NH, 128, S], F32)
    i_s = scratch("s_i", [S, D], BF16)
    g_s = scratch("s_g", [NH, 128, S], BF16)
    ga_s = scratch("s_ga", [NH, 128, S], BF16)
    gr_s = scratch("s_gr", [NH, 128, S], BF16)
    ya_s = scratch("s_ya", [NH, 128, S], BF16)
    yr_s = scratch("s_yr", [NH, 128, S], BF16)
    m_s = scratch("s_m", [NH, 128, S], BF16)
    h_s = scratch("s_h", [S, D], F32)
    u2_s = scratch("s_u2", [S, D], BF16)
    lg_s = scratch("s_lg", [128, 16, 36], F32)
    xslot = scratch("s_xslot", [64 * 128, D], BF16)
    yslot = scratch("s_yslot", [64 * 128, D], F32)
    rt_s = scratch("s_rt", [128, 16, 8], F32)

    with ExitStack() as es:
        P = Prog(nc, es)
        ident_bf = es.enter_context(nc.sbuf_tensor("ident_bf", [128, 128], BF16))
        ident_f = es.enter_context(nc.sbuf_tensor("ident_f", [128, 128], F32))
        ones_f = es.enter_context(nc.sbuf_tensor("ones_f", [128, 128], F32))
        ones_bf = es.enter_context(nc.sbuf_tensor("ones_bf", [128, 128], BF16))
        P.dma("sp", lambda e: e.dma_start(out=ident_f[:], in_=C["ident_bf"]), "c0", writes=["ident_f"])
        P.dma("sp", lambda e: e.dma_start(out=ones_f[:], in_=C["ones_f"]), "c1", writes=["ones_f"])
        P.op("dve", lambda e: e.tensor_copy(out=ident_bf[:], in_=ident_f[:]), reads=["ident_f"], writes=["ident_bf"])
        P.op("dve", lambda e: e.tensor_copy(out=ones_bf[:], in_=ones_f[:]), reads=["ones_f"], writes=["ones_bf"])
        desti = es.enter_context(nc.sbuf_tensor("desti", [128, 32], I32))
        wts = es.enter_context(nc.sbuf_tensor("wts", [128, 32], F32))
        offs_gu = es.enter_context(nc.sbuf_tensor("offs_gu", [128, 64], I32))
        offs_d = es.enter_context(nc.sbuf_tensor("offs_d", [128, 64], I32))
        P.barrier()

        with ExitStack() as ps:
            c_sb = ps.enter_context(nc.sbuf_tensor("c_sb", [128, 16], F32))
            L = ps.enter_context(nc.sbuf_tensor("adaL", [128, 16, 128], BF16))
            modsb = ps.enter_context(nc.sbuf_tensor("modsb", [128, 6 * D], F32))
            wst = [ps.enter_context(nc.sbuf_tensor(f"a_wst{i}", [128, 16, 512], F32)) for i in range(2)]
            wbf = [ps.enter_context(nc.sbuf_tensor(f"a_wbf{i}", [128, 16, 512], BF16)) for i in range(2)]
            pp = [ps.enter_context(nc.psum_tensor(f"a_ps{i}", [128, 512], F32)) for i in range(2)]
            P.dma("sp", lambda e: e.dma_start(out=c_sb[:], in_=I["c_t"]), "c0", writes=["c_sb"])
            P.dma("sp", lambda e: e.dma_start(out=modsb[:], in_=I["ada_b"].partition_broadcast(128)), "c1", writes=["modsb"])
            P.op("act", lambda e: e.activation(out=c_sb[:], in_=c_sb[:], func=AF.Silu), reads=["c_sb"], writes=["c_sb"])

            def mkL(e):
                ins = None
                for j in range(16):
                    ins = e.tensor_scalar(out=L[:, j, :], in0=ones_f[:], scalar1=c_sb[:, j:j + 1], scalar2=None, op0=ALU.mult)
                return ins
            P.op("dve", mkL, reads=["c_sb"], writes=["L"])
            awv = I["ada_w"].rearrange("(kc p) n -> p kc n", p=128)
            for ct in range(0 if 0 in skip else 24):
                s = ct % 2
                P.dma("sp", lambda e, s=s, ct=ct: e.dma_start(out=wst[s][:], in_=awv[:, :, 512 * ct:512 * ct + 512]),
                      f"w{s}", writes=[("wst", s)])
                P.op("pool", lambda e, s=s: e.tensor_copy(out=wbf[s][:], in_=wst[s][:]), reads=[("wst", s)], writes=[("wbf", s)])

                def mm(e, s=s):
                    ins = None
                    for kc in range(16):
                        ins = e.matmul(pp[s][:], lhsT=L[:, kc, :], rhs=wbf[s][:, kc, :], start=(kc == 0), stop=(kc == 15))
                    return ins
                P.op("pe", mm, reads=["L", ("wbf", s)], writes=[("pp", s)])
                P.op("dve", lambda e, s=s, ct=ct: e.tensor_tensor(out=modsb[:, 512 * ct:512 * ct + 512], in0=pp[s][:],
                                                                 in1=modsb[:, 512 * ct:512 * ct + 512], op=ALU.add),
                     reads=[("pp", s), "modsb"], writes=["modsb"])
            P.dma("sp", lambda e: e.dma_start(out=modbc, in_=modsb[:]), "c0", reads=["modsb"], writes=["modbc"])
            P.barrier()
        if stop_after <= 0:
            P.barrier(); P.emit(); return nc

        ps12 = es.enter_context(ExitStack())
        uT = ps12.enter_context(nc.sbuf_tensor("uT", [128, 16, S], BF16))
        with ExitStack() as ps:
            G1 = ps.enter_context(nc.sbuf_tensor("G1", [128, D], F32))
            shm = ps.enter_context(nc.sbuf_tensor("shm", [128, D], F32))
            tmpg = ps.enter_context(nc.sbuf_tensor("tmpg", [128, D], F32))
            xt = [ps.enter_context(nc.sbuf_tensor(f"xt{i}", [128, D], F32)) for i in range(2)]
            junk = ps.enter_context(nc.sbuf_tensor("junk", [128, D], BF16))
            tf = ps.enter_context(nc.sbuf_tensor("tf", [128, D], F32))
            ub = [ps.enter_context(nc.sbuf_tensor(f"ub{i}", [128, D], BF16)) for i in range(2)]
            st = [ps.enter_context(nc.sbuf_tensor(f"st{i}", [128, 4], F32)) for i in range(2)]
            ptr = [ps.enter_context(nc.psum_tensor(f"ptr{i}", [128, 16, 128], BF16)) for i in range(2)]
            P.dma("sp", lambda e: e.dma_start(out=shm[:], in_=modbc[:, 0:D]), "c0", writes=["shm"])
            P.dma("sp", lambda e: e.dma_start(out=tmpg[:], in_=modbc[:, D:2 * D]), "c1", writes=["tmpg"])
            P.dma("sp", lambda e: e.dma_start(out=G1[:], in_=I["mix_norm_g"].partition_broadcast(128)), "c2", writes=["G1"])
            P.op("dve", lambda e: e.scalar_tensor_tensor(out=G1[:], in0=tmpg[:], scalar=1.0, in1=G1[:], op0=ALU.add, op1=ALU.mult),
                 reads=["tmpg", "G1"], writes=["G1"])
            for i in range(0 if 1 in skip else 16):
                s = i % 2
                P.dma("sp", lambda e, s=s, i=i: e.dma_start(out=xt[s][:], in_=I["x"][128 * i:128 * i + 128, :]), f"w{s}", writes=[("xt", s)])
                P.op("act", lambda e, s=s: e.activation(out=junk[:], in_=xt[s][:], func=AF.Square, accum_out=st[s][:, 0:1]),
                     reads=[("xt", s)], writes=["junk", ("st", s)])
                P.op("act", lambda e, s=s: e.activation(out=st[s][:, 1:2], in_=st[s][:, 0:1], func=AF.Sqrt, scale=1.0 / D, bias=EPS),
                     reads=[("st", s)], writes=[("st", s)])
                P.op("dve", lambda e, s=s: e.reciprocal(out=st[s][:, 2:3], in_=st[s][:, 1:2]), reads=[("st", s)], writes=[("st", s)])
                P.op("dve", lambda e, s=s: e.scalar_tensor_tensor(out=tf[:], in0=xt[s][:], scalar=st[s][:, 2:3], in1=G1[:], op0=ALU.mult, op1=ALU.mult),
                     reads=[("xt", s), ("st", s), "G1"], writes=["tf"])
                P.op("pool", lambda e, s=s: e.tensor_tensor(out=ub[s][:], in0=tf[:], in1=shm[:], op=ALU.add),
                     reads=["tf", "shm"], writes=[("ub", s)])

                def tr(e, s=s):
                    ins = None
                    for kc in range(16):
                        ins = e.transpose(out=ptr[s][:, kc, :], in_=ub[s][:, 128 * kc:128 * kc + 128], identity=ident_bf[:])
                    return ins
                P.op("pe", tr, reads=[("ub", s), "ident_bf"], writes=[("ptr", s)])
                P.op("act", lambda e, s=s, i=i: e.activation(out=uT[:, :, 128 * i:128 * i + 128], in_=ptr[s][:], func=AF.Copy),
                     reads=[("ptr", s)], writes=["uT"])
            P.barrier()

        if stop_after <= 1:
            P.barrier(); P.emit(); return nc
        PI = float(np.pi)
        with ExitStack() as ps:
            cosF = ps.enter_context(nc.sbuf_tensor("cosF", [128, S], F32))
            sinF = ps.enter_context(nc.sbuf_tensor("sinF", [128, S], F32))
            permb = ps.enter_context(nc.sbuf_tensor("permb", [128, 128], BF16))
            permf = ps.enter_context(nc.sbuf_tensor("permf", [128, 128], F32))
            with ExitStack() as ts:
                cosT = ts.enter_context(nc.sbuf_tensor("cosT", [32, S], F32))
                sinT = ts.enter_context(nc.sbuf_tensor("sinT", [32, S], F32))
                posi = ts.enter_context(nc.sbuf_tensor("posi", [32, S], I32))
                ang = ts.enter_context(nc.sbuf_tensor("ang", [32, S], F32))
                kq = ts.enter_context(nc.sbuf_tensor("kq", [32, S], F32))
                ki = ts.enter_context(nc.sbuf_tensor("ki", [32, S], I32))
                rr = ts.enter_context(nc.sbuf_tensor("rr", [32, S], F32))
                mm_ = ts.enter_context(nc.sbuf_tensor("mm_", [32, S], F32))
                sm = ts.enter_context(nc.sbuf_tensor("sm", [32, 40], F32))
                P.dma("sp", lambda e: e.dma_start(out=posi[:], in_=I["pos"].partition_broadcast(32)), "c0", writes=["posi"])
                P.dma("sp", lambda e: e.dma_start(out=sm[:, 0:1], in_=C["invf"]), "c1", writes=["sm0"])
                P.dma("sp", lambda e: e.dma_start(out=sm[:, 1:2], in_=C["sinsign"]), "c2", writes=["sm1"])
                P.dma("sp", lambda e: e.dma_start(out=permf[:], in_=C["perm"]), "c3", writes=["permf"])
                P.op("dve", lambda e: e.tensor_copy(out=permb[:], in_=permf[:]), reads=["permf"], writes=["permb"])
                P.op("dve", lambda e: e.tensor_copy(out=ang[:], in_=posi[:]), reads=["posi"], writes=["ang"])
                P.op("dve", lambda e: e.tensor_scalar(out=ang[:], in0=ang[:], scalar1=sm[:, 0:1], scalar2=None, op0=ALU.mult),
                     reads=["ang", "sm0"], writes=["ang"])

                def reduce_to(dst, shift, tag):
                    P.op("dve", lambda e: e.tensor_scalar(out=kq[:], in0=ang[:], scalar1=shift, scalar2=1.0 / (2 * PI), op0=ALU.add, op1=ALU.mult),
                         reads=["ang"], writes=["kq"])
                    P.op("dve", lambda e: e.tensor_copy(out=ki[:], in_=kq[:]), reads=["kq"], writes=["ki"])
                    P.op("dve", lambda e: e.tensor_copy(out=kq[:], in_=ki[:]), reads=["ki"], writes=["kq"])
                    P.op("dve", lambda e: e.scalar_tensor_tensor(out=rr[:], in0=kq[:], scalar=-2 * PI, in1=ang[:], op0=ALU.mult, op1=ALU.add),
                         reads=["kq", "ang"], writes=["rr"])
                    if shift != 0.0:
                        P.op("dve", lambda e: e.tensor_scalar(out=rr[:], in0=rr[:], scalar1=shift, scalar2=None, op0=ALU.add),
                             reads=["rr"], writes=["rr"])
                    P.op("dve", lambda e: e.tensor_scalar(out=mm_[:], in0=rr[:], scalar1=PI, scalar2=-2 * PI, op0=ALU.is_gt, op1=ALU.mult),
                         reads=["rr"], writes=["mm_"])
                    P.op("dve", lambda e: e.tensor_tensor(out=rr[:], in0=rr[:], in1=mm_[:], op=ALU.add), reads=["rr", "mm_"], writes=["rr"])
                    P.op("dve", lambda e: e.tensor_scalar(out=mm_[:], in0=rr[:], scalar1=-PI, scalar2=2 * PI, op0=ALU.is_lt, op1=ALU.mult),
                         reads=["rr"], writes=["mm_"])
                    P.op("dve", lambda e: e.tensor_tensor(out=rr[:], in0=rr[:], in1=mm_[:], op=ALU.add), reads=["rr", "mm_"], writes=["rr"])
                    P.op("dve", lambda e: e.tensor_scalar(out=rr[:], in0=rr[:], scalar1=3.1415925, scalar2=-3.1415925, op0=ALU.min, op1=ALU.max),
                         reads=["rr"], writes=["rr"])
                    P.op("act", lambda e: e.activation(out=dst[:], in_=rr[:], func=AF.Sin), reads=["rr"], writes=[tag])
                reduce_to(sinT, 0.0, "sinT")
                P.op("dve", lambda e: e.tensor_scalar(out=sinT[:], in0=sinT[:], scalar1=sm[:, 1:2], scalar2=None, op0=ALU.mult),
                     reads=["sinT", "sm1"], writes=["sinT"])
                reduce_to(cosT, PI / 2, "cosT")
                P.op("pool", lambda e: e.memset(cosF[:], 1.0), writes=["cosF"])
                P.op("pool", lambda e: e.memset(sinF[:], 0.0), writes=["sinF"])
                P.op("dve", lambda e: e.tensor_copy(out=cosF[0:32, :], in_=cosT[:]), reads=["cosT", "cosF"], writes=["cosF"])
                P.op("dve", lambda e: e.tensor_copy(out=sinF[0:32, :], in_=sinT[:]), reads=["sinT", "sinF"], writes=["sinF"])
                P.barrier()

            wst2 = [ps.enter_context(nc.sbuf_tensor(f"p2_wst{i}", [128, 16, 256], F32)) for i in range(2)]
            wbf2 = [ps.enter_context(nc.sbuf_tensor(f"p2_wbf{i}", [128, 16, 256], BF16)) for i in range(2)]
            stg = [ps.enter_context(nc.sbuf_tensor(f"p2_stg{i}", [128, S], BF16)) for i in range(2)]
            stgf = [ps.enter_context(nc.sbuf_tensor(f"p2_stgf{i}", [128, S], F32)) for i in range(2)]
            stgt = [ps.enter_context(nc.sbuf_tensor(f"p2_stgt{i}", [128, 256], BF16)) for i in range(2)]
            rt1 = ps.enter_context(nc.sbuf_tensor("p2_rt1", [128, 512], F32))
            rt2 = ps.enter_context(nc.sbuf_tensor("p2_rt2", [128, 512], F32))
            xb = ps.enter_context(nc.sbuf_tensor("p2_xb", [128, 512], BF16))
            xf = ps.enter_context(nc.sbuf_tensor("p2_xf", [128, 512], F32))
            pf = ps.enter_context(nc.sbuf_tensor("p2_pf", [128, 512], F32))
            pp2 = [ps.enter_context(nc.psum_tensor(f"p2_ps{i}", [128, 512], F32)) for i in range(4)]
            psw = [ps.enter_context(nc.psum_tensor(f"p2_psw{i}", [128, 512], F32)) for i in range(2)]
            wv = I["w_in"].rearrange("(kc p) n -> p kc n", p=128)
            SEC = [("rot", qT_s), ("rot", kT_s), ("tok", v_s), ("silu", qr_s), ("f32", f_s), ("tok", i_s), ("silu", g_s), ("sig", ga_s), ("sig", gr_s)]
            ppi = 0
            swi = 0
            sti = 0
            stt = 0
            for ct in range(0 if 2 in skip else p2_ct):
                s = ct % 2
                sec = ct // 8
                kind, dst = SEC[sec]
                P.dma("sp", lambda e, s=s, ct=ct: e.dma_start(out=wst2[s][:], in_=wv[:, :, 256 * ct:256 * ct + 256]), f"w{s}", writes=[("wst", s)])
                P.op("pool", lambda e, s=s: e.tensor_copy(out=wbf2[s][:], in_=wst2[s][:]), reads=[("wst", s)], writes=[("wbf", s)])
                if kind == "tok":
                    c0 = 256 * (ct % 8)
                    for tt in range(16):
                        b = ppi % 4; ppi += 1
                        def mm(e, s=s, tt=tt, b=b):
                            ins = None
                            for kc in range(16):
                                ins = e.matmul(pp2[b][:, 0:256], lhsT=uT[:, kc, 128 * tt:128 * tt + 128], rhs=wbf2[s][:, kc, :], start=(kc == 0), stop=(kc == 15))
                            return ins
                        P.op("pe", mm, reads=[("wbf", s)], writes=[("pp", b)])
                        q = stt % 2; stt += 1
                        P.op("act", lambda e, b=b, q=q: e.activation(out=stgt[q][:], in_=pp2[b][:, 0:256], func=AF.Copy), reads=[("pp", b)], writes=[("stgt", q)])
                        P.dma("sp", lambda e, q=q, tt=tt, c0=c0, dst=dst: e.dma_start(out=dst[128 * tt:128 * tt + 128, c0:c0 + 256], in_=stgt[q][:]),
                              f"o{q}", reads=[("stgt", q)], writes=[("dtok", ct, tt)])
                    continue
                for sub in range(2):
                    h = (ct % 8) * 2 + sub
                    q = sti % 2; sti += 1
                    sbuf_dst = stgf[q] if kind == "f32" else stg[q]
                    skey = ("stgf", q) if kind == "f32" else ("stg", q)
                    for tc in range(4):
                        b = ppi % 4; ppi += 1
                        def mm(e, s=s, sub=sub, tc=tc, b=b):
                            ins = None
                            for kc in range(16):
                                ins = e.matmul(pp2[b][:], lhsT=wbf2[s][:, kc, 128 * sub:128 * sub + 128], rhs=uT[:, kc, 512 * tc:512 * tc + 512], start=(kc == 0), stop=(kc == 15))
                            return ins
                        P.op("pe", mm, reads=[("wbf", s)], writes=[("pp", b)])
                        func = {"rot": AF.Copy, "silu": AF.Silu, "f32": AF.Copy, "sig": AF.Sigmoid}[kind]
                        cs = slice(512 * tc, 512 * tc + 512)
                        if kind == "rot" and rot:
                            w = swi % 2; swi += 1
                            P.op("act", lambda e, b=b: e.activation(out=xb[:], in_=pp2[b][:], func=AF.Copy), reads=[("pp", b)], writes=["xb"])
                            P.op("act", lambda e, b=b: e.activation(out=xf[:], in_=pp2[b][:], func=AF.Copy), reads=[("pp", b)], writes=["xf"])
                            P.op("pe", lambda e, w=w: e.matmul(psw[w][:], lhsT=permb[:], rhs=xb[:], start=True, stop=True),
                                 reads=["xb"], writes=[("psw", w)])
                            P.op("act", lambda e, w=w: e.activation(out=pf[:], in_=psw[w][:], func=AF.Copy), reads=[("psw", w)], writes=["pf"])
                            P.op("dve", lambda e, cs=cs: e.tensor_tensor(out=rt1[:], in0=xf[:], in1=cosF[:, cs], op=ALU.mult),
                                 reads=["xf"], writes=["rt1"])
                            P.op("dve", lambda e, cs=cs: e.tensor_tensor(out=rt2[:], in0=pf[:], in1=sinF[:, cs], op=ALU.mult),
                                 reads=["pf"], writes=["rt2"])
                            P.op("pool", lambda e, cs=cs, sbuf_dst=sbuf_dst: e.tensor_tensor(out=sbuf_dst[:, cs], in0=rt1[:], in1=rt2[:], op=ALU.add),
                                 reads=["rt1", "rt2"], writes=[skey])
                        else:
                            P.op("act", lambda e, b=b, cs=cs, sbuf_dst=sbuf_dst, func=func: e.activation(out=sbuf_dst[:, cs], in_=pp2[b][:], func=func),
                                 reads=[("pp", b)], writes=[skey])
                    P.dma("sp", lambda e, h=h, dst=dst, sbuf_dst=sbuf_dst: e.dma_start(out=dst[h], in_=sbuf_dst[:]), f"o{q}f" if kind == "f32" else f"o{q}",
                          reads=[skey], writes=[("dfm", sec, h)])
            P.barrier()

        ps12.close()
        if stop_after <= 2:
            P.barrier(); P.emit(); return nc

        def phase4():
            with ExitStack() as ps:
                A = lambda n, shp, dt: ps.enter_context(nc.sbuf_tensor("p4_" + n, shp, dt))
                lbl = A("lbl", [128, 2, 16], F32)
                lbT = A("lbT", [128, 16], F32)
                oml = A("oml", [128, 16], F32)
                noml = A("noml", [128, 16], F32)
                rng = A("rng", [128, 1], F32)
                resetm = A("resetm", [128, S], F32)
                causal = A("causal", [64, 512], F32)
                f4 = A("f", [128, S], F32)
                lf = A("lf", [128, S], F32)
                kk = A("kk", [128, S], F32)
                bcs = A("bcs", [128, S], F32)
                eb = A("eb", [128, S], F32)
                enb = A("enb", [128, S], F32)
                oT = A("oT", [128, S], F32)
                qr4 = A("qr", [128, S], BF16)
                g4 = A("g", [128, S], BF16)
                qe = A("qe", [128, S], BF16)
                ke = A("ke", [128, S], BF16)
                sq = A("sq", [128, S], BF16)
                yr = A("yr", [128, S], BF16)
                vch = A("vch", [64, 32, 128], BF16)
                keT = A("keT", [64, 32, 128], BF16)
                Vsb = A("Vsb", [128, 32, 128], F32)
                Sst = A("Sst", [128, 32, 128], F32)
                Sbf = A("Sbf", [128, 32, 128], BF16)
                scs = [A(f"scs{i}", [64, 512], BF16) for i in range(4)]
                psA = [ps.enter_context(nc.psum_tensor(f"p4_A{i}", [64, 512], F32)) for i in range(2)]
                psB = ps.enter_context(nc.psum_tensor("p4_B", [64, 8, 128], BF16))
                psC = [ps.enter_context(nc.psum_tensor(f"p4_C{i}", [128, 4, 128], F32)) for i in range(2)]
                psE = [ps.enter_context(nc.psum_tensor(f"p4_E{i}", [128, 512], F32)) for i in range(2)]
                psF = ps.enter_context(nc.psum_tensor("p4_F", [128, 512], F32))
                P.dma("sp", lambda e: e.dma_start(out=lbl[:], in_=I["lb_logits_t"]), "c0", writes=["lbl"])
                P.dma("sp", lambda e: e.dma_start(out=rng[:], in_=I["rec_norm_g"]), "c1", writes=["rng"])
                P.dma("sp", lambda e: e.dma_start(out=resetm[:], in_=C["resetm"]), "c2", writes=["resetm"])
                P.dma("sp", lambda e: e.dma_start(out=causal[:], in_=C["causal"]), "c3", writes=["causal"])
                P.op("dve", lambda e: e.tensor_tensor(out=lbT[:], in0=lbl[:, 0, :], in1=lbl[:, 1, :], op=ALU.subtract), reads=["lbl"], writes=["lbT"])
                P.op("act", lambda e: e.activation(out=lbT[:], in_=lbT[:], func=AF.Sigmoid), reads=["lbT"], writes=["lbT"])
                P.op("dve", lambda e: e.tensor_scalar(out=oml[:], in0=lbT[:], scalar1=-1.0, scalar2=1.0, op0=ALU.mult, op1=ALU.add), reads=["lbT"], writes=["oml"])
                P.op("dve", lambda e: e.tensor_scalar(out=noml[:], in0=oml[:], scalar1=-1.0, scalar2=None, op0=ALU.mult), reads=["oml"], writes=["noml"])
                P.op("pool", lambda e: e.memset(Sst[:, 0, :], 0.0), writes=["Sst0"])
                for h in range(nh4):
                    P.dma("sp", lambda e, h=h: e.dma_start(out=f4[:], in_=f_s[h]), "w0", writes=["f4"])
                    P.dma("sp", lambda e, h=h: e.dma_start(out=qr4[:], in_=qr_s[h]), "w1", writes=["qr4"])
                    P.dma("sp", lambda e, h=h: e.dma_start(out=g4[:], in_=g_s[h]), "o0", writes=["g4"])
                    P.dma("sp", lambda e, h=h: e.dma_start(out=vch[:], in_=i_s[:, 128 * h:128 * h + 128].rearrange("(c s) d -> s c d", s=64)), "o1", writes=["vch"])
                    hc = slice(h, h + 1)
                    P.op("act", lambda e: e.activation(out=f4[:], in_=f4[:], func=AF.Sigmoid), reads=["f4"], writes=["f4"])
                    P.op("act", lambda e, hc=hc: e.activation(out=lf[:], in_=f4[:], func=AF.Ln, scale=oml[:, hc], bias=lbT[:, hc]), reads=["f4", "oml", "lbT"], writes=["lf"])
                    P.op("dve", lambda e, hc=hc: e.tensor_scalar(out=kk[:], in0=f4[:], scalar1=noml[:, hc], scalar2=oml[:, hc], op0=ALU.mult, op1=ALU.add),
                         reads=["f4", "oml", "noml"], writes=["kk"])
                    P.op("dve", lambda e: e.tensor_tensor_scan(out=bcs[:], data0=resetm[:], data1=lf[:], initial=0.0, op0=ALU.mult, op1=ALU.add),
                         reads=["resetm", "lf"], writes=["bcs"])
                    P.op("act", lambda e: e.activation(out=eb[:], in_=bcs[:], func=AF.Exp), reads=["bcs"], writes=["eb"])
                    P.op("act", lambda e: e.activation(out=enb[:], in_=bcs[:], func=AF.Exp, scale=-1.0), reads=["bcs"], writes=["enb"])
                    P.op("dve", lambda e: e.tensor_tensor(out=qe[:], in0=qr4[:], in1=eb[:], op=ALU.mult), reads=["qr4", "eb"], writes=["qe"])
                    P.op("pool", lambda e: e.tensor_tensor(out=ke[:], in0=kk[:], in1=enb[:], op=ALU.mult), reads=["kk", "enb"], writes=["ke"])
                    for g in range(4):
                        a = g % 2
                        def mmA(e, g=g, a=a):
                            ins = None
                            for c8 in range(8):
                                c = 8 * g + c8
                                ins = e.matmul(psA[a][:, 64 * c8:64 * c8 + 64], lhsT=ke[:, 64 * c:64 * c + 64], rhs=qe[:, 64 * c:64 * c + 64], start=True, stop=True)
                            return ins
                        P.op("pe", mmA, reads=["ke", "qe"], writes=[("psA", a)])
                        P.op("dve", lambda e, g=g, a=a: e.tensor_tensor(out=scs[g][:], in0=psA[a][:], in1=causal[:], op=ALU.mult),
                             reads=[("psA", a), "causal"], writes=[("scs", g)])
                    for g in range(4):
                        def trB(e, g=g):
                            ins = None
                            for c8 in range(8):
                                c = 8 * g + c8
                                ins = e.transpose(out=psB[:, c8, :], in_=ke[:, 64 * c:64 * c + 64], identity=ident_bf[:])
                            return ins
                        P.op("pe", trB, reads=["ke"], writes=["psB"])
                        P.op("act", lambda e, g=g: e.activation(out=keT[:, 8 * g:8 * g + 8, :], in_=psB[:], func=AF.Copy), reads=["psB"], writes=["keT"])
                    for g in range(8):
                        a = g % 2
                        def mmC(e, g=g, a=a):
                            ins = None
                            for c4 in range(4):
                                c = 4 * g + c4
                                ins = e.matmul(psC[a][:, c4, :], lhsT=keT[:, c, :], rhs=vch[:, c, :], start=True, stop=True)
                            return ins
                        P.op("pe", mmC, reads=["keT", "vch"], writes=[("psC", a)])
                        def evC(e, g=g, a=a):
                            ins = None
                            for c4 in range(4):
                                c = 4 * g + c4
                                ins = e.activation(out=Vsb[:, c, :], in_=psC[a][:, c4, :], func=AF.Copy, scale=eb[:, 64 * c + 63:64 * c + 64])
                            return ins
                        P.op("act", evC, reads=[("psC", a), "eb"], writes=["Vsb"])
                    def scanD(e):
                        ins = None
                        for c in range(31):
                            ins = e.scalar_tensor_tensor(out=Sst[:, c + 1, :], in0=Sst[:, c, :], scalar=eb[:, 64 * c + 63:64 * c + 64], in1=Vsb[:, c, :], op0=ALU.mult, op1=ALU.add)
                        return ins
                    P.op("dve", scanD, reads=["Vsb", "eb", "Sst0"], writes=["Sst"])
                    P.op("pool", lambda e: e.tensor_copy(out=Sbf[:], in_=Sst[:]), reads=["Sst", "Sst0"], writes=["Sbf"])
                    for g in range(4):
                        a = g % 2
                        def mmE(e, g=g, a=a):
                            ins = None
                            for c8 in range(8):
                                c = 8 * g + c8
                                reg = psE[a][:, 64 * c8:64 * c8 + 64]
                                e.matmul(reg, lhsT=Sbf[:, c, :], rhs=qe[:, 64 * c:64 * c + 64], start=True, stop=False)
                                ins = e.matmul(reg, lhsT=vch[:, c, :], rhs=scs[g][:, 64 * c8:64 * c8 + 64], start=False, stop=True)
                            return ins
                        P.op("pe", mmE, reads=["Sbf", "qe", "vch", ("scs", g)], writes=[("psE", a)])
                        P.op("act", lambda e, g=g, a=a: e.activation(out=oT[:, 512 * g:512 * g + 512], in_=psE[a][:], func=AF.Copy), reads=[("psE", a)], writes=["oT"])
                    P.op("act", lambda e: e.activation(out=sq[:], in_=oT[:], func=AF.Square), reads=["oT"], writes=["sq"])
                    for g in range(4):
                        gs = slice(512 * g, 512 * g + 512)
                        P.op("pe", lambda e, gs=gs: e.matmul(psF[:], lhsT=ones_bf[:], rhs=sq[:, gs], start=True, stop=True), reads=["sq"], writes=["psF"])
                        P.op("act", lambda e, gs=gs: e.activation(out=bcs[:, gs], in_=psF[:], func=AF.Sqrt, scale=1.0 / 128, bias=EPS), reads=["psF"], writes=["bcs"])
                    P.op("dve", lambda e: e.reciprocal(out=bcs[:], in_=bcs[:]), reads=["bcs"], writes=["bcs"])
                    P.op("dve", lambda e: e.tensor_tensor(out=oT[:], in0=oT[:], in1=bcs[:], op=ALU.mult), reads=["oT", "bcs"], writes=["oT"])
                    P.op("dve", lambda e: e.scalar_tensor_tensor(out=yr[:], in0=oT[:], scalar=rng[:, 0:1], in1=g4[:], op0=ALU.mult, op1=ALU.mult),
                         reads=["oT", "rng", "g4"], writes=["yr"])
                    P.dma("sp", lambda e, h=h: e.dma_start(out=yr_s[h], in_=yr[:]), "y0", reads=["yr"], writes=[("yr_s", h)])
                P.barrier()

        def phase3():
            with ExitStack() as ps:
                mbf = ps.enter_context(nc.sbuf_tensor("p3_mbf", [128, 256], F32))
                maskb = ps.enter_context(nc.sbuf_tensor("p3_maskb", [128, 256], BF16))
                q3 = [ps.enter_context(nc.sbuf_tensor(f"p3_q{i}", [128, S], BF16)) for i in range(2)]
                k3 = [ps.enter_context(nc.sbuf_tensor(f"p3_k{i}", [128, S], BF16)) for i in range(2)]
                v3 = [[ps.enter_context(nc.sbuf_tensor(f"p3_v{i}_{r}", [128, 16, 128], BF16)) for r in range(3)] for i in range(2)]
                num = ps.enter_context(nc.sbuf_tensor("p3_num", [128, S], F32))
                den = ps.enter_context(nc.sbuf_tensor("p3_den", [128, S], F32))
                ya = [ps.enter_context(nc.sbuf_tensor(f"p3_ya{i}", [128, S], BF16)) for i in range(2)]
                pT = [ps.enter_context(nc.sbuf_tensor(f"p3_pT{i}", [128, 256], BF16)) for i in range(4)]
                pstb = [ps.enter_context(nc.psum_tensor(f"p3_pst{i}", [128, 512], F32)) for i in range(4)]
                pnum = [ps.enter_context(nc.psum_tensor(f"p3_pn{i}", [128, 512], F32)) for i in range(2)]
                pden = [ps.enter_context(nc.psum_tensor(f"p3_pd{i}", [128, 512], F32)) for i in range(2)]
                P.dma("sp", lambda e: e.dma_start(out=mbf[:], in_=C["maskb"]), "c0", writes=["mbf"])
                P.op("dve", lambda e: e.tensor_copy(out=maskb[:], in_=mbf[:]), reads=["mbf"], writes=["maskb"])
                SC = float(DH ** -0.5)
                RS = p3rs

                def pst(slot):
                    return pstb[slot][:, 0:256]

                def sview(t, r, j):
                    if r == 1:
                        return t[:]
                    return t[:].rearrange("p (n r) -> p r n", r=r)[:, j, :]

                for h in range(nh3):
                    s = h % 2
                    P.dma("sp", lambda e, s=s, h=h: e.dma_start(out=q3[s][:], in_=qT_s[h]), f"w{s}", writes=[("q3", s)])
                    P.dma("sp", lambda e, s=s, h=h: e.dma_start(out=k3[s][:], in_=kT_s[h]), f"o{s}", writes=[("k3", s)])
                    for ri, r in enumerate(RS if p3v >= 0 else ()):
                        src = v_s[:, 128 * h:128 * h + 128].rearrange("(i n r) d -> n r i d", n=128, r=r)
                        dstv = v3[s][ri][:].rearrange("p (r i) d -> p r i d", r=r)
                        P.dma("sp", lambda e, src=src, dstv=dstv: e.dma_start(out=dstv, in_=src), f"v{s}{ri}", writes=[("v3", s, ri)])
                    P.op("pool", lambda e: e.memset(num[:], 0.0), writes=["num"])
                    P.op("pool", lambda e: e.memset(den[:], 0.0), writes=["den"])
                    tasks = []
                    for ri, r in enumerate(RS):
                        nb = 16 // r
                        for j in range(r):
                            for i in range(nb):
                                tasks.append((ri, r, j, i, nb))
                    if p3v <= 0:
                        tasks = []
                    T = len(tasks)

                    def emit_scores(t):
                        ri, r, j, i, nb = tasks[t]
                        slot = t % 4
                        nq = 256 if i < nb - 1 else 128
                        kv = sview(k3[s], r, j)
                        qv = sview(q3[s], r, j)

                        def mm(e, slot=slot, nq=nq, kv=kv, qv=qv, i=i):
                            e.matmul(pst(slot)[:, 0:nq], lhsT=kv[:, 128 * i:128 * i + 128], rhs=qv[:, 128 * i:128 * i + nq], start=True, stop=False)
                            return e.matmul(pst(slot)[:, 0:nq], lhsT=ident_bf[:], rhs=maskb[:, 0:nq], start=False, stop=True)
                        P.op("pe", mm, reads=[("q3", s), ("k3", s), "maskb"], writes=[("pst", slot)])
                        P.op("act", lambda e, slot=slot, nq=nq: e.activation(out=pT[slot][:, 0:nq], in_=pst(slot)[:, 0:nq], func=AF.Exp, scale=SC),
                             reads=[("pst", slot)], writes=[("pT", slot)])

                    def emit_pv(t):
                        ri, r, j, i, nb = tasks[t]
                        slot = t % 4
                        bank = (t // 4) % 2
                        qi = t % 4
                        vr = v3[s][ri]
                        bi = j * nb + i

                        def mm(e, slot=slot, bank=bank, qi=qi, vr=vr, bi=bi, i=i):
                            ins = None
                            for dst_, lget in ((pnum[bank], lambda b_: vr[:, b_, :]), (pden[bank], lambda b_: ones_bf[:])):
                                reg = dst_[:, 128 * qi:128 * qi + 128]
                                if i > 0:
                                    e.matmul(reg, lhsT=lget(bi - 1), rhs=pT[(slot - 1) % 4][:, 128:256], start=True, stop=False)
                                    ins = e.matmul(reg, lhsT=lget(bi), rhs=pT[slot][:, 0:128], start=False, stop=True)
                                else:
                                    ins = e.matmul(reg, lhsT=lget(bi), rhs=pT[slot][:, 0:128], start=True, stop=True)
                            return ins
                        rd = [("pT", slot), ("v3", s, ri)]
                        if i > 0:
                            rd.append(("pT", (slot - 1) % 4))
                        P.op("pe", mm, reads=rd, writes=[("pnum", bank), ("pden", bank)])
                        if qi == 3 and p3v >= 3:
                            g = t // 4
                            if r == 1:
                                tg = i // 4
                                nv = num[:, 512 * tg:512 * tg + 512]
                                dv = den[:, 512 * tg:512 * tg + 512]
                                pn, pd = pnum[bank][:], pden[bank][:]
                            elif r == 4:
                                nv = sview(num, 4, j)
                                dv = sview(den, 4, j)
                                pn, pd = pnum[bank][:], pden[bank][:]
                            else:
                                j0 = j - 3
                                nv = num[:].rearrange("p (n r) -> p r n", r=16)[:, j0:j0 + 4, :]
                                dv = den[:].rearrange("p (n r) -> p r n", r=16)[:, j0:j0 + 4, :]
                                pn = pnum[bank][:].rearrange("p (a n) -> p a n", a=4)
                                pd = pden[bank][:].rearrange("p (a n) -> p a n", a=4)
                            P.op("dve", lambda e, nv=nv, pn=pn: e.tensor_tensor(out=nv, in0=pn, in1=nv, op=ALU.add), reads=[("pnum", bank), "num"], writes=["num"])
                            P.op("dve", lambda e, dv=dv, pd=pd: e.tensor_tensor(out=dv, in0=pd, in1=dv, op=ALU.add), reads=[("pden", bank), "den"], writes=["den"])

                    for t in range(T + 1):
                        if t < T:
                            emit_scores(t)
                        if t >= 1 and p3v >= 2 and T > 0:
                            emit_pv(t - 1)
                    P.op("dve", lambda e: e.reciprocal(out=den[:], in_=den[:]), reads=["den"], writes=["den"])
                    P.op("dve", lambda e, s=s: e.tensor_tensor(out=ya[s][:], in0=num[:], in1=den[:], op=ALU.mult), reads=["num", "den"], writes=[("ya", s)])
                    P.dma("sp", lambda e, s=s, h=h: e.dma_start(out=ya_s[h], in_=ya[s][:]), f"y{s}", reads=[("ya", s)], writes=[("ya_s", h)])
                P.barrier()
        if 3 not in skip:
            phase3()


        def phase5a():
            with ExitStack() as ps:
                A = lambda n, shp, dt: ps.enter_context(nc.sbuf_tensor("p5a_" + n, shp, dt))
                yaT = A("yaT", [128, 16, 512], BF16)
                yrT = A("yrT", [128, 16, 512], BF16)
                wsta = [A(f"wsta{i}", [128, 16, 256], F32) for i in range(2)]
                wstr = [A(f"wstr{i}", [128, 16, 256], F32) for i in range(2)]
                wba = [A(f"wba{i}", [128, 16, 256], BF16) for i in range(2)]
                wbr = [A(f"wbr{i}", [128, 16, 256], BF16) for i in range(2)]
                gat = [A(f"gat{i}", [128, 512], BF16) for i in range(2)]
                grt = [A(f"grt{i}", [128, 512], BF16) for i in range(2)]
                t1 = A("t1", [128, 512], F32)
                t2 = A("t2", [128, 512], F32)
                mst = [A(f"mst{i}", [128, 512], BF16) for i in range(2)]
                pA = [ps.enter_context(nc.psum_tensor(f"p5a_A{i}", [128, 512], F32)) for i in range(2)]
                pR = [ps.enter_context(nc.psum_tensor(f"p5a_R{i}", [128, 512], F32)) for i in range(2)]
                wav = I["w_a"].rearrange("(kc p) n -> p kc n", p=128)
                wrv = I["w_r"].rearrange("(kc p) n -> p kc n", p=128)
                it = 0
                for tt in range(4):
                    tsl = slice(512 * tt, 512 * tt + 512)
                    P.dma("sp", lambda e, tsl=tsl: e.dma_start(out=yaT[:], in_=ya_s.rearrange("h p t -> p h t")[:, :, tsl]), "c0", writes=["yaT"])
                    P.dma("sp", lambda e, tsl=tsl: e.dma_start(out=yrT[:], in_=yr_s.rearrange("h p t -> p h t")[:, :, tsl]), "c1", writes=["yrT"])
                    for ct in range(8):
                        s = ct % 2
                        P.dma("sp", lambda e, s=s, ct=ct: e.dma_start(out=wsta[s][:], in_=wav[:, :, 256 * ct:256 * ct + 256]), f"w{s}", writes=[("wsta", s)])
                        P.dma("sp", lambda e, s=s, ct=ct: e.dma_start(out=wstr[s][:], in_=wrv[:, :, 256 * ct:256 * ct + 256]), f"v{s}0", writes=[("wstr", s)])
                        P.op("pool", lambda e, s=s: e.tensor_copy(out=wba[s][:], in_=wsta[s][:]), reads=[("wsta", s)], writes=[("wba", s)])
                        P.op("pool", lambda e, s=s: e.tensor_copy(out=wbr[s][:], in_=wstr[s][:]), reads=[("wstr", s)], writes=[("wbr", s)])
                        for sub in range(2):
                            cc = 2 * ct + sub
                            q = it % 2; it += 1
                            P.dma("sp", lambda e, q=q, cc=cc, tsl=tsl: e.dma_start(out=gat[q][:], in_=ga_s[cc][:, tsl]), f"o{q}", writes=[("gat", q)])
                            P.dma("sp", lambda e, q=q, cc=cc, tsl=tsl: e.dma_start(out=grt[q][:], in_=gr_s[cc][:, tsl]), f"o{q}f", writes=[("grt", q)])
                            def mmA(e, s=s, sub=sub, q=q):
                                ins = None
                                for kc in range(16):
                                    ins = e.matmul(pA[q][:], lhsT=wba[s][:, kc, 128 * sub:128 * sub + 128], rhs=yaT[:, kc, :], start=(kc == 0), stop=(kc == 15))
                                return ins
                            def mmR(e, s=s, sub=sub, q=q):
                                ins = None
                                for kc in range(16):
                                    ins = e.matmul(pR[q][:], lhsT=wbr[s][:, kc, 128 * sub:128 * sub + 128], rhs=yrT[:, kc, :], start=(kc == 0), stop=(kc == 15))
                                return ins
                            P.op("pe", mmA, reads=[("wba", s), "yaT"], writes=[("pA", q)])
                            P.op("pe", mmR, reads=[("wbr", s), "yrT"], writes=[("pR", q)])
                            P.op("dve", lambda e, q=q: e.tensor_tensor(out=t1[:], in0=pA[q][:], in1=gat[q][:], op=ALU.mult), reads=[("pA", q), ("gat", q)], writes=["t1"])
                            P.op("dve", lambda e, q=q: e.tensor_tensor(out=t2[:], in0=pR[q][:], in1=grt[q][:], op=ALU.mult), reads=[("pR", q), ("grt", q)], writes=["t2"])
                            P.op("pool", lambda e, q=q: e.tensor_tensor(out=mst[q][:], in0=t1[:], in1=t2[:], op=ALU.add), reads=["t1", "t2"], writes=[("mst", q)])
                            P.dma("sp", lambda e, q=q, cc=cc, tsl=tsl: e.dma_start(out=m_s[cc][:, tsl], in_=mst[q][:]), f"y{q}", reads=[("mst", q)], writes=[("m_s", cc, tt)])
                P.barrier()

        def phase5b():
            with ExitStack() as ps:
                A = lambda n, shp, dt: ps.enter_context(nc.sbuf_tensor("p5b_" + n, shp, dt))
                wo = A("wo", [128, 16, D], BF16)
                wst5 = [A(f"wst{i}", [128, 16, 256], F32) for i in range(2)]
                gtm = A("gtm", [128, D], F32)
                G2 = A("G2", [128, D], F32)
                shf = A("shf", [128, D], F32)
                mT = [A(f"mT{i}", [128, 16, 128], BF16) for i in range(2)]
                xt5 = [A(f"xt{i}", [128, D], F32) for i in range(2)]
                ht = A("ht", [128, D], F32)
                tf5 = A("tf", [128, D], F32)
                u2f = A("u2f", [128, D], F32)
                u2b = [A(f"u2b{i}", [128, D], BF16) for i in range(2)]
                junk5 = A("junk", [128, D], BF16)
                st5 = [A(f"st{i}", [128, 4], F32) for i in range(2)]
                u2T = A("u2T", [128, 16, 128], F32)
                wr = A("wr", [128, 16, 36], F32)
                brt = A("brt", [128, 36], F32)
                lgall = A("lgall", [128, 16, 36], F32)
                pM = [ps.enter_context(nc.psum_tensor(f"p5b_M{i}", [128, 512], F32)) for i in range(4)]
                pT5 = ps.enter_context(nc.psum_tensor("p5b_T", [128, 8, 128], F32))
                pL = ps.enter_context(nc.psum_tensor("p5b_L", [128, 36], F32))
                wov = I["w_o"].rearrange("(kc p) n -> p kc n", p=128)
                for ct in range(8):
                    s = ct % 2
                    P.dma("sp", lambda e, s=s, ct=ct: e.dma_start(out=wst5[s][:], in_=wov[:, :, 256 * ct:256 * ct + 256]), f"w{s}", writes=[("wst5", s)])
                    P.op("pool", lambda e, s=s, ct=ct: e.tensor_copy(out=wo[:, :, 256 * ct:256 * ct + 256], in_=wst5[s][:]), reads=[("wst5", s)], writes=["wo"])
                P.dma("sp", lambda e: e.dma_start(out=gtm[:], in_=modbc[:, 2 * D:3 * D]), "c0", writes=["gtm"])
                P.dma("sp", lambda e: e.dma_start(out=shf[:], in_=modbc[:, 3 * D:4 * D]), "c1", writes=["shf"])
                P.dma("sp", lambda e: e.dma_start(out=tf5[:], in_=modbc[:, 4 * D:5 * D]), "c2", writes=["tf5"])
                P.dma("sp", lambda e: e.dma_start(out=G2[:], in_=I["ffn_norm_g"].partition_broadcast(128)), "c3", writes=["G2"])
                P.op("dve", lambda e: e.scalar_tensor_tensor(out=G2[:], in0=tf5[:], scalar=1.0, in1=G2[:], op0=ALU.add, op1=ALU.mult), reads=["tf5", "G2"], writes=["G2"])
                P.dma("sp", lambda e: e.dma_start(out=wr[:], in_=I["w_router"].rearrange("(kc p) n -> p kc n", p=128)), "o0", writes=["wr"])
                P.dma("sp", lambda e: e.dma_start(out=brt[:], in_=I["b_router"].partition_broadcast(128)), "o1", writes=["brt"])
                for i in range(16):
                    s = i % 2
                    tsl = slice(128 * i, 128 * i + 128)
                    P.dma("sp", lambda e, s=s, tsl=tsl: e.dma_start(out=mT[s][:], in_=m_s.rearrange("h p t -> p h t")[:, :, tsl]), f"v{s}0", writes=[("mT", s)])
                    P.dma("sp", lambda e, s=s, tsl=tsl: e.dma_start(out=xt5[s][:], in_=I["x"][tsl, :]), f"v{s}1", writes=[("xt5", s)])
                    for cc in range(4):
                        csl = slice(512 * cc, 512 * cc + 512)
                        def mmM(e, s=s, cc=cc, csl=csl):
                            ins = None
                            for kc in range(16):
                                ins = e.matmul(pM[cc][:], lhsT=mT[s][:, kc, :], rhs=wo[:, kc, csl], start=(kc == 0), stop=(kc == 15))
                            return ins
                        P.op("pe", mmM, reads=[("mT", s), "wo"], writes=[("pM", cc)])
                        P.op("dve", lambda e, cc=cc, csl=csl: e.tensor_tensor(out=tf5[:, csl], in0=pM[cc][:], in1=gtm[:, csl], op=ALU.mult), reads=[("pM", cc), "gtm"], writes=[("tf5", cc)])
                    P.op("pool", lambda e, s=s: e.tensor_tensor(out=ht[:], in0=tf5[:], in1=xt5[s][:], op=ALU.add), reads=[("tf5", 0), ("tf5", 1), ("tf5", 2), ("tf5", 3), ("xt5", s)], writes=["ht"])
                    P.dma("sp", lambda e, tsl=tsl: e.dma_start(out=h_s[tsl, :], in_=ht[:]), "y0", reads=["ht"], writes=[("h_s", i)])
                    P.op("act", lambda e, s=s: e.activation(out=junk5[:], in_=ht[:], func=AF.Square, accum_out=st5[s][:, 0:1]), reads=["ht"], writes=["junk5", ("st5", s)])
                    P.op("act", lambda e, s=s: e.activation(out=st5[s][:, 1:2], in_=st5[s][:, 0:1], func=AF.Sqrt, scale=1.0 / D, bias=EPS), reads=[("st5", s)], writes=[("st5", s)])
                    P.op("dve", lambda e, s=s: e.reciprocal(out=st5[s][:, 2:3], in_=st5[s][:, 1:2]), reads=[("st5", s)], writes=[("st5", s)])
                    P.op("dve", lambda e, s=s: e.scalar_tensor_tensor(out=u2f[:], in0=ht[:], scalar=st5[s][:, 2:3], in1=G2[:], op0=ALU.mult, op1=ALU.mult),
                         reads=["ht", ("st5", s), "G2"], writes=["u2f"])
                    P.op("pool", lambda e: e.tensor_tensor(out=u2f[:], in0=u2f[:], in1=shf[:], op=ALU.add), reads=["u2f", "shf"], writes=["u2f"])
                    P.op("act", lambda e, s=s: e.activation(out=u2b[s][:], in_=u2f[:], func=AF.Copy), reads=["u2f"], writes=[("u2b", s)])
                    P.dma("sp", lambda e, s=s, tsl=tsl: e.dma_start(out=u2_s[tsl, :], in_=u2b[s][:]), f"y1{s}", reads=[("u2b", s)], writes=[("u2_s", i)])
                    for hf in range(2):
                        def tr5(e, hf=hf):
                            ins = None
                            for k8 in range(8):
                                kc = 8 * hf + k8
                                ins = e.transpose(out=pT5[:, k8, :], in_=u2f[:, 128 * kc:128 * kc + 128], identity=ident_f[:])
                            return ins
                        P.op("pe", tr5, reads=["u2f"], writes=["pT5"])
                        P.op("act", lambda e, hf=hf: e.activation(out=u2T[:, 8 * hf:8 * hf + 8, :], in_=pT5[:], func=AF.Copy), reads=["pT5"], writes=[("u2T", hf)])
                    def mmL(e):
                        ins = None
                        for kc in range(16):
                            ins = e.matmul(pL[:], lhsT=u2T[:, kc, :], rhs=wr[:, kc, :], start=(kc == 0), stop=(kc == 15))
                        return ins
                    P.op("pe", mmL, reads=[("u2T", 0), ("u2T", 1), "wr"], writes=["pL"])
                    P.op("dve", lambda e, i=i: e.tensor_tensor(out=lgall[:, i, :], in0=pL[:], in1=brt[:], op=ALU.add), reads=["pL", "brt"], writes=["lgall"])
                P.dma("sp", lambda e: e.dma_start(out=lg_s, in_=lgall[:]), "y2", reads=["lgall"], writes=["lg_s"])
                P.barrier()

        def phase6():
            with ExitStack() as ps:
                A = lambda n, shp, dt: ps.enter_context(nc.sbuf_tensor("p6_" + n, shp, dt))
                lg = A("lg", [128, 16, 36], F32)
                iota_e = A("iota_e", [128, 32], F32)
                ltf = A("ltf", [128, 128], F32)
                ltb = A("ltb", [128, 128], BF16)
                thrb = A("thrb", [128, 64], F32)
                iotap = A("iotap", [128, 1], F32)
                eidx = A("eidx", [128, 32], F32)
                E = A("E", [128, 32, 32], BF16)
                gm = A("gm", [128, 1], F32)
                sh = A("sh", [128, 4], F32)
                ex = A("ex", [128, 4], F32)
                se = A("se", [128, 1], F32)
                pg = A("pg", [128, 1], F32)
                pen = A("pen", [128, 4], F32)
                lem = A("lem", [128, 32], F32)
                mx8 = A("mx8", [128, 8], F32)
                ix8 = A("ix8", [128, 8], U32)
                dl = A("dl", [128, 4], F32)
                base = A("base", [128, 32, 32], F32)
                tmp = A("tmp", [128, 32, 32], F32)
                cnt = A("cnt", [128, 32], F32)
                ci = A("ci", [128, 32], I32)
                padded = A("padded", [128, 32], F32)
                pend = A("pend", [128, 32], F32)
                pstart = A("pstart", [128, 32], F32)
                dest = A("dest", [128, 32], F32)
                acc = A("acc", [128, 64], F32)
                of = A("of", [128, 64], F32)
                u2t = [A(f"u2t{i}", [128, D], BF16) for i in range(2)]
                pPre = ps.enter_context(nc.psum_tensor("p6_pre", [128, 1024], F32))
                pTot = ps.enter_context(nc.psum_tensor("p6_tot", [128, 1024], F32))
                P.dma("sp", lambda e: e.dma_start(out=lg[:], in_=lg_s), "c0", writes=["lg"])
                P.dma("sp", lambda e: e.dma_start(out=iota_e[:], in_=C["iota_e"]), "c1", writes=["iota_e"])
                P.dma("sp", lambda e: e.dma_start(out=ltf[:], in_=C["ltri"]), "c2", writes=["ltf"])
                P.dma("sp", lambda e: e.dma_start(out=thrb[:], in_=C["thrb"]), "c3", writes=["thrb"])
                P.dma("sp", lambda e: e.dma_start(out=iotap[:], in_=C["iota_p"]), "o0", writes=["iotap"])
                CH = ["p6chain"]

                def X(eng, fn):
                    P.op(eng, fn, reads=CH, writes=CH)
                P.op("dve", lambda e: e.tensor_copy(out=ltb[:], in_=ltf[:]), reads=["ltf", "lg", "iota_e", "thrb", "iotap"], writes=CH)
                for t in range(16):
                    lgt = lg[:, t, 0:4]
                    let = lg[:, t, 4:36]
                    X("dve", lambda e, lgt=lgt: e.tensor_reduce(out=gm[:], in_=lgt, axis=AX.X, op=ALU.max))
                    X("dve", lambda e, lgt=lgt: e.tensor_scalar(out=sh[:], in0=lgt, scalar1=gm[:, 0:1], scalar2=None, op0=ALU.subtract))
                    X("dve", lambda e: e.tensor_scalar(out=pen[:], in0=sh[:], scalar1=0.0, scalar2=None, op0=ALU.is_equal))
                    X("dve", lambda e: e.tensor_scalar(out=pen[:], in0=pen[:], scalar1=-1.0, scalar2=1e30, op0=ALU.add, op1=ALU.mult))
                    for g in range(4):
                        X("dve", lambda e, g=g, let=let: e.tensor_scalar(out=lem[:, 8 * g:8 * g + 8], in0=let[:, 8 * g:8 * g + 8], scalar1=pen[:, g:g + 1], scalar2=None, op0=ALU.add))
                    X("act", lambda e: e.activation(out=ex[:], in_=sh[:], func=AF.Exp, accum_out=se[:, 0:1]))
                    X("dve", lambda e: e.max(out=mx8[:], in_=lem[:]))
                    X("dve", lambda e: e.max_index(out=ix8[:], in_max=mx8[:], in_values=lem[:]))
                    X("dve", lambda e, t=t: e.tensor_copy(out=eidx[:, 2 * t:2 * t + 2], in_=ix8[:, 0:2]))
                    X("dve", lambda e: e.tensor_tensor(out=dl[:, 0:1], in0=mx8[:, 1:2], in1=mx8[:, 0:1], op=ALU.subtract))
                    X("act", lambda e: e.activation(out=dl[:, 1:2], in_=dl[:, 0:1], func=AF.Exp))
                    X("dve", lambda e: e.reciprocal(out=pg[:], in_=se[:]))
                    X("dve", lambda e: e.tensor_scalar(out=dl[:, 2:3], in0=dl[:, 1:2], scalar1=1.0, scalar2=None, op0=ALU.add))
                    X("dve", lambda e: e.reciprocal(out=dl[:, 3:4], in_=dl[:, 2:3]))
                    X("dve", lambda e, t=t: e.tensor_tensor(out=wts[:, 2 * t:2 * t + 1], in0=pg[:], in1=dl[:, 3:4], op=ALU.mult))
                    X("dve", lambda e, t=t: e.tensor_tensor(out=wts[:, 2 * t + 1:2 * t + 2], in0=wts[:, 2 * t:2 * t + 1], in1=dl[:, 1:2], op=ALU.mult))
                for g in range(32):
                    X("dve", lambda e, g=g: e.tensor_scalar(out=E[:, g, :], in0=iota_e[:], scalar1=eidx[:, g:g + 1], scalar2=None, op0=ALU.is_equal))
                Ef = E[:].rearrange("p g e -> p (g e)")
                X("pe", lambda e: e.matmul(pPre[:, 0:512], lhsT=ltb[:], rhs=Ef[:, 0:512], start=True, stop=True))
                X("pe", lambda e: e.matmul(pPre[:, 512:1024], lhsT=ltb[:], rhs=Ef[:, 512:1024], start=True, stop=True))
                X("pe", lambda e: e.matmul(pTot[:, 0:512], lhsT=ones_bf[:], rhs=Ef[:, 0:512], start=True, stop=True))
                X("pe", lambda e: e.matmul(pTot[:, 512:1024], lhsT=ones_bf[:], rhs=Ef[:, 512:1024], start=True, stop=True))
                pTv = pTot[:].rearrange("p (g e) -> p g e", e=32)
                pPv = pPre[:].rearrange("p (g e) -> p g e", e=32)
                X("dve", lambda e: e.memset(base[:, 0, :], 0.0))
                for g in range(1, 32):
                    X("dve", lambda e, g=g: e.tensor_tensor(out=base[:, g, :], in0=pTv[:, g - 1, :], in1=base[:, g - 1, :], op=ALU.add))
                X("dve", lambda e: e.tensor_tensor(out=cnt[:], in0=pTv[:, 31, :], in1=base[:, 31, :], op=ALU.add))
                X("dve", lambda e: e.memset(padded[:], 0.0))
                for jj in range(32):
                    X("dve", lambda e, jj=jj: e.scalar_tensor_tensor(out=padded[:], in0=cnt[:], scalar=128.0 * jj, in1=padded[:], op0=ALU.is_gt, op1=ALU.add))
                X("dve", lambda e: e.tensor_scalar(out=padded[:], in0=padded[:], scalar1=128.0, scalar2=None, op0=ALU.mult))
                X("dve", lambda e: e.tensor_tensor_scan(out=pend[:], data0=ones_f[:, 0:32], data1=padded[:], initial=0.0, op0=ALU.mult, op1=ALU.add))
                X("dve", lambda e: e.tensor_tensor(out=pstart[:], in0=pend[:], in1=padded[:], op=ALU.subtract))
                X("dve", lambda e: e.tensor_tensor(out=tmp[:, 0:16, :], in0=pPv[:, 0:16, :], in1=base[:, 0:16, :], op=ALU.add))
                X("dve", lambda e: e.tensor_tensor(out=tmp[:, 16:32, :], in0=pPv[:, 16:32, :], in1=base[:, 16:32, :], op=ALU.add))
                for g in range(32):
                    X("dve", lambda e, g=g: e.tensor_tensor(out=tmp[:, g, :], in0=tmp[:, g, :], in1=pstart[:], op=ALU.add))
                X("dve", lambda e: e.tensor_tensor(out=tmp[:], in0=tmp[:], in1=E[:], op=ALU.mult))
                X("dve", lambda e: e.tensor_reduce(out=dest[:], in_=tmp[:], axis=AX.X, op=ALU.add))
                X("dve", lambda e: e.tensor_copy(out=desti[:], in_=dest[:]))
                X("dve", lambda e: e.memset(acc[:], 0.0))
                for ee in range(32):
                    X("dve", lambda e, ee=ee: e.scalar_tensor_tensor(out=acc[:], in0=thrb[:], scalar=pend[:, ee:ee + 1], in1=acc[:], op0=ALU.is_ge, op1=ALU.add))
                X("dve", lambda e: e.tensor_scalar(out=acc[:], in0=acc[:], scalar1=31.0, scalar2=None, op0=ALU.min))
                X("dve", lambda e: e.tensor_scalar(out=of[:], in0=acc[:], scalar1=2048.0, scalar2=iotap[:, 0:1], op0=ALU.mult, op1=ALU.add))
                X("dve", lambda e: e.tensor_copy(out=offs_gu[:], in_=of[:]))
                X("dve", lambda e: e.tensor_scalar(out=of[:], in0=acc[:], scalar1=1024.0, scalar2=iotap[:, 0:1], op0=ALU.mult, op1=ALU.add))
                P.op("dve", lambda e: e.tensor_copy(out=offs_d[:], in_=of[:]), reads=CH, writes=CH + ["desti", "offs"])
                if debug:
                    rt = A("rt", [128, 16, 8], F32)
                    X("dve", lambda e: e.memset(rt[:], 0.0))
                    X("dve", lambda e: e.tensor_copy(out=rt[:, :, 0:2], in_=eidx[:].rearrange("p (t k) -> p t k", k=2)))
                    X("dve", lambda e: e.tensor_copy(out=rt[:, :, 2:4], in_=wts[:].rearrange("p (t k) -> p t k", k=2)))
                    X("dve", lambda e: e.tensor_copy(out=rt[:, :, 4:6], in_=dest[:].rearrange("p (t k) -> p t k", k=2)))
                    P.dma("sp", lambda e: e.dma_start(out=rt_s, in_=rt[:]), "o1", reads=CH, writes=["rt_s"])
                for t in range(16):
                    s = t % 2
                    P.dma("sp", lambda e, s=s, t=t: e.dma_start(out=u2t[s][:], in_=u2_s[128 * t:128 * t + 128, :]), f"w{s}", writes=[("u2t", s)])
                    for k in range(2):
                        g = 2 * t + k
                        P.dma("pool", lambda e, s=s, g=g: e.indirect_dma_start(out=xslot, out_offset=bass.IndirectOffsetOnAxis(ap=desti[:, g:g + 1], axis=0),
                                                                              in_=u2t[s][:], in_offset=None),
                              f"v{s}{k}", reads=[("u2t", s), "desti"], writes=[("xslot", g)])
                P.barrier()

        def phase7():
            with ExitStack() as ps:
                A = lambda n, shp, dt: ps.enter_context(nc.sbuf_tensor("p7_" + n, shp, dt))
                xt7 = [A(f"xt{i}", [128, D], BF16) for i in range(2)]
                xT7 = [A(f"xT{i}", [128, 16, 128], BF16) for i in range(2)]
                guf = [A(f"guf{i}", [128, 1024], F32) for i in range(4)]
                gub = [A(f"gub{i}", [128, 1024], BF16) for i in range(4)]
                dnf = [A(f"dnf{i}", [128, D], F32) for i in range(2)]
                dnb = [A(f"dnb{i}", [128, D], BF16) for i in range(2)]
                sg7 = A("sg", [128, 1024], F32)
                hd7 = A("hd", [128, 1024], BF16)
                hT7 = A("hT", [128, 8, 128], BF16)
                ys7 = [A(f"ys{i}", [128, D], F32) for i in range(2)]
                pGU = ps.enter_context(nc.psum_tensor("p7_GU", [128, 4, 512], F32))
                pTr = [ps.enter_context(nc.psum_tensor(f"p7_Tr{i}", [128, 8, 128], BF16)) for i in range(2)]
                gi = 0
                di = 0
                tri = 0
                for b in range(nb7):
                    s = b % 2
                    P.dma("sp", lambda e, s=s, b=b: e.dma_start(out=xt7[s][:], in_=xslot[128 * b:128 * b + 128, :]), f"w{s}", writes=[("xt7", s)])
                    for hf in range(2):
                        w = tri % 2; tri += 1
                        def trx(e, s=s, hf=hf, w=w):
                            ins = None
                            for k8 in range(8):
                                kc = 8 * hf + k8
                                ins = e.transpose(out=pTr[w][:, k8, :], in_=xt7[s][:, 128 * kc:128 * kc + 128], identity=ident_bf[:])
                            return ins
                        P.op("pe", trx, reads=[("xt7", s)], writes=[("pTr", w)])
                        P.op("act", lambda e, s=s, hf=hf, w=w: e.activation(out=xT7[s][:, 8 * hf:8 * hf + 8, :], in_=pTr[w][:], func=AF.Copy),
                             reads=[("pTr", w)], writes=[("xT7", s, hf)])
                    for kc in range(16):
                        for which in range(2):
                            r = gi % 4; gi += 1
                            src = I["w_gate"] if which == 0 else I["w_up"]
                            P.dma("pool", lambda e, r=r, b=b, kc=kc, src=src: e.indirect_dma_start(
                                out=guf[r][:], out_offset=None, in_=src, in_offset=bass.IndirectOffsetOnAxis(ap=offs_gu[:, b:b + 1], axis=0),
                                element_offset=kc * 128 * 1024), f"g{r}", reads=["offs"], writes=[("guf", r)])
                            ceng = "act" if (gi % 2 == 0) else "dve"
                            if ceng == "act":
                                P.op("act", lambda e, r=r: e.activation(out=gub[r][:], in_=guf[r][:], func=AF.Copy), reads=[("guf", r)], writes=[("gub", r)])
                            else:
                                P.op("dve", lambda e, r=r: e.tensor_copy(out=gub[r][:], in_=guf[r][:]), reads=[("guf", r)], writes=[("gub", r)])
                            def mmg(e, s=s, r=r, kc=kc, which=which):
                                e.matmul(pGU[:, 2 * which, :], lhsT=xT7[s][:, kc, :], rhs=gub[r][:, 0:512], start=(kc == 0), stop=(kc == 15))
                                return e.matmul(pGU[:, 2 * which + 1, :], lhsT=xT7[s][:, kc, :], rhs=gub[r][:, 512:1024], start=(kc == 0), stop=(kc == 15))
                            P.op("pe", mmg, reads=[("gub", r), ("xT7", s, 0), ("xT7", s, 1)], writes=[("pgu", 2 * which), ("pgu", 2 * which + 1)])
                    P.op("act", lambda e: e.activation(out=sg7[:], in_=pGU[:, 0:2, :].rearrange("p a n -> p (a n)"), func=AF.Silu), reads=[("pgu", 0), ("pgu", 1)], writes=["sg7"])
                    P.op("dve", lambda e: e.tensor_tensor(out=hd7[:], in0=pGU[:, 2:4, :].rearrange("p a n -> p (a n)"), in1=sg7[:], op=ALU.mult),
                         reads=[("pgu", 2), ("pgu", 3), "sg7"], writes=["hd7"])
                    w = tri % 2; tri += 1
                    def trh(e, w=w):
                        ins = None
                        for k8 in range(8):
                            ins = e.transpose(out=pTr[w][:, k8, :], in_=hd7[:, 128 * k8:128 * k8 + 128], identity=ident_bf[:])
                        return ins
                    P.op("pe", trh, reads=["hd7"], writes=[("pTr", w)])
                    P.op("act", lambda e, w=w: e.activation(out=hT7[:], in_=pTr[w][:], func=AF.Copy), reads=[("pTr", w)], writes=["hT7"])
                    for k2 in range(8):
                        r = di % 2; di += 1
                        P.dma("pool", lambda e, r=r, b=b, k2=k2: e.indirect_dma_start(
                            out=dnf[r][:], out_offset=None, in_=I["w_down"], in_offset=bass.IndirectOffsetOnAxis(ap=offs_d[:, b:b + 1], axis=0),
                            element_offset=k2 * 128 * D), f"d{r}", reads=["offs"], writes=[("dnf", r)])
                        P.op("act", lambda e, r=r: e.activation(out=dnb[r][:, 0:1024], in_=dnf[r][:, 0:1024], func=AF.Copy), reads=[("dnf", r)], writes=[("dnb", r, 0)])
                        P.op("dve", lambda e, r=r: e.tensor_copy(out=dnb[r][:, 1024:2048], in_=dnf[r][:, 1024:2048]), reads=[("dnf", r)], writes=[("dnb", r, 1)])
                        def mmd(e, r=r, k2=k2):
                            ins = None
                            for cc in range(4):
                                ins = e.matmul(pGU[:, cc, :], lhsT=hT7[:, k2, :], rhs=dnb[r][:, 512 * cc:512 * cc + 512], start=(k2 == 0), stop=(k2 == 7))
                            return ins
                        P.op("pe", mmd, reads=[("dnb", r, 0), ("dnb", r, 1), "hT7"], writes=[("pgu", 0), ("pgu", 1), ("pgu", 2), ("pgu", 3)])
                    P.op("act", lambda e, s=s: e.activation(out=ys7[s][:, 0:1024], in_=pGU[:, 0:2, :].rearrange("p a n -> p (a n)"), func=AF.Copy),
                         reads=[("pgu", 0), ("pgu", 1)], writes=[("ys7", s, 0)])
                    P.op("dve", lambda e, s=s: e.tensor_copy(out=ys7[s][:, 1024:2048], in_=pGU[:, 2:4, :].rearrange("p a n -> p (a n)")),
                         reads=[("pgu", 2), ("pgu", 3)], writes=[("ys7", s, 1)])
                    P.dma("sp", lambda e, s=s, b=b: e.dma_start(out=yslot[128 * b:128 * b + 128, :], in_=ys7[s][:]), f"o{s}",
                          reads=[("ys7", s, 0), ("ys7", s, 1)], writes=[("yslot", b)])
                P.barrier()

        def phase8(mode):
            full = (mode == 'full')
            with ExitStack() as ps:
                Gf = ps.enter_context(nc.sbuf_tensor("p8_G", [128, D], F32))
                gtf = ps.enter_context(nc.sbuf_tensor("p8_gtf", [128, D], F32))
                xt8 = [ps.enter_context(nc.sbuf_tensor(f"p8_x{i}", [128, D], F32)) for i in range(2)]
                y08 = [ps.enter_context(nc.sbuf_tensor(f"p8_y0{i}", [128, D], F32)) for i in range(2)]
                y18 = [ps.enter_context(nc.sbuf_tensor(f"p8_y1{i}", [128, D], F32)) for i in range(2)]
                ot8 = [ps.enter_context(nc.sbuf_tensor(f"p8_o{i}", [128, D], F32)) for i in range(2)]
                junk8 = ps.enter_context(nc.sbuf_tensor("p8_junk", [128, D], BF16))
                st8 = [ps.enter_context(nc.sbuf_tensor(f"p8_st{i}", [128, 4], F32)) for i in range(2)]
                P.dma("sp", lambda e: e.dma_start(out=Gf[:], in_=I["final_norm_g"].partition_broadcast(128)), "c0", writes=["Gf"])
                if full:
                    P.dma("sp", lambda e: e.dma_start(out=gtf[:], in_=modbc[:, 5 * D:6 * D]), "c1", writes=["gtf"])
                hsrc = I["x"] if mode == 'x' else h_s
                for i in range(16):
                    s = i % 2
                    P.dma("sp", lambda e, s=s, i=i: e.dma_start(out=xt8[s][:], in_=hsrc[128 * i:128 * i + 128, :]), f"w{s}", writes=[("xt8", s)])
                    if full:
                        P.dma("pool", lambda e, s=s, i=i: e.indirect_dma_start(out=y08[s][:], out_offset=None, in_=yslot,
                                                                              in_offset=bass.IndirectOffsetOnAxis(ap=desti[:, 2 * i:2 * i + 1], axis=0)),
                              f"g{s}", writes=[("y08", s)])
                        P.dma("pool", lambda e, s=s, i=i: e.indirect_dma_start(out=y18[s][:], out_offset=None, in_=yslot,
                                                                              in_offset=bass.IndirectOffsetOnAxis(ap=desti[:, 2 * i + 1:2 * i + 2], axis=0)),
                              f"d{s}", writes=[("y18", s)])
                        P.op("dve", lambda e, s=s, i=i: e.tensor_scalar(out=y08[s][:], in0=y08[s][:], scalar1=wts[:, 2 * i:2 * i + 1], scalar2=None, op0=ALU.mult),
                             reads=[("y08", s)], writes=[("y08", s)])
                        P.op("dve", lambda e, s=s, i=i: e.scalar_tensor_tensor(out=y18[s][:], in0=y18[s][:], scalar=wts[:, 2 * i + 1:2 * i + 2], in1=y08[s][:], op0=ALU.mult, op1=ALU.add),
                             reads=[("y08", s), ("y18", s)], writes=[("y18", s)])
                        P.op("pool", lambda e, s=s: e.tensor_tensor(out=y18[s][:], in0=y18[s][:], in1=gtf[:], op=ALU.mult), reads=[("y18", s), "gtf"], writes=[("y18", s)])
                        P.op("pool", lambda e, s=s: e.tensor_tensor(out=xt8[s][:], in0=xt8[s][:], in1=y18[s][:], op=ALU.add), reads=[("y18", s), ("xt8", s)], writes=[("xt8", s)])
                    P.op("act", lambda e, s=s: e.activation(out=junk8[:], in_=xt8[s][:], func=AF.Square, accum_out=st8[s][:, 0:1]),
                         reads=[("xt8", s)], writes=["junk8", ("st8", s)])
                    P.op("act", lambda e, s=s: e.activation(out=st8[s][:, 1:2], in_=st8[s][:, 0:1], func=AF.Sqrt, scale=1.0 / D, bias=EPS),
                         reads=[("st8", s)], writes=[("st8", s)])
                    P.op("dve", lambda e, s=s: e.reciprocal(out=st8[s][:, 2:3], in_=st8[s][:, 1:2]), reads=[("st8", s)], writes=[("st8", s)])
                    P.op("dve", lambda e, s=s: e.scalar_tensor_tensor(out=ot8[s][:], in0=xt8[s][:], scalar=st8[s][:, 2:3], in1=Gf[:], op0=ALU.mult, op1=ALU.mult),
                         reads=[("xt8", s), ("st8", s), "Gf"], writes=[("ot8", s)])
                    P.dma("sp", lambda e, s=s, i=i: e.dma_start(out=out[128 * i:128 * i + 128, :], in_=ot8[s][:]), f"o{s}", reads=[("ot8", s)], writes=[("out", i)])
                P.barrier()
        if stop_after >= 4 and 4 not in skip:
            phase4()
        if stop_after >= 5 and 5 not in skip:
            phase5a()
        if stop_after >= 5 and 55 not in skip:
            phase5b()
        if stop_after >= 6 and 6 not in skip:
            phase6()
        if stop_after >= 7 and 7 not in skip:
            phase7()
        phase8('full' if (stop_after >= 7 and 7 not in skip) else ('h' if (stop_after >= 5 and 55 not in skip) else 'x'))
        P.barrier(); P.emit(); return nc
        if debug and stop_after <= 1:
            uT_d = scratch('s_uT', [128, 16, S], BF16)
            P.dma('sp', lambda e: e.dma_start(out=uT_d, in_=uT[:]), 'c0', reads=['uT'], writes=['uT_d'])
            P.barrier()
        P.emit()
    return nc


def _prep(inp, b):
    m = {}
    m["x"] = np.ascontiguousarray(inp["x"][b])
    m["c_t"] = np.ascontiguousarray(np.asarray(inp["c"][b]).reshape(16, 128).T)
    m["pos"] = np.ascontiguousarray(inp["positions"][b])
    m["ada_w"] = inp["ada_w"][0]
    m["ada_b"] = inp["ada_b"][0]
    m["mix_norm_g"] = inp["mix_norm_g"][0]
    m["w_in"] = inp["w_in"][0]
    m["w_a"] = inp["w_attn_branch"][0]
    m["w_r"] = inp["w_rec_branch"][0]
    m["w_o"] = inp["w_mix_out"][0]
    m["rec_norm_g"] = np.ascontiguousarray(np.asarray(inp["rec_norm_g"][0]).reshape(128, 1))
    m["lb_logits_t"] = np.ascontiguousarray(np.asarray(inp["rec_lb_logits"]).reshape(2, 16, 128).transpose(2, 0, 1))
    m["ffn_norm_g"] = inp["ffn_norm_g"][0]
    m["w_router"] = np.ascontiguousarray(np.concatenate([inp["router_group_w"][0], inp["router_expert_w"][0]], axis=1))
    m["b_router"] = np.ascontiguousarray(np.concatenate([inp["router_group_b"][0], inp["router_expert_b"][0]]))
    m["w_gate"] = np.asarray(inp["expert_w_gate"][0]).reshape(32 * D, 1024)
    m["w_up"] = np.asarray(inp["expert_w_up"][0]).reshape(32 * D, 1024)
    m["w_down"] = np.asarray(inp["expert_w_down"][0]).reshape(32 * 1024, D)
    m["final_norm_g"] = inp["final_norm_g"]
    return m


STOP_AFTER = 7


def kernel(**inputs):
    inp = {k: np.asarray(v) for k, v in inputs.items()}
    consts = {"c_" + k: v for k, v in host_consts().items()}
    nc = build(stop_after=STOP_AFTER)
    in_maps = []
    for b in range(8):
        m = _prep(inp, b)
        m.update(consts)
        in_maps.append(m)
    res = run_bass_kernel_spmd(nc, in_maps, core_ids=list(range(8)))
    return np.stack([np.asarray(r["out"], dtype=np.float32) for r in res.results], axis=0)
```

```python
import numpy as np
from contextlib import ExitStack
import concourse.bass as bass
import concourse.mybir as mybir
from concourse.bass_utils import run_bass_kernel_spmd

F32 = mybir.dt.float32
BF16 = mybir.dt.bfloat16
I32 = mybir.dt.int32
U32 = mybir.dt.uint32
AF = mybir.ActivationFunctionType
ALU = mybir.AluOpType
AX = mybir.AxisListType

D = 2048
S = 2048
NH = 16
DH = 128
INW = 18432
EPS = 1e-6
NEG = -30000.0
ROPE_THETA = 500000.0


class Prog:
    COMPUTE = ("pe", "act", "dve", "pool")
    ENG = ("pe", "act", "dve", "pool", "sp")

    def __init__(self, nc, es):
        self.nc, self.es = nc, es
        self.streams = {e: [] for e in self.ENG}
        self.esem = {e: es.enter_context(nc.semaphore(f"sem_{e}")) for e in self.COMPUTE}
        self.ecount = {e: 0 for e in self.COMPUTE}
        self.known = {e: {} for e in self.ENG}
        self.state = {}
        self.dsem = {}

    def _need(self, eng, ev):
        if ev is None:
            return
        name, sem, val, owner = ev
        if owner == eng and eng == "pe":
            return
        if self.known[eng].get(name, 0) >= val:
            return
        self.known[eng][name] = val
        self.streams[eng].append(("wait", sem, val))

    def _deps(self, eng, reads, writes):
        for k in reads:
            st = self.state.get(k)
            if st:
                self._need(eng, st["w"])
        for k in writes:
            st = self.state.get(k)
            if st:
                self._need(eng, st["w"])
                for ev in st["r"]:
                    self._need(eng, ev)

    def _commit(self, ev, reads, writes):
        for k in reads:
            st = self.state.setdefault(k, {"w": None, "r": []})
            st["r"].append(ev)
            if len(st["r"]) > 24:
                st["r"] = st["r"][-24:] if False else st["r"]
        for k in writes:
            self.state[k] = {"w": ev, "r": []}

    def op(self, eng, fn, reads=(), writes=()):
        self._deps(eng, reads, writes)
        self.ecount[eng] += 1
        ev = (f"e_{eng}", self.esem[eng], self.ecount[eng], eng)
        self.streams[eng].append(("op", fn, self.esem[eng], 1))
        self._commit(ev, reads, writes)
        return ev

    def dma(self, q, fn, semname, reads=(), writes=()):
        self._deps(q, reads, writes)
        if semname not in self.dsem:
            self.dsem[semname] = [self.es.enter_context(self.nc.semaphore(f"d_{semname}")), 0]
        d = self.dsem[semname]
        d[1] += 16
        ev = (f"d_{semname}", d[0], d[1], "dma")
        self.streams[q].append(("op", fn, d[0], 16))
        self._commit(ev, reads, writes)
        return ev

    def barrier(self):
        evs = []
        for e in self.COMPUTE:
            if self.ecount[e]:
                evs.append((f"e_{e}", self.esem[e], self.ecount[e], e))
        for name, (sem, cnt) in self.dsem.items():
            if cnt:
                evs.append((f"d_{name}", sem, cnt, "dma"))
        for eng in self.ENG:
            for ev in evs:
                self._need(eng, ev)
        self.state = {}

    def emit(self):
        nc = self.nc
        engmap = {"pe": "tensor", "act": "scalar", "dve": "vector", "pool": "gpsimd", "sp": "sync"}
        with nc.Block() as block:
            for e in self.ENG:
                stream = self.streams[e]
                if not stream:
                    continue

                def body(eng, stream=stream):
                    for item in stream:
                        if item[0] == "wait":
                            eng.wait_ge(item[1], item[2])
                        else:
                            ins = item[1](eng)
                            ins.then_inc(item[2], item[3])

                getattr(block, engmap[e])(body)


def host_consts():
    c = {}
    c["ident_bf"] = np.eye(128, dtype=np.float32)
    c["ones_f"] = np.ones((128, 128), np.float32)
    inv = ROPE_THETA ** (-np.arange(0, 32, 2, dtype=np.float32) / np.float32(32))
    c["invf"] = np.concatenate([inv, inv]).astype(np.float32).reshape(32, 1)
    c["sinsign"] = np.concatenate([-np.ones(16), np.ones(16)]).astype(np.float32).reshape(32, 1)
    pm = np.eye(128, dtype=np.float32)
    pm[:32, :32] = 0.0
    for m in range(32):
        pm[(m + 16) % 32, m] = 1.0
    c["perm"] = pm
    kk = np.arange(128)[:, None]
    qq = np.arange(128)[None, :]
    mb = np.zeros((128, 256), np.float32)
    mb[:, :128] = np.where(kk <= qq, 0.0, NEG)
    mb[:, 128:] = np.where(kk >= qq, 0.0, NEG)
    c["maskb"] = mb
    rm = np.ones((128, S), np.float32)
    rm[:, ::64] = 0.0
    c["resetm"] = rm
    ss = np.arange(64)[:, None]
    tt = np.arange(64)[None, :]
    c["causal"] = np.tile((ss <= tt).astype(np.float32), (1, 8))
    c["ltri"] = (np.arange(128)[:, None] < np.arange(128)[None, :]).astype(np.float32)
    c["iota_p"] = np.arange(128, dtype=np.float32).reshape(128, 1)
    c["thrb"] = np.broadcast_to((np.arange(64, dtype=np.float32) * 128.0)[None, :], (128, 64)).copy()
    c["iota_e"] = np.broadcast_to(np.arange(32, dtype=np.float32)[None, :], (128, 32)).copy()
    return c


CONST_SHAPES = {k: v.shape for k, v in host_consts().items()}

IN_SHAPES = {
    "x": ([S, D], F32), "c_t": ([128, 16], F32), "pos": ([S], I32),
    "ada_w": ([D, 6 * D], F32), "ada_b": ([6 * D], F32), "mix_norm_g": ([D], F32),
    "w_in": ([D, INW], F32), "w_a": ([D, D], F32), "w_r": ([D, D], F32), "w_o": ([D, D], F32),
    "rec_norm_g": ([128, 1], F32), "lb_logits_t": ([128, 2, 16], F32), "ffn_norm_g": ([D], F32),
    "w_router": ([D, 36], F32), "b_router": ([36], F32),
    "w_gate": ([32 * D, 1024], F32), "w_up": ([32 * D, 1024], F32), "w_down": ([32 * 1024, D], F32),
    "final_norm_g": ([D], F32),
}


def build(stop_after=99, debug=False, rot=3, trig=True, p2_ct=72, skip=(), nh3=NH, nh4=NH, nb7=64, p3v=3, p3rs=(1, 4, 16)):
    nc = bass.Bass("TRN2", target_bir_lowering=False)
    I = {}
    for k, (shp, dt) in IN_SHAPES.items():
        I[k] = nc.dram_tensor(k, list(shp), dt, kind="ExternalInput").ap()
    C = {}
    for k, shp in CONST_SHAPES.items():
        C[k] = nc.dram_tensor("c_" + k, list(shp), F32, kind="ExternalInput").ap()
    out = nc.dram_tensor("out", [S, D], F32, kind="ExternalOutput").ap()
    skind = "ExternalOutput" if debug else "Internal"

    def scratch(name, shape, dt):
        return nc.dram_tensor(name, list(shape), dt, kind=skind).ap()

    modbc = scratch("s_modbc", [128, 6 * D], F32)
    qT_s = scratch("s_qT", [NH, 128, S], BF16)
    kT_s = scratch("s_kT", [NH, 128, S], BF16)
    v_s = scratch("s_v", [S, D], BF16)
    qr_s = scratch("s_qr", [NH, 128, S], BF16)
    f_s = scratch("s_f", [NH, 128, S], F32)
    i_s = scratch("s_i", [S, D], BF16)
    g_s = scratch("s_g", [NH, 128, S], BF16)
    ga_s = scratch("s_ga", [NH, 128, S], BF16)
    gr_s = scratch("s_gr", [NH, 128, S], BF16)
    ya_s = scratch("s_ya", [NH, 128, S], BF16)
    yr_s = scratch("s_yr", [NH, 128, S], BF16)
    m_s = scratch("s_m", [NH, 128, S], BF16)
    h_s = scratch("s_h", [S, D], F32)
    u2_s = scratch("s_u2", [S, D], BF16)
    lg_s = scratch("s_lg", [128, 16, 36], F32)
    xslot = scratch("s_xslot", [64 * 128, D], BF16)
    yslot = scratch("s_yslot", [64 * 128, D], F32)
    rt_s = scratch("s_rt", [128, 16, 8], F32)

    with ExitStack() as es:
        P = Prog(nc, es)
        ident_bf = es.enter_context(nc.sbuf_tensor("ident_bf", [128, 128], BF16))
        ident_f = es.enter_context(nc.sbuf_tensor("ident_f", [128, 128], F32))
        ones_f = es.enter_context(nc.sbuf_tensor("ones_f", [128, 128], F32))
        ones_bf = es.enter_context(nc.sbuf_tensor("ones_bf", [128, 128], BF16))
        P.dma("sp", lambda e: e.dma_start(out=ident_f[:], in_=C["ident_bf"]), "c0", writes=["ident_f"])
        P.dma("sp", lambda e: e.dma_start(out=ones_f[:], in_=C["ones_f"]), "c1", writes=["ones_f"])
        P.op("dve", lambda e: e.tensor_copy(out=ident_bf[:], in_=ident_f[:]), reads=["ident_f"], writes=["ident_bf"])
        P.op("dve", lambda e: e.tensor_copy(out=ones_bf[:], in_=ones_f[:]), reads=["ones_f"], writes=["ones_bf"])
        desti = es.enter_context(nc.sbuf_tensor("desti", [128, 32], I32))
        wts = es.enter_context(nc.sbuf_tensor("wts", [128, 32], F32))
        offs_gu = es.enter_context(nc.sbuf_tensor("offs_gu", [128, 64], I32))
        offs_d = es.enter_context(nc.sbuf_tensor("offs_d", [128, 64], I32))
        P.barrier()

        with ExitStack() as ps:
            c_sb = ps.enter_context(nc.sbuf_tensor("c_sb", [128, 16], F32))
            L = ps.enter_context(nc.sbuf_tensor("adaL", [128, 16, 128], BF16))
            modsb = ps.enter_context(nc.sbuf_tensor("modsb", [128, 6 * D], F32))
            wst = [ps.enter_context(nc.sbuf_tensor(f"a_wst{i}", [128, 16, 512], F32)) for i in range(2)]
            wbf = [ps.enter_context(nc.sbuf_tensor(f"a_wbf{i}", [128, 16, 512], BF16)) for i in range(2)]
            pp = [ps.enter_context(nc.psum_tensor(f"a_ps{i}", [128, 512], F32)) for i in range(2)]
            P.dma("sp", lambda e: e.dma_start(out=c_sb[:], in_=I["c_t"]), "c0", writes=["c_sb"])
            P.dma("sp", lambda e: e.dma_start(out=modsb[:], in_=I["ada_b"].partition_broadcast(128)), "c1", writes=["modsb"])
            P.op("act", lambda e: e.activation(out=c_sb[:], in_=c_sb[:], func=AF.Silu), reads=["c_sb"], writes=["c_sb"])

            def mkL(e):
                ins = None
                for j in range(16):
                    ins = e.tensor_scalar(out=L[:, j, :], in0=ones_f[:], scalar1=c_sb[:, j:j + 1], scalar2=None, op0=ALU.mult)
                return ins
            P.op("dve", mkL, reads=["c_sb"], writes=["L"])
            awv = I["ada_w"].rearrange("(kc p) n -> p kc n", p=128)
            for ct in range(0 if 0 in skip else 24):
                s = ct % 2
                P.dma("sp", lambda e, s=s, ct=ct: e.dma_start(out=wst[s][:], in_=awv[:, :, 512 * ct:512 * ct + 512]),
                      f"w{s}", writes=[("wst", s)])
                P.op("pool", lambda e, s=s: e.tensor_copy(out=wbf[s][:], in_=wst[s][:]), reads=[("wst", s)], writes=[("wbf", s)])

                def mm(e, s=s):
                    ins = None
                    for kc in range(16):
                        ins = e.matmul(pp[s][:], lhsT=L[:, kc, :], rhs=wbf[s][:, kc, :], start=(kc == 0), stop=(kc == 15))
                    return ins
                P.op("pe", mm, reads=["L", ("wbf", s)], writes=[("pp", s)])
                P.op("dve", lambda e, s=s, ct=ct: e.tensor_tensor(out=modsb[:, 512 * ct:512 * ct + 512], in0=pp[s][:],
                                                                 in1=modsb[:, 512 * ct:512 * ct + 512], op=ALU.add),
                     reads=[("pp", s), "modsb"], writes=["modsb"])
            P.dma("sp", lambda e: e.dma_start(out=modbc, in_=modsb[:]), "c0", reads=["modsb"], writes=["modbc"])
            P.barrier()
        if stop_after <= 0:
            P.barrier(); P.emit(); return nc

        ps12 = es.enter_context(ExitStack())
        uT = ps12.enter_context(nc.sbuf_tensor("uT", [128, 16, S], BF16))
        with ExitStack() as ps:
            G1 = ps.enter_context(nc.sbuf_tensor("G1", [128, D], F32))
            shm = ps.enter_context(nc.sbuf_tensor("shm", [128, D], F32))
            tmpg = ps.enter_context(nc.sbuf_tensor("tmpg", [128, D], F32))
            xt = [ps.enter_context(nc.sbuf_tensor(f"xt{i}", [128, D], F32)) for i in range(2)]
            junk = ps.enter_context(nc.sbuf_tensor("junk", [128, D], BF16))
            tf = ps.enter_context(nc.sbuf_tensor("tf", [128, D], F32))
            ub = [ps.enter_context(nc.sbuf_tensor(f"ub{i}", [128, D], BF16)) for i in range(2)]
            st = [ps.enter_context(nc.sbuf_tensor(f"st{i}", [128, 4], F32)) for i in range(2)]
            ptr = [ps.enter_context(nc.psum_tensor(f"ptr{i}", [128, 16, 128], BF16)) for i in range(2)]
            P.dma("sp", lambda e: e.dma_start(out=shm[:], in_=modbc[:, 0:D]), "c0", writes=["shm"])
            P.dma("sp", lambda e: e.dma_start(out=tmpg[:], in_=modbc[:, D:2 * D]), "c1", writes=["tmpg"])
            P.dma("sp", lambda e: e.dma_start(out=G1[:], in_=I["mix_norm_g"].partition_broadcast(128)), "c2", writes=["G1"])
            P.op("dve", lambda e: e.scalar_tensor_tensor(out=G1[:], in0=tmpg[:], scalar=1.0, in1=G1[:], op0=ALU.add, op1=ALU.mult),
                 reads=["tmpg", "G1"], writes=["G1"])
            for i in range(0 if 1 in skip else 16):
                s = i % 2
                P.dma("sp", lambda e, s=s, i=i: e.dma_start(out=xt[s][:], in_=I["x"][128 * i:128 * i + 128, :]), f"w{s}", writes=[("xt", s)])
                P.op("act", lambda e, s=s: e.activation(out=junk[:], in_=xt[s][:], func=AF.Square, accum_out=st[s][:, 0:1]),
                     reads=[("xt", s)], writes=["junk", ("st", s)])
                P.op("act", lambda e, s=s: e.activation(out=st[s][:, 1:2], in_=st[s][:, 0:1], func=AF.Sqrt, scale=1.0 / D, bias=EPS),
                     reads=[("st", s)], writes=[("st", s)])
                P.op("dve", lambda e, s=s: e.reciprocal(out=st[s][:, 2:3], in_=st[s][:, 1:2]), reads=[("st", s)], writes=[("st", s)])
                P.op("dve", lambda e, s=s: e.scalar_tensor_tensor(out=tf[:], in0=xt[s][:], scalar=st[s][:, 2:3], in1=G1[:], op0=ALU.mult, op1=ALU.mult),
                     reads=[("xt", s), ("st", s), "G1"], writes=["tf"])
                P.op("pool", lambda e, s=s: e.tensor_tensor(out=ub[s][:], in0=tf[:], in1=shm[:], op=ALU.add),
                     reads=["tf", "shm"], writes=[("ub", s)])

                def tr(e, s=s):
                    ins = None
                    for kc in range(16):
                        ins = e.transpose(out=ptr[s][:, kc, :], in_=ub[s][:, 128 * kc:128 * kc + 128], identity=ident_bf[:])
                    return ins
                P.op("pe", tr, reads=[("ub", s), "ident_bf"], writes=[("ptr", s)])
                P.op("act", lambda e, s=s, i=i: e.activation(out=uT[:, :, 128 * i:128 * i + 128], in_=ptr[s][:], func=AF.Copy),
                     reads=[("ptr", s)], writes=["uT"])
            P.barrier()

        if stop_after <= 1:
            P.barrier(); P.emit(); return nc
        PI = float(np.pi)
        with ExitStack() as ps:
            cosF = ps.enter_context(nc.sbuf_tensor("cosF", [128, S], F32))
            sinF = ps.enter_context(nc.sbuf_tensor("sinF", [128, S], F32))
            permb = ps.enter_context(nc.sbuf_tensor("permb", [128, 128], BF16))
            permf = ps.enter_context(nc.sbuf_tensor("permf", [128, 128], F32))
            with ExitStack() as ts:
                cosT = ts.enter_context(nc.sbuf_tensor("cosT", [32, S], F32))
                sinT = ts.enter_context(nc.sbuf_tensor("sinT", [32, S], F32))
                posi = ts.enter_context(nc.sbuf_tensor("posi", [32, S], I32))
                ang = ts.enter_context(nc.sbuf_tensor("ang", [32, S], F32))
                kq = ts.enter_context(nc.sbuf_tensor("kq", [32, S], F32))
                ki = ts.enter_context(nc.sbuf_tensor("ki", [32, S], I32))
                rr = ts.enter_context(nc.sbuf_tensor("rr", [32, S], F32))
                mm_ = ts.enter_context(nc.sbuf_tensor("mm_", [32, S], F32))
                sm = ts.enter_context(nc.sbuf_tensor("sm", [32, 40], F32))
                P.dma("sp", lambda e: e.dma_start(out=posi[:], in_=I["pos"].partition_broadcast(32)), "c0", writes=["posi"])
                P.dma("sp", lambda e: e.dma_start(out=sm[:, 0:1], in_=C["invf"]), "c1", writes=["sm0"])
                P.dma("sp", lambda e: e.dma_start(out=sm[:, 1:2], in_=C["sinsign"]), "c2", writes=["sm1"])
                P.dma("sp", lambda e: e.dma_start(out=permf[:], in_=C["perm"]), "c3", writes=["permf"])
                P.op("dve", lambda e: e.tensor_copy(out=permb[:], in_=permf[:]), reads=["permf"], writes=["permb"])
                P.op("dve", lambda e: e.tensor_copy(out=ang[:], in_=posi[:]), reads=["posi"], writes=["ang"])
                P.op("dve", lambda e: e.tensor_scalar(out=ang[:], in0=ang[:], scalar1=sm[:, 0:1], scalar2=None, op0=ALU.mult),
                     reads=["ang", "sm0"], writes=["ang"])

                def reduce_to(dst, shift, tag):
                    P.op("dve", lambda e: e.tensor_scalar(out=kq[:], in0=ang[:], scalar1=shift, scalar2=1.0 / (2 * PI), op0=ALU.add, op1=ALU.mult),
                         reads=["ang"], writes=["kq"])
                    P.op("dve", lambda e: e.tensor_copy(out=ki[:], in_=kq[:]), reads=["kq"], writes=["ki"])
                    P.op("dve", lambda e: e.tensor_copy(out=kq[:], in_=ki[:]), reads=["ki"], writes=["kq"])
                    P.op("dve", lambda e: e.scalar_tensor_tensor(out=rr[:], in0=kq[:], scalar=-2 * PI, in1=ang[:], op0=ALU.mult, op1=ALU.add),
                         reads=["kq", "ang"], writes=["rr"])
                    if shift != 0.0:
                        P.op("dve", lambda e: e.tensor_scalar(out=rr[:], in0=rr[:], scalar1=shift, scalar2=None, op0=ALU.add),
                             reads=["rr"], writes=["rr"])
                    P.op("dve", lambda e: e.tensor_scalar(out=mm_[:], in0=rr[:], scalar1=PI, scalar2=-2 * PI, op0=ALU.is_gt, op1=ALU.mult),
                         reads=["rr"], writes=["mm_"])
                    P.op("dve", lambda e: e.tensor_tensor(out=rr[:], in0=rr[:], in1=mm_[:], op=ALU.add), reads=["rr", "mm_"], writes=["rr"])
                    P.op("dve", lambda e: e.tensor_scalar(out=mm_[:], in0=rr[:], scalar1=-PI, scalar2=2 * PI, op0=ALU.is_lt, op1=ALU.mult),
                         reads=["rr"], writes=["mm_"])
                    P.op("dve", lambda e: e.tensor_tensor(out=rr[:], in0=rr[:], in1=mm_[:], op=ALU.add), reads=["rr", "mm_"], writes=["rr"])
                    P.op("dve", lambda e: e.tensor_scalar(out=rr[:], in0=rr[:], scalar1=3.1415925, scalar2=-3.1415925, op0=ALU.min, op1=ALU.max),
                         reads=["rr"], writes=["rr"])
                    P.op("act", lambda e: e.activation(out=dst[:], in_=rr[:], func=AF.Sin), reads=["rr"], writes=[tag])
                reduce_to(sinT, 0.0, "sinT")
                P.op("dve", lambda e: e.tensor_scalar(out=sinT[:], in0=sinT[:], scalar1=sm[:, 1:2], scalar2=None, op0=ALU.mult),
                     reads=["sinT", "sm1"], writes=["sinT"])
                reduce_to(cosT, PI / 2, "cosT")
                P.op("pool", lambda e: e.memset(cosF[:], 1.0), writes=["cosF"])
                P.op("pool", lambda e: e.memset(sinF[:], 0.0), writes=["sinF"])
                P.op("dve", lambda e: e.tensor_copy(out=cosF[0:32, :], in_=cosT[:]), reads=["cosT", "cosF"], writes=["cosF"])
                P.op("dve", lambda e: e.tensor_copy(out=sinF[0:32, :], in_=sinT[:]), reads=["sinT", "sinF"], writes=["sinF"])
                P.barrier()

            wst2 = [ps.enter_context(nc.sbuf_tensor(f"p2_wst{i}", [128, 16, 256], F32)) for i in range(2)]
            wbf2 = [ps.enter_context(nc.sbuf_tensor(f"p2_wbf{i}", [128, 16, 256], BF16)) for i in range(2)]
            stg = [ps.enter_context(nc.sbuf_tensor(f"p2_stg{i}", [128, S], BF16)) for i in range(2)]
            stgf = [ps.enter_context(nc.sbuf_tensor(f"p2_stgf{i}", [128, S], F32)) for i in range(2)]
            stgt = [ps.enter_context(nc.sbuf_tensor(f"p2_stgt{i}", [128, 256], BF16)) for i in range(2)]
            rt1 = ps.enter_context(nc.sbuf_tensor("p2_rt1", [128, 512], F32))
            rt2 = ps.enter_context(nc.sbuf_tensor("p2_rt2", [128, 512], F32))
            xb = ps.enter_context(nc.sbuf_tensor("p2_xb", [128, 512], BF16))
            xf = ps.enter_context(nc.sbuf_tensor("p2_xf", [128, 512], F32))
            pf = ps.enter_context(nc.sbuf_tensor("p2_pf", [128, 512], F32))
            pp2 = [ps.enter_context(nc.psum_tensor(f"p2_ps{i}", [128, 512], F32)) for i in range(4)]
            psw = [ps.enter_context(nc.psum_tensor(f"p2_psw{i}", [128, 512], F32)) for i in range(2)]
            wv = I["w_in"].rearrange("(kc p) n -> p kc n", p=128)
            SEC = [("rot", qT_s), ("rot", kT_s), ("tok", v_s), ("silu", qr_s), ("f32", f_s), ("tok", i_s), ("silu", g_s), ("sig", ga_s), ("sig", gr_s)]
            ppi = 0
            swi = 0
            sti = 0
            stt = 0
            for ct in range(0 if 2 in skip else p2_ct):
                s = ct % 2
                sec = ct // 8
                kind, dst = SEC[sec]
                if ct == 0:
                    P.dma("act", lambda e: e.dma_start(out=wst2[0][:], in_=wv[:, :, 0:256]), "w0", writes=[("wst", 0)])
                if ct + 1 < p2_ct:
                    P.dma("act", lambda e, s1=(ct + 1) % 2, c1=ct + 1: e.dma_start(out=wst2[s1][:], in_=wv[:, :, 256 * c1:256 * c1 + 256]), f"w{(ct + 1) % 2}", writes=[("wst", (ct + 1) % 2)])
                P.op("pool", lambda e, s=s: e.tensor_copy(out=wbf2[s][:], in_=wst2[s][:]), reads=[("wst", s)], writes=[("wbf", s)])
                if kind == "tok":
                    c0 = 256 * (ct % 8)
                    for tt in range(16):
                        b = ppi % 4; ppi += 1
                        def mm(e, s=s, tt=tt, b=b):
                            ins = None
                            for kc in range(16):
                                ins = e.matmul(pp2[b][:, 0:256], lhsT=uT[:, kc, 128 * tt:128 * tt + 128], rhs=wbf2[s][:, kc, :], start=(kc == 0), stop=(kc == 15))
                            return ins
                        P.op("pe", mm, reads=[("wbf", s)], writes=[("pp", b)])
                        q = stt % 2; stt += 1
                        P.op("act", lambda e, b=b, q=q: e.activation(out=stgt[q][:], in_=pp2[b][:, 0:256], func=AF.Copy), reads=[("pp", b)], writes=[("stgt", q)])
                        P.dma("sp", lambda e, q=q, tt=tt, c0=c0, dst=dst: e.dma_start(out=dst[128 * tt:128 * tt + 128, c0:c0 + 256], in_=stgt[q][:]),
                              f"o{q}", reads=[("stgt", q)], writes=[("dtok", ct, tt)])
                    continue
                for sub in range(2):
                    h = (ct % 8) * 2 + sub
                    q = sti % 2; sti += 1
                    sbuf_dst = stgf[q] if kind == "f32" else stg[q]
                    skey = ("stgf", q) if kind == "f32" else ("stg", q)
                    for tc in range(4):
                        b = ppi % 4; ppi += 1
                        def mm(e, s=s, sub=sub, tc=tc, b=b):
                            ins = None
                            for kc in range(16):
                                ins = e.matmul(pp2[b][:], lhsT=wbf2[s][:, kc, 128 * sub:128 * sub + 128], rhs=uT[:, kc, 512 * tc:512 * tc + 512], start=(kc == 0), stop=(kc == 15))
                            return ins
                        P.op("pe", mm, reads=[("wbf", s)], writes=[("pp", b)])
                        func = {"rot": AF.Copy, "silu": AF.Silu, "f32": AF.Copy, "sig": AF.Sigmoid}[kind]
                        cs = slice(512 * tc, 512 * tc + 512)
                        if kind == "rot" and rot:
                            w = swi % 2; swi += 1
                            P.op("act", lambda e, b=b: e.activation(out=xb[:], in_=pp2[b][:], func=AF.Copy), reads=[("pp", b)], writes=["xb"])
                            P.op("act", lambda e, b=b: e.activation(out=xf[:], in_=pp2[b][:], func=AF.Copy), reads=[("pp", b)], writes=["xf"])
                            P.op("pe", lambda e, w=w: e.matmul(psw[w][:], lhsT=permb[:], rhs=xb[:], start=True, stop=True),
                                 reads=["xb"], writes=[("psw", w)])
                            P.op("act", lambda e, w=w: e.activation(out=pf[:], in_=psw[w][:], func=AF.Copy), reads=[("psw", w)], writes=["pf"])
                            P.op("dve", lambda e, cs=cs: e.tensor_tensor(out=rt1[:], in0=xf[:], in1=cosF[:, cs], op=ALU.mult),
                                 reads=["xf"], writes=["rt1"])
                            P.op("dve", lambda e, cs=cs: e.tensor_tensor(out=rt2[:], in0=pf[:], in1=sinF[:, cs], op=ALU.mult),
                                 reads=["pf"], writes=["rt2"])
                            P.op("pool", lambda e, cs=cs, sbuf_dst=sbuf_dst: e.tensor_tensor(out=sbuf_dst[:, cs], in0=rt1[:], in1=rt2[:], op=ALU.add),
                                 reads=["rt1", "rt2"], writes=[skey])
                        else:
                            P.op("act", lambda e, b=b, cs=cs, sbuf_dst=sbuf_dst, func=func: e.activation(out=sbuf_dst[:, cs], in_=pp2[b][:], func=func),
                                 reads=[("pp", b)], writes=[skey])
                    P.dma("sp", lambda e, h=h, dst=dst, sbuf_dst=sbuf_dst: e.dma_start(out=dst[h], in_=sbuf_dst[:]), f"o{q}f" if kind == "f32" else f"o{q}",
                          reads=[skey], writes=[("dfm", sec, h)])
            P.barrier()

        ps12.close()
        if stop_after <= 2:
            P.barrier(); P.emit(); return nc

        def phase4():
            with ExitStack() as ps:
                A = lambda n, shp, dt: ps.enter_context(nc.sbuf_tensor("p4_" + n, shp, dt))
                lbl = A("lbl", [128, 2, 16], F32)
                lbT = A("lbT", [128, 16], F32)
                oml = A("oml", [128, 16], F32)
                noml = A("noml", [128, 16], F32)
                rng = A("rng", [128, 1], F32)
                resetm = A("resetm", [128, S], F32)
                causal = A("causal", [64, 512], F32)
                f4 = A("f", [128, S], F32)
                lf = A("lf", [128, S], F32)
                kk = A("kk", [128, S], F32)
                bcs = A("bcs", [128, S], F32)
                eb = A("eb", [128, S], F32)
                enb = A("enb", [128, S], F32)
                oT = A("oT", [128, S], F32)
                qr4 = A("qr", [128, S], BF16)
                g4 = A("g", [128, S], BF16)
                qe = A("qe", [128, S], BF16)
                ke = A("ke", [128, S], BF16)
                sq = A("sq", [128, S], BF16)
                yr = A("yr", [128, S], BF16)
                vch = A("vch", [64, 32, 128], BF16)
                keT = A("keT", [64, 32, 128], BF16)
                Vsb = A("Vsb", [128, 32, 128], F32)
                Sst = A("Sst", [128, 32, 128], F32)
                Sbf = A("Sbf", [128, 32, 128], BF16)
                scs = [A(f"scs{i}", [64, 512], BF16) for i in range(4)]
                psA = [ps.enter_context(nc.psum_tensor(f"p4_A{i}", [64, 512], F32)) for i in range(2)]
                psB = ps.enter_context(nc.psum_tensor("p4_B", [64, 8, 128], BF16))
                psC = [ps.enter_context(nc.psum_tensor(f"p4_C{i}", [128, 4, 128], F32)) for i in range(2)]
                psE = [ps.enter_context(nc.psum_tensor(f"p4_E{i}", [128, 512], F32)) for i in range(2)]
                psF = ps.enter_context(nc.psum_tensor("p4_F", [128, 512], F32))
                P.dma("sp", lambda e: e.dma_start(out=lbl[:], in_=I["lb_logits_t"]), "c0", writes=["lbl"])
                P.dma("sp", lambda e: e.dma_start(out=rng[:], in_=I["rec_norm_g"]), "c1", writes=["rng"])
                P.dma("sp", lambda e: e.dma_start(out=resetm[:], in_=C["resetm"]), "c2", writes=["resetm"])
                P.dma("sp", lambda e: e.dma_start(out=causal[:], in_=C["causal"]), "c3", writes=["causal"])
                P.op("dve", lambda e: e.tensor_tensor(out=lbT[:], in0=lbl[:, 0, :], in1=lbl[:, 1, :], op=ALU.subtract), reads=["lbl"], writes=["lbT"])
                P.op("act", lambda e: e.activation(out=lbT[:], in_=lbT[:], func=AF.Sigmoid), reads=["lbT"], writes=["lbT"])
                P.op("dve", lambda e: e.tensor_scalar(out=oml[:], in0=lbT[:], scalar1=-1.0, scalar2=1.0, op0=ALU.mult, op1=ALU.add), reads=["lbT"], writes=["oml"])
                P.op("dve", lambda e: e.tensor_scalar(out=noml[:], in0=oml[:], scalar1=-1.0, scalar2=None, op0=ALU.mult), reads=["oml"], writes=["noml"])
                P.op("pool", lambda e: e.memset(Sst[:, 0, :], 0.0), writes=["Sst0"])
                for h in range(nh4):
                    P.dma("sp", lambda e, h=h: e.dma_start(out=f4[:], in_=f_s[h]), "w0", writes=["f4"])
                    P.dma("sp", lambda e, h=h: e.dma_start(out=qr4[:], in_=qr_s[h]), "w1", writes=["qr4"])
                    P.dma("sp", lambda e, h=h: e.dma_start(out=g4[:], in_=g_s[h]), "o0", writes=["g4"])
                    P.dma("sp", lambda e, h=h: e.dma_start(out=vch[:], in_=i_s[:, 128 * h:128 * h + 128].rearrange("(c s) d -> s c d", s=64)), "o1", writes=["vch"])
                    hc = slice(h, h + 1)
                    P.op("act", lambda e: e.activation(out=f4[:], in_=f4[:], func=AF.Sigmoid), reads=["f4"], writes=["f4"])
                    P.op("act", lambda e, hc=hc: e.activation(out=lf[:], in_=f4[:], func=AF.Ln, scale=oml[:, hc], bias=lbT[:, hc]), reads=["f4", "oml", "lbT"], writes=["lf"])
                    P.op("dve", lambda e, hc=hc: e.tensor_scalar(out=kk[:], in0=f4[:], scalar1=noml[:, hc], scalar2=oml[:, hc], op0=ALU.mult, op1=ALU.add),
                         reads=["f4", "oml", "noml"], writes=["kk"])
                    P.op("dve", lambda e: e.tensor_tensor_scan(out=bcs[:], data0=resetm[:], data1=lf[:], initial=0.0, op0=ALU.mult, op1=ALU.add),
                         reads=["resetm", "lf"], writes=["bcs"])
                    P.op("act", lambda e: e.activation(out=eb[:], in_=bcs[:], func=AF.Exp), reads=["bcs"], writes=["eb"])
                    P.op("act", lambda e: e.activation(out=enb[:], in_=bcs[:], func=AF.Exp, scale=-1.0), reads=["bcs"], writes=["enb"])
                    P.op("dve", lambda e: e.tensor_tensor(out=qe[:], in0=qr4[:], in1=eb[:], op=ALU.mult), reads=["qr4", "eb"], writes=["qe"])
                    P.op("pool", lambda e: e.tensor_tensor(out=ke[:], in0=kk[:], in1=enb[:], op=ALU.mult), reads=["kk", "enb"], writes=["ke"])
                    for g in range(4):
                        a = g % 2
                        def mmA(e, g=g, a=a):
                            ins = None
                            for c8 in range(8):
                                c = 8 * g + c8
                                ins = e.matmul(psA[a][:, 64 * c8:64 * c8 + 64], lhsT=ke[:, 64 * c:64 * c + 64], rhs=qe[:, 64 * c:64 * c + 64], start=True, stop=True)
                            return ins
                        P.op("pe", mmA, reads=["ke", "qe"], writes=[("psA", a)])
                        P.op("dve", lambda e, g=g, a=a: e.tensor_tensor(out=scs[g][:], in0=psA[a][:], in1=causal[:], op=ALU.mult),
                             reads=[("psA", a), "causal"], writes=[("scs", g)])
                    for g in range(4):
                        def trB(e, g=g):
                            ins = None
                            for c8 in range(8):
                                c = 8 * g + c8
                                ins = e.transpose(out=psB[:, c8, :], in_=ke[:, 64 * c:64 * c + 64], identity=ident_bf[:])
                            return ins
                        P.op("pe", trB, reads=["ke"], writes=["psB"])
                        P.op("act", lambda e, g=g: e.activation(out=keT[:, 8 * g:8 * g + 8, :], in_=psB[:], func=AF.Copy), reads=["psB"], writes=["keT"])
                    for g in range(8):
                        a = g % 2
                        def mmC(e, g=g, a=a):
                            ins = None
                            for c4 in range(4):
                                c = 4 * g + c4
                                ins = e.matmul(psC[a][:, c4, :], lhsT=keT[:, c, :], rhs=vch[:, c, :], start=True, stop=True)
                            return ins
                        P.op("pe", mmC, reads=["keT", "vch"], writes=[("psC", a)])
                        def evC(e, g=g, a=a):
                            ins = None
                            for c4 in range(4):
                                c = 4 * g + c4
                                ins = e.activation(out=Vsb[:, c, :], in_=psC[a][:, c4, :], func=AF.Copy, scale=eb[:, 64 * c + 63:64 * c + 64])
                            return ins
                        P.op("act", evC, reads=[("psC", a), "eb"], writes=["Vsb"])
                    def scanD(e):
                        ins = None
                        for c in range(31):
                            ins = e.scalar_tensor_tensor(out=Sst[:, c + 1, :], in0=Sst[:, c, :], scalar=eb[:, 64 * c + 63:64 * c + 64], in1=Vsb[:, c, :], op0=ALU.mult, op1=ALU.add)
                        return ins
                    P.op("dve", scanD, reads=["Vsb", "eb", "Sst0"], writes=["Sst"])
                    P.op("pool", lambda e: e.tensor_copy(out=Sbf[:], in_=Sst[:]), reads=["Sst", "Sst0"], writes=["Sbf"])
                    for g in range(4):
                        a = g % 2
                        def mmE(e, g=g, a=a):
                            ins = None
                            for c8 in range(8):
                                c = 8 * g + c8
                                reg = psE[a][:, 64 * c8:64 * c8 + 64]
                                e.matmul(reg, lhsT=Sbf[:, c, :], rhs=qe[:, 64 * c:64 * c + 64], start=True, stop=False)
                                ins = e.matmul(reg, lhsT=vch[:, c, :], rhs=scs[g][:, 64 * c8:64 * c8 + 64], start=False, stop=True)
                            return ins
                        P.op("pe", mmE, reads=["Sbf", "qe", "vch", ("scs", g)], writes=[("psE", a)])
                        P.op("act", lambda e, g=g, a=a: e.activation(out=oT[:, 512 * g:512 * g + 512], in_=psE[a][:], func=AF.Copy), reads=[("psE", a)], writes=["oT"])
                    P.op("act", lambda e: e.activation(out=sq[:], in_=oT[:], func=AF.Square), reads=["oT"], writes=["sq"])
                    for g in range(4):
                        gs = slice(512 * g, 512 * g + 512)
                        P.op("pe", lambda e, gs=gs: e.matmul(psF[:], lhsT=ones_bf[:], rhs=sq[:, gs], start=True, stop=True), reads=["sq"], writes=["psF"])
                        P.op("act", lambda e, gs=gs: e.activation(out=bcs[:, gs], in_=psF[:], func=AF.Sqrt, scale=1.0 / 128, bias=EPS), reads=["psF"], writes=["bcs"])
                    P.op("dve", lambda e: e.reciprocal(out=bcs[:], in_=bcs[:]), reads=["bcs"], writes=["bcs"])
                    P.op("dve", lambda e: e.tensor_tensor(out=oT[:], in0=oT[:], in1=bcs[:], op=ALU.mult), reads=["oT", "bcs"], writes=["oT"])
                    P.op("dve", lambda e: e.scalar_tensor_tensor(out=yr[:], in0=oT[:], scalar=rng[:, 0:1], in1=g4[:], op0=ALU.mult, op1=ALU.mult),
                         reads=["oT", "rng", "g4"], writes=["yr"])
                    P.dma("sp", lambda e, h=h: e.dma_start(out=yr_s[h], in_=yr[:]), "y0", reads=["yr"], writes=[("yr_s", h)])
                P.barrier()

        def phase3():
            with ExitStack() as ps:
                mbf = ps.enter_context(nc.sbuf_tensor("p3_mbf", [128, 256], F32))
                maskb = ps.enter_context(nc.sbuf_tensor("p3_maskb", [128, 256], BF16))
                q3 = [ps.enter_context(nc.sbuf_tensor(f"p3_q{i}", [128, S], BF16)) for i in range(2)]
                k3 = [ps.enter_context(nc.sbuf_tensor(f"p3_k{i}", [128, S], BF16)) for i in range(2)]
                v3 = [[ps.enter_context(nc.sbuf_tensor(f"p3_v{i}_{r}", [128, 16, 128], BF16)) for r in range(3)] for i in range(2)]
                num = ps.enter_context(nc.sbuf_tensor("p3_num", [128, S], F32))
                den = ps.enter_context(nc.sbuf_tensor("p3_den", [128, S], F32))
                ya = [ps.enter_context(nc.sbuf_tensor(f"p3_ya{i}", [128, S], BF16)) for i in range(2)]
                pT = [ps.enter_context(nc.sbuf_tensor(f"p3_pT{i}", [128, 256], BF16)) for i in range(4)]
                pstb = [ps.enter_context(nc.psum_tensor(f"p3_pst{i}", [128, 512], F32)) for i in range(4)]
                pnum = [ps.enter_context(nc.psum_tensor(f"p3_pn{i}", [128, 512], F32)) for i in range(2)]
                pden = [ps.enter_context(nc.psum_tensor(f"p3_pd{i}", [128, 512], F32)) for i in range(2)]
                P.dma("sp", lambda e: e.dma_start(out=mbf[:], in_=C["maskb"]), "c0", writes=["mbf"])
                P.op("dve", lambda e: e.tensor_copy(out=maskb[:], in_=mbf[:]), reads=["mbf"], writes=["maskb"])
                SC = float(DH ** -0.5)
                RS = p3rs

                def pst(slot):
                    return pstb[slot][:, 0:256]

                def sview(t, r, j):
                    if r == 1:
                        return t[:]
                    return t[:].rearrange("p (n r) -> p r n", r=r)[:, j, :]

                def load3(h):
                    s = h % 2
                    P.dma("sp", lambda e, s=s, h=h: e.dma_start(out=q3[s][:], in_=qT_s[h]), f"w{s}", writes=[("q3", s)])
                    P.dma("sp", lambda e, s=s, h=h: e.dma_start(out=k3[s][:], in_=kT_s[h]), f"o{s}", writes=[("k3", s)])
                    for ri, r in enumerate(RS if p3v >= 0 else ()):
                        src = v_s[:, 128 * h:128 * h + 128].rearrange("(i n r) d -> n r i d", n=128, r=r)
                        dstv = v3[s][ri][:].rearrange("p (r i) d -> p r i d", r=r)
                        P.dma("sp", lambda e, src=src, dstv=dstv: e.dma_start(out=dstv, in_=src), f"v{s}{ri}", writes=[("v3", s, ri)])
                if nh3 > 0:
                    load3(0)
                for h in range(nh3):
                    s = h % 2
                    if h + 1 < nh3:
                        load3(h + 1)
                    P.op("pool", lambda e: e.memset(num[:], 0.0), writes=["num"])
                    P.op("pool", lambda e: e.memset(den[:], 0.0), writes=["den"])
                    tasks = []
                    for ri, r in enumerate(RS):
                        nb = 16 // r
                        for j in range(r):
                            for i in range(nb):
                                tasks.append((ri, r, j, i, nb))
                    if p3v <= 0:
                        tasks = []
                    T = len(tasks)

                    def emit_scores(t):
                        ri, r, j, i, nb = tasks[t]
                        slot = t % 4
                        nq = 256 if i < nb - 1 else 128
                        kv = sview(k3[s], r, j)
                        qv = sview(q3[s], r, j)

                        def mm(e, slot=slot, nq=nq, kv=kv, qv=qv, i=i):
                            e.matmul(pst(slot)[:, 0:nq], lhsT=kv[:, 128 * i:128 * i + 128], rhs=qv[:, 128 * i:128 * i + nq], start=True, stop=False)
                            return e.matmul(pst(slot)[:, 0:nq], lhsT=ident_bf[:], rhs=maskb[:, 0:nq], start=False, stop=True)
                        P.op("pe", mm, reads=[("q3", s), ("k3", s), "maskb"], writes=[("pst", slot)])
                        P.op("act", lambda e, slot=slot, nq=nq: e.activation(out=pT[slot][:, 0:nq], in_=pst(slot)[:, 0:nq], func=AF.Exp, scale=SC),
                             reads=[("pst", slot)], writes=[("pT", slot)])

                    def emit_pv(t):
                        ri, r, j, i, nb = tasks[t]
                        slot = t % 4
                        bank = (t // 4) % 2
                        qi = t % 4
                        vr = v3[s][ri]
                        bi = j * nb + i

                        def mm(e, slot=slot, bank=bank, qi=qi, vr=vr, bi=bi, i=i):
                            ins = None
                            for dst_, lget in ((pnum[bank], lambda b_: vr[:, b_, :]), (pden[bank], lambda b_: ones_bf[:])):
                                reg = dst_[:, 128 * qi:128 * qi + 128]
                                if i > 0:
                                    e.matmul(reg, lhsT=lget(bi - 1), rhs=pT[(slot - 1) % 4][:, 128:256], start=True, stop=False)
                                    ins = e.matmul(reg, lhsT=lget(bi), rhs=pT[slot][:, 0:128], start=False, stop=True)
                                else:
                                    ins = e.matmul(reg, lhsT=lget(bi), rhs=pT[slot][:, 0:128], start=True, stop=True)
                            return ins
                        rd = [("pT", slot), ("v3", s, ri)]
                        if i > 0:
                            rd.append(("pT", (slot - 1) % 4))
                        P.op("pe", mm, reads=rd, writes=[("pnum", bank), ("pden", bank)])
                        if qi == 3 and p3v >= 3:
                            g = t // 4
                            if r == 1:
                                tg = i // 4
                                nv = num[:, 512 * tg:512 * tg + 512]
                                dv = den[:, 512 * tg:512 * tg + 512]
                                pn, pd = pnum[bank][:], pden[bank][:]
                            elif r == 4:
                                nv = sview(num, 4, j)
                                dv = sview(den, 4, j)
                                pn, pd = pnum[bank][:], pden[bank][:]
                            else:
                                j0 = j - 3
                                nv = num[:].rearrange("p (n r) -> p r n", r=16)[:, j0:j0 + 4, :]
                                dv = den[:].rearrange("p (n r) -> p r n", r=16)[:, j0:j0 + 4, :]
                                pn = pnum[bank][:].rearrange("p (a n) -> p a n", a=4)
                                pd = pden[bank][:].rearrange("p (a n) -> p a n", a=4)
                            P.op("dve", lambda e, nv=nv, pn=pn: e.tensor_tensor(out=nv, in0=pn, in1=nv, op=ALU.add), reads=[("pnum", bank), "num"], writes=["num"])
                            P.op("dve", lambda e, dv=dv, pd=pd: e.tensor_tensor(out=dv, in0=pd, in1=dv, op=ALU.add), reads=[("pden", bank), "den"], writes=["den"])

                    for t in range(T + 1):
                        if t < T:
                            emit_scores(t)
                        if t >= 1 and p3v >= 2 and T > 0:
                            emit_pv(t - 1)
                    P.op("dve", lambda e: e.reciprocal(out=den[:], in_=den[:]), reads=["den"], writes=["den"])
                    P.op("dve", lambda e, s=s: e.tensor_tensor(out=ya[s][:], in0=num[:], in1=den[:], op=ALU.mult), reads=["num", "den"], writes=[("ya", s)])
                    P.dma("sp", lambda e, s=s, h=h: e.dma_start(out=ya_s[h], in_=ya[s][:]), f"y{s}", reads=[("ya", s)], writes=[("ya_s", h)])
                P.barrier()
        if 3 not in skip:
            phase3()


        def phase5a():
            with ExitStack() as ps:
                A = lambda n, shp, dt: ps.enter_context(nc.sbuf_tensor("p5a_" + n, shp, dt))
                yaT = A("yaT", [128, 16, 512], BF16)
                yrT = A("yrT", [128, 16, 512], BF16)
                wsta = [A(f"wsta{i}", [128, 16, 256], F32) for i in range(2)]
                wstr = [A(f"wstr{i}", [128, 16, 256], F32) for i in range(2)]
                wba = [A(f"wba{i}", [128, 16, 256], BF16) for i in range(2)]
                wbr = [A(f"wbr{i}", [128, 16, 256], BF16) for i in range(2)]
                gat = [A(f"gat{i}", [128, 512], BF16) for i in range(2)]
                grt = [A(f"grt{i}", [128, 512], BF16) for i in range(2)]
                t1 = A("t1", [128, 512], F32)
                t2 = A("t2", [128, 512], F32)
                mst = [A(f"mst{i}", [128, 512], BF16) for i in range(2)]
                pA = [ps.enter_context(nc.psum_tensor(f"p5a_A{i}", [128, 512], F32)) for i in range(2)]
                pR = [ps.enter_context(nc.psum_tensor(f"p5a_R{i}", [128, 512], F32)) for i in range(2)]
                wav = I["w_a"].rearrange("(kc p) n -> p kc n", p=128)
                wrv = I["w_r"].rearrange("(kc p) n -> p kc n", p=128)
                it = 0

                def loadw5(step):
                    ct_, s_ = step % 8, step % 2
                    P.dma("act", lambda e: e.dma_start(out=wsta[s_][:], in_=wav[:, :, 256 * ct_:256 * ct_ + 256]), f"w{s_}", writes=[("wsta", s_)])
                    P.dma("act", lambda e: e.dma_start(out=wstr[s_][:], in_=wrv[:, :, 256 * ct_:256 * ct_ + 256]), f"v{s_}0", writes=[("wstr", s_)])
                for tt in range(4):
                    tsl = slice(512 * tt, 512 * tt + 512)
                    P.dma("sp", lambda e, tsl=tsl: e.dma_start(out=yaT[:], in_=ya_s.rearrange("h p t -> p h t")[:, :, tsl]), "c0", writes=["yaT"])
                    P.dma("sp", lambda e, tsl=tsl: e.dma_start(out=yrT[:], in_=yr_s.rearrange("h p t -> p h t")[:, :, tsl]), "c1", writes=["yrT"])
                    for ct in range(8):
                        s = ct % 2
                        step = 8 * tt + ct
                        if step == 0:
                            loadw5(0)
                        if step + 1 < 32:
                            loadw5(step + 1)
                        P.op("pool", lambda e, s=s: e.tensor_copy(out=wba[s][:], in_=wsta[s][:]), reads=[("wsta", s)], writes=[("wba", s)])
                        P.op("pool", lambda e, s=s: e.tensor_copy(out=wbr[s][:], in_=wstr[s][:]), reads=[("wstr", s)], writes=[("wbr", s)])
                        for sub in range(2):
                            cc = 2 * ct + sub
                            q = it % 2; it += 1
                            P.dma("sp", lambda e, q=q, cc=cc, tsl=tsl: e.dma_start(out=gat[q][:], in_=ga_s[cc][:, tsl]), f"o{q}", writes=[("gat", q)])
                            P.dma("sp", lambda e, q=q, cc=cc, tsl=tsl: e.dma_start(out=grt[q][:], in_=gr_s[cc][:, tsl]), f"o{q}f", writes=[("grt", q)])
                            def mmA(e, s=s, sub=sub, q=q):
                                ins = None
                                for kc in range(16):
                                    ins = e.matmul(pA[q][:], lhsT=wba[s][:, kc, 128 * sub:128 * sub + 128], rhs=yaT[:, kc, :], start=(kc == 0), stop=(kc == 15))
                                return ins
                            def mmR(e, s=s, sub=sub, q=q):
                                ins = None
                                for kc in range(16):
                                    ins = e.matmul(pR[q][:], lhsT=wbr[s][:, kc, 128 * sub:128 * sub + 128], rhs=yrT[:, kc, :], start=(kc == 0), stop=(kc == 15))
                                return ins
                            P.op("pe", mmA, reads=[("wba", s), "yaT"], writes=[("pA", q)])
                            P.op("pe", mmR, reads=[("wbr", s), "yrT"], writes=[("pR", q)])
                            P.op("dve", lambda e, q=q: e.tensor_tensor(out=t1[:], in0=pA[q][:], in1=gat[q][:], op=ALU.mult), reads=[("pA", q), ("gat", q)], writes=["t1"])
                            P.op("dve", lambda e, q=q: e.tensor_tensor(out=t2[:], in0=pR[q][:], in1=grt[q][:], op=ALU.mult), reads=[("pR", q), ("grt", q)], writes=["t2"])
                            P.op("pool", lambda e, q=q: e.tensor_tensor(out=mst[q][:], in0=t1[:], in1=t2[:], op=ALU.add), reads=["t1", "t2"], writes=[("mst", q)])
                            P.dma("sp", lambda e, q=q, cc=cc, tsl=tsl: e.dma_start(out=m_s[cc][:, tsl], in_=mst[q][:]), f"y{q}", reads=[("mst", q)], writes=[("m_s", cc, tt)])
                P.barrier()

        def phase5b():
            with ExitStack() as ps:
                A = lambda n, shp, dt: ps.enter_context(nc.sbuf_tensor("p5b_" + n, shp, dt))
                wo = A("wo", [128, 16, D], BF16)
                wst5 = [A(f"wst{i}", [128, 16, 256], F32) for i in range(2)]
                gtm = A("gtm", [128, D], F32)
                G2 = A("G2", [128, D], F32)
                shf = A("shf", [128, D], F32)
                mT = [A(f"mT{i}", [128, 16, 128], BF16) for i in range(2)]
                xt5 = [A(f"xt{i}", [128, D], F32) for i in range(2)]
                ht = A("ht", [128, D], F32)
                tf5 = A("tf", [128, D], F32)
                u2f = A("u2f", [128, D], F32)
                u2b = [A(f"u2b{i}", [128, D], BF16) for i in range(2)]
                junk5 = A("junk", [128, D], BF16)
                st5 = [A(f"st{i}", [128, 4], F32) for i in range(2)]
                u2T = A("u2T", [128, 16, 128], F32)
                wr = A("wr", [128, 16, 36], F32)
                brt = A("brt", [128, 36], F32)
                lgall = A("lgall", [128, 16, 36], F32)
                pM = [ps.enter_context(nc.psum_tensor(f"p5b_M{i}", [128, 512], F32)) for i in range(4)]
                pT5 = ps.enter_context(nc.psum_tensor("p5b_T", [128, 8, 128], F32))
                pL = ps.enter_context(nc.psum_tensor("p5b_L", [128, 36], F32))
                wov = I["w_o"].rearrange("(kc p) n -> p kc n", p=128)
                for ct in range(8):
                    s = ct % 2
                    P.dma("sp", lambda e, s=s, ct=ct: e.dma_start(out=wst5[s][:], in_=wov[:, :, 256 * ct:256 * ct + 256]), f"w{s}", writes=[("wst5", s)])
                    P.op("pool", lambda e, s=s, ct=ct: e.tensor_copy(out=wo[:, :, 256 * ct:256 * ct + 256], in_=wst5[s][:]), reads=[("wst5", s)], writes=["wo"])
                P.dma("sp", lambda e: e.dma_start(out=gtm[:], in_=modbc[:, 2 * D:3 * D]), "c0", writes=["gtm"])
                P.dma("sp", lambda e: e.dma_start(out=shf[:], in_=modbc[:, 3 * D:4 * D]), "c1", writes=["shf"])
                P.dma("sp", lambda e: e.dma_start(out=tf5[:], in_=modbc[:, 4 * D:5 * D]), "c2", writes=["tf5"])
                P.dma("sp", lambda e: e.dma_start(out=G2[:], in_=I["ffn_norm_g"].partition_broadcast(128)), "c3", writes=["G2"])
                P.op("dve", lambda e: e.scalar_tensor_tensor(out=G2[:], in0=tf5[:], scalar=1.0, in1=G2[:], op0=ALU.add, op1=ALU.mult), reads=["tf5", "G2"], writes=["G2"])
                P.dma("sp", lambda e: e.dma_start(out=wr[:], in_=I["w_router"].rearrange("(kc p) n -> p kc n", p=128)), "o0", writes=["wr"])
                P.dma("sp", lambda e: e.dma_start(out=brt[:], in_=I["b_router"].partition_broadcast(128)), "o1", writes=["brt"])
                def load5b(i):
                    s = i % 2
                    tsl = slice(128 * i, 128 * i + 128)
                    P.dma("sp", lambda e, s=s, tsl=tsl: e.dma_start(out=mT[s][:], in_=m_s.rearrange("h p t -> p h t")[:, :, tsl]), f"v{s}0", writes=[("mT", s)])
                    P.dma("sp", lambda e, s=s, tsl=tsl: e.dma_start(out=xt5[s][:], in_=I["x"][tsl, :]), f"v{s}1", writes=[("xt5", s)])
                load5b(0)
                for i in range(16):
                    s = i % 2
                    tsl = slice(128 * i, 128 * i + 128)
                    if i + 1 < 16:
                        load5b(i + 1)
                    for cc in range(4):
                        csl = slice(512 * cc, 512 * cc + 512)
                        def mmM(e, s=s, cc=cc, csl=csl):
                            ins = None
                            for kc in range(16):
                                ins = e.matmul(pM[cc][:], lhsT=mT[s][:, kc, :], rhs=wo[:, kc, csl], start=(kc == 0), stop=(kc == 15))
                            return ins
                        P.op("pe", mmM, reads=[("mT", s), "wo"], writes=[("pM", cc)])
                        P.op("dve", lambda e, cc=cc, csl=csl: e.tensor_tensor(out=tf5[:, csl], in0=pM[cc][:], in1=gtm[:, csl], op=ALU.mult), reads=[("pM", cc), "gtm"], writes=[("tf5", cc)])
                    P.op("pool", lambda e, s=s: e.tensor_tensor(out=ht[:], in0=tf5[:], in1=xt5[s][:], op=ALU.add), reads=[("tf5", 0), ("tf5", 1), ("tf5", 2), ("tf5", 3), ("xt5", s)], writes=["ht"])
                    P.dma("sp", lambda e, tsl=tsl: e.dma_start(out=h_s[tsl, :], in_=ht[:]), "y0", reads=["ht"], writes=[("h_s", i)])
                    P.op("act", lambda e, s=s: e.activation(out=junk5[:], in_=ht[:], func=AF.Square, accum_out=st5[s][:, 0:1]), reads=["ht"], writes=["junk5", ("st5", s)])
                    P.op("act", lambda e, s=s: e.activation(out=st5[s][:, 1:2], in_=st5[s][:, 0:1], func=AF.Sqrt, scale=1.0 / D, bias=EPS), reads=[("st5", s)], writes=[("st5", s)])
                    P.op("dve", lambda e, s=s: e.reciprocal(out=st5[s][:, 2:3], in_=st5[s][:, 1:2]), reads=[("st5", s)], writes=[("st5", s)])
                    P.op("dve", lambda e, s=s: e.scalar_tensor_tensor(out=u2f[:], in0=ht[:], scalar=st5[s][:, 2:3], in1=G2[:], op0=ALU.mult, op1=ALU.mult),
                         reads=["ht", ("st5", s), "G2"], writes=["u2f"])
                    P.op("pool", lambda e: e.tensor_tensor(out=u2f[:], in0=u2f[:], in1=shf[:], op=ALU.add), reads=["u2f", "shf"], writes=["u2f"])
                    P.op("act", lambda e, s=s: e.activation(out=u2b[s][:], in_=u2f[:], func=AF.Copy), reads=["u2f"], writes=[("u2b", s)])
                    P.dma("sp", lambda e, s=s, tsl=tsl: e.dma_start(out=u2_s[tsl, :], in_=u2b[s][:]), f"y1{s}", reads=[("u2b", s)], writes=[("u2_s", i)])
                    for hf in range(2):
                        def tr5(e, hf=hf):
                            ins = None
                            for k8 in range(8):
                                kc = 8 * hf + k8
                                ins = e.transpose(out=pT5[:, k8, :], in_=u2f[:, 128 * kc:128 * kc + 128], identity=ident_f[:])
                            return ins
                        P.op("pe", tr5, reads=["u2f"], writes=["pT5"])
                        P.op("act", lambda e, hf=hf: e.activation(out=u2T[:, 8 * hf:8 * hf + 8, :], in_=pT5[:], func=AF.Copy), reads=["pT5"], writes=[("u2T", hf)])
                    def mmL(e):
                        ins = None
                        for kc in range(16):
                            ins = e.matmul(pL[:], lhsT=u2T[:, kc, :], rhs=wr[:, kc, :], start=(kc == 0), stop=(kc == 15))
                        return ins
                    P.op("pe", mmL, reads=[("u2T", 0), ("u2T", 1), "wr"], writes=["pL"])
                    P.op("dve", lambda e, i=i: e.tensor_tensor(out=lgall[:, i, :], in0=pL[:], in1=brt[:], op=ALU.add), reads=["pL", "brt"], writes=["lgall"])
                P.dma("sp", lambda e: e.dma_start(out=lg_s, in_=lgall[:]), "y2", reads=["lgall"], writes=["lg_s"])
                P.barrier()

        def phase6():
            with ExitStack() as ps:
                A = lambda n, shp, dt: ps.enter_context(nc.sbuf_tensor("p6_" + n, shp, dt))
                lg = A("lg", [128, 16, 36], F32)
                iota_e = A("iota_e", [128, 32], F32)
                ltf = A("ltf", [128, 128], F32)
                ltb = A("ltb", [128, 128], BF16)
                thrb = A("thrb", [128, 64], F32)
                iotap = A("iotap", [128, 1], F32)
                eidx = A("eidx", [128, 32], F32)
                E = A("E", [128, 32, 32], BF16)
                gm = A("gm", [128, 1], F32)
                sh = A("sh", [128, 4], F32)
                ex = A("ex", [128, 4], F32)
                se = A("se", [128, 1], F32)
                pg = A("pg", [128, 1], F32)
                pen = A("pen", [128, 4], F32)
                lem = A("lem", [128, 32], F32)
                mx8 = A("mx8", [128, 8], F32)
                ix8 = A("ix8", [128, 8], U32)
                dl = A("dl", [128, 4], F32)
                base = A("base", [128, 32, 32], F32)
                tmp = A("tmp", [128, 32, 32], F32)
                cnt = A("cnt", [128, 32], F32)
                ci = A("ci", [128, 32], I32)
                padded = A("padded", [128, 32], F32)
                pend = A("pend", [128, 32], F32)
                pstart = A("pstart", [128, 32], F32)
                dest = A("dest", [128, 32], F32)
                acc = A("acc", [128, 64], F32)
                of = A("of", [128, 64], F32)
                over = A("over", [128, 64], F32)
                pmk = A("pmk", [128, 1], F32)
                u2t = [A(f"u2t{i}", [128, D], BF16) for i in range(2)]
                pPre = ps.enter_context(nc.psum_tensor("p6_pre", [128, 1024], F32))
                pTot = ps.enter_context(nc.psum_tensor("p6_tot", [128, 1024], F32))
                P.dma("sp", lambda e: e.dma_start(out=lg[:], in_=lg_s), "c0", writes=["lg"])
                P.dma("sp", lambda e: e.dma_start(out=iota_e[:], in_=C["iota_e"]), "c1", writes=["iota_e"])
                P.dma("sp", lambda e: e.dma_start(out=ltf[:], in_=C["ltri"]), "c2", writes=["ltf"])
                P.dma("sp", lambda e: e.dma_start(out=thrb[:], in_=C["thrb"]), "c3", writes=["thrb"])
                P.dma("sp", lambda e: e.dma_start(out=iotap[:], in_=C["iota_p"]), "o0", writes=["iotap"])
                CH = ["p6chain"]

                def X(eng, fn):
                    P.op(eng, fn, reads=CH, writes=CH)
                P.op("dve", lambda e: e.tensor_copy(out=ltb[:], in_=ltf[:]), reads=["ltf", "lg", "iota_e", "thrb", "iotap"], writes=CH)
                for t in range(16):
                    lgt = lg[:, t, 0:4]
                    let = lg[:, t, 4:36]
                    X("dve", lambda e, lgt=lgt: e.tensor_reduce(out=gm[:], in_=lgt, axis=AX.X, op=ALU.max))
                    X("dve", lambda e, lgt=lgt: e.tensor_scalar(out=sh[:], in0=lgt, scalar1=gm[:, 0:1], scalar2=None, op0=ALU.subtract))
                    X("dve", lambda e: e.tensor_scalar(out=pen[:], in0=sh[:], scalar1=0.0, scalar2=None, op0=ALU.is_equal))
                    X("dve", lambda e: e.tensor_scalar(out=pen[:], in0=pen[:], scalar1=-1.0, scalar2=1e30, op0=ALU.add, op1=ALU.mult))
                    for g in range(4):
                        X("dve", lambda e, g=g, let=let: e.tensor_scalar(out=lem[:, 8 * g:8 * g + 8], in0=let[:, 8 * g:8 * g + 8], scalar1=pen[:, g:g + 1], scalar2=None, op0=ALU.add))
                    X("act", lambda e: e.activation(out=ex[:], in_=sh[:], func=AF.Exp, accum_out=se[:, 0:1]))
                    X("dve", lambda e: e.max(out=mx8[:], in_=lem[:]))
                    X("dve", lambda e: e.max_index(out=ix8[:], in_max=mx8[:], in_values=lem[:]))
                    X("dve", lambda e, t=t: e.tensor_copy(out=eidx[:, 2 * t:2 * t + 2], in_=ix8[:, 0:2]))
                    X("dve", lambda e: e.tensor_tensor(out=dl[:, 0:1], in0=mx8[:, 1:2], in1=mx8[:, 0:1], op=ALU.subtract))
                    X("act", lambda e: e.activation(out=dl[:, 1:2], in_=dl[:, 0:1], func=AF.Exp))
                    X("dve", lambda e: e.reciprocal(out=pg[:], in_=se[:]))
                    X("dve", lambda e: e.tensor_scalar(out=dl[:, 2:3], in0=dl[:, 1:2], scalar1=1.0, scalar2=None, op0=ALU.add))
                    X("dve", lambda e: e.reciprocal(out=dl[:, 3:4], in_=dl[:, 2:3]))
                    X("dve", lambda e, t=t: e.tensor_tensor(out=wts[:, 2 * t:2 * t + 1], in0=pg[:], in1=dl[:, 3:4], op=ALU.mult))
                    X("dve", lambda e, t=t: e.tensor_tensor(out=wts[:, 2 * t + 1:2 * t + 2], in0=wts[:, 2 * t:2 * t + 1], in1=dl[:, 1:2], op=ALU.mult))
                for g in range(32):
                    X("dve", lambda e, g=g: e.tensor_scalar(out=E[:, g, :], in0=iota_e[:], scalar1=eidx[:, g:g + 1], scalar2=None, op0=ALU.is_equal))
                Ef = E[:].rearrange("p g e -> p (g e)")
                X("pe", lambda e: e.matmul(pPre[:, 0:512], lhsT=ltb[:], rhs=Ef[:, 0:512], start=True, stop=True))
                X("pe", lambda e: e.matmul(pPre[:, 512:1024], lhsT=ltb[:], rhs=Ef[:, 512:1024], start=True, stop=True))
                X("pe", lambda e: e.matmul(pTot[:, 0:512], lhsT=ones_bf[:], rhs=Ef[:, 0:512], start=True, stop=True))
                X("pe", lambda e: e.matmul(pTot[:, 512:1024], lhsT=ones_bf[:], rhs=Ef[:, 512:1024], start=True, stop=True))
                pTv = pTot[:].rearrange("p (g e) -> p g e", e=32)
                pPv = pPre[:].rearrange("p (g e) -> p g e", e=32)
                X("dve", lambda e: e.memset(base[:, 0, :], 0.0))
                for g in range(1, 32):
                    X("dve", lambda e, g=g: e.tensor_tensor(out=base[:, g, :], in0=pTv[:, g - 1, :], in1=base[:, g - 1, :], op=ALU.add))
                X("dve", lambda e: e.tensor_tensor(out=cnt[:], in0=pTv[:, 31, :], in1=base[:, 31, :], op=ALU.add))
                X("dve", lambda e: e.memset(padded[:], 0.0))
                for jj in range(32):
                    X("dve", lambda e, jj=jj: e.scalar_tensor_tensor(out=padded[:], in0=cnt[:], scalar=128.0 * jj, in1=padded[:], op0=ALU.is_gt, op1=ALU.add))
                X("dve", lambda e: e.tensor_scalar(out=padded[:], in0=padded[:], scalar1=128.0, scalar2=None, op0=ALU.mult))
                X("dve", lambda e: e.tensor_tensor_scan(out=pend[:], data0=ones_f[:, 0:32], data1=padded[:], initial=0.0, op0=ALU.mult, op1=ALU.add))
                X("dve", lambda e: e.tensor_tensor(out=pstart[:], in0=pend[:], in1=padded[:], op=ALU.subtract))
                X("dve", lambda e: e.tensor_tensor(out=tmp[:, 0:16, :], in0=pPv[:, 0:16, :], in1=base[:, 0:16, :], op=ALU.add))
                X("dve", lambda e: e.tensor_tensor(out=tmp[:, 16:32, :], in0=pPv[:, 16:32, :], in1=base[:, 16:32, :], op=ALU.add))
                for g in range(32):
                    X("dve", lambda e, g=g: e.tensor_tensor(out=tmp[:, g, :], in0=tmp[:, g, :], in1=pstart[:], op=ALU.add))
                X("dve", lambda e: e.tensor_tensor(out=tmp[:], in0=tmp[:], in1=E[:], op=ALU.mult))
                X("dve", lambda e: e.tensor_reduce(out=dest[:], in_=tmp[:], axis=AX.X, op=ALU.add))
                X("dve", lambda e: e.tensor_copy(out=desti[:], in_=dest[:]))
                X("dve", lambda e: e.memset(acc[:], 0.0))
                for ee in range(32):
                    X("dve", lambda e, ee=ee: e.scalar_tensor_tensor(out=acc[:], in0=thrb[:], scalar=pend[:, ee:ee + 1], in1=acc[:], op0=ALU.is_ge, op1=ALU.add))
                X("dve", lambda e: e.tensor_scalar(out=pmk[:], in0=iotap[:], scalar1=1.0, scalar2=None, op0=ALU.min))
                X("dve", lambda e: e.tensor_scalar(out=over[:], in0=acc[:], scalar1=32.0, scalar2=None, op0=ALU.is_ge))
                X("dve", lambda e: e.tensor_scalar(out=over[:], in0=over[:], scalar1=pmk[:, 0:1], scalar2=100000.0, op0=ALU.mult, op1=ALU.mult))
                X("dve", lambda e: e.tensor_scalar(out=acc[:], in0=acc[:], scalar1=31.0, scalar2=None, op0=ALU.min))
                X("dve", lambda e: e.tensor_scalar(out=of[:], in0=acc[:], scalar1=2048.0, scalar2=iotap[:, 0:1], op0=ALU.mult, op1=ALU.add))
                X("dve", lambda e: e.tensor_copy(out=offs_gu[:], in_=of[:]))
                X("dve", lambda e: e.tensor_scalar(out=of[:], in0=acc[:], scalar1=1024.0, scalar2=iotap[:, 0:1], op0=ALU.mult, op1=ALU.add))
                P.op("dve", lambda e: e.tensor_copy(out=offs_d[:], in_=of[:]), reads=CH, writes=CH + ["desti", "offs"])
                if debug:
                    rt = A("rt", [128, 16, 8], F32)
                    X("dve", lambda e: e.memset(rt[:], 0.0))
                    X("dve", lambda e: e.tensor_copy(out=rt[:, :, 0:2], in_=eidx[:].rearrange("p (t k) -> p t k", k=2)))
                    X("dve", lambda e: e.tensor_copy(out=rt[:, :, 2:4], in_=wts[:].rearrange("p (t k) -> p t k", k=2)))
                    X("dve", lambda e: e.tensor_copy(out=rt[:, :, 4:6], in_=dest[:].rearrange("p (t k) -> p t k", k=2)))
                    P.dma("sp", lambda e: e.dma_start(out=rt_s, in_=rt[:]), "o1", reads=CH, writes=["rt_s"])
                for t in range(16):
                    s = t % 2
                    P.dma("sp", lambda e, s=s, t=t: e.dma_start(out=u2t[s][:], in_=u2_s[128 * t:128 * t + 128, :]), f"w{s}", writes=[("u2t", s)])
                    for k in range(2):
                        g = 2 * t + k
                        P.dma("pool", lambda e, s=s, g=g: e.indirect_dma_start(out=xslot, out_offset=bass.IndirectOffsetOnAxis(ap=desti[:, g:g + 1], axis=0),
                                                                              in_=u2t[s][:], in_offset=None),
                              f"v{s}{k}", reads=[("u2t", s), "desti"], writes=[("xslot", g)])
                P.barrier()

        def phase7():
            with ExitStack() as ps:
                A = lambda n, shp, dt: ps.enter_context(nc.sbuf_tensor("p7_" + n, shp, dt))
                xt7 = [A(f"xt{i}", [128, D], BF16) for i in range(2)]
                xT7 = [A(f"xT{i}", [128, 16, 128], BF16) for i in range(2)]
                guf = [A(f"guf{i}", [128, 1024], F32) for i in range(6)]
                gub = [A(f"gub{i}", [128, 1024], BF16) for i in range(6)]
                dnf = [A(f"dnf{i}", [128, D], F32) for i in range(3)]
                dnb = [A(f"dnb{i}", [128, D], BF16) for i in range(3)]
                sg7 = A("sg", [128, 1024], F32)
                hd7 = A("hd", [128, 1024], BF16)
                hT7 = A("hT", [128, 8, 128], BF16)
                ys7 = [A(f"ys{i}", [128, D], F32) for i in range(2)]
                pGU = ps.enter_context(nc.psum_tensor("p7_GU", [128, 4, 512], F32))
                pTr = [ps.enter_context(nc.psum_tensor(f"p7_Tr{i}", [128, 8, 128], BF16)) for i in range(2)]
                gi = 0
                di = 0
                tri = 0
                def loadx7(b):
                    s = b % 2
                    P.dma("sp", lambda e, s=s, b=b: e.dma_start(out=xt7[s][:], in_=xslot[128 * b:128 * b + 128, :]), f"w{s}", writes=[("xt7", s)])
                if nb7 > 0:
                    loadx7(0)
                for b in range(nb7):
                    s = b % 2
                    if b + 1 < nb7:
                        loadx7(b + 1)
                    for hf in range(2):
                        w = tri % 2; tri += 1
                        def trx(e, s=s, hf=hf, w=w):
                            ins = None
                            for k8 in range(8):
                                kc = 8 * hf + k8
                                ins = e.transpose(out=pTr[w][:, k8, :], in_=xt7[s][:, 128 * kc:128 * kc + 128], identity=ident_bf[:])
                            return ins
                        P.op("pe", trx, reads=[("xt7", s)], writes=[("pTr", w)])
                        P.op("act", lambda e, s=s, hf=hf, w=w: e.activation(out=xT7[s][:, 8 * hf:8 * hf + 8, :], in_=pTr[w][:], func=AF.Copy),
                             reads=[("pTr", w)], writes=[("xT7", s, hf)])
                    for kc in range(16):
                        for which in range(2):
                            r = gi % 6; gi += 1
                            src = I["w_gate"] if which == 0 else I["w_up"]
                            P.dma("pool", lambda e, r=r, b=b, kc=kc, src=src: e.indirect_dma_start(
                                out=guf[r][:], out_offset=None, in_=src, in_offset=bass.IndirectOffsetOnAxis(ap=offs_gu[:, b:b + 1], axis=0),
                                element_offset=kc * 128 * 1024), f"g{r}", reads=["offs"], writes=[("guf", r)])
                            ceng = "act" if (gi % 2 == 0) else "dve"
                            if ceng == "act":
                                P.op("act", lambda e, r=r: e.activation(out=gub[r][:], in_=guf[r][:], func=AF.Copy), reads=[("guf", r)], writes=[("gub", r)])
                            else:
                                P.op("dve", lambda e, r=r: e.tensor_copy(out=gub[r][:], in_=guf[r][:]), reads=[("guf", r)], writes=[("gub", r)])
                            def mmg(e, s=s, r=r, kc=kc, which=which):
                                e.matmul(pGU[:, 2 * which, :], lhsT=xT7[s][:, kc, :], rhs=gub[r][:, 0:512], start=(kc == 0), stop=(kc == 15))
                                return e.matmul(pGU[:, 2 * which + 1, :], lhsT=xT7[s][:, kc, :], rhs=gub[r][:, 512:1024], start=(kc == 0), stop=(kc == 15))
                            P.op("pe", mmg, reads=[("gub", r), ("xT7", s, 0), ("xT7", s, 1)], writes=[("pgu", 2 * which), ("pgu", 2 * which + 1)])
                    P.op("act", lambda e: e.activation(out=sg7[:], in_=pGU[:, 0:2, :].rearrange("p a n -> p (a n)"), func=AF.Silu), reads=[("pgu", 0), ("pgu", 1)], writes=["sg7"])
                    P.op("dve", lambda e: e.tensor_tensor(out=hd7[:], in0=pGU[:, 2:4, :].rearrange("p a n -> p (a n)"), in1=sg7[:], op=ALU.mult),
                         reads=[("pgu", 2), ("pgu", 3), "sg7"], writes=["hd7"])
                    w = tri % 2; tri += 1
                    def trh(e, w=w):
                        ins = None
                        for k8 in range(8):
                            ins = e.transpose(out=pTr[w][:, k8, :], in_=hd7[:, 128 * k8:128 * k8 + 128], identity=ident_bf[:])
                        return ins
                    P.op("pe", trh, reads=["hd7"], writes=[("pTr", w)])
                    P.op("act", lambda e, w=w: e.activation(out=hT7[:], in_=pTr[w][:], func=AF.Copy), reads=[("pTr", w)], writes=["hT7"])
                    for k2 in range(8):
                        r = di % 3; di += 1
                        P.dma("pool", lambda e, r=r, b=b, k2=k2: e.indirect_dma_start(
                            out=dnf[r][:], out_offset=None, in_=I["w_down"], in_offset=bass.IndirectOffsetOnAxis(ap=offs_d[:, b:b + 1], axis=0),
                            element_offset=k2 * 128 * D), f"d{r}", reads=["offs"], writes=[("dnf", r)])
                        P.op("act", lambda e, r=r: e.activation(out=dnb[r][:, 0:1024], in_=dnf[r][:, 0:1024], func=AF.Copy), reads=[("dnf", r)], writes=[("dnb", r, 0)])
                        P.op("dve", lambda e, r=r: e.tensor_copy(out=dnb[r][:, 1024:2048], in_=dnf[r][:, 1024:2048]), reads=[("dnf", r)], writes=[("dnb", r, 1)])
                        def mmd(e, r=r, k2=k2):
                            ins = None
                            for cc in range(4):
                                ins = e.matmul(pGU[:, cc, :], lhsT=hT7[:, k2, :], rhs=dnb[r][:, 512 * cc:512 * cc + 512], start=(k2 == 0), stop=(k2 == 7))
                            return ins
                        P.op("pe", mmd, reads=[("dnb", r, 0), ("dnb", r, 1), "hT7"], writes=[("pgu", 0), ("pgu", 1), ("pgu", 2), ("pgu", 3)])
                    P.op("act", lambda e, s=s: e.activation(out=ys7[s][:, 0:1024], in_=pGU[:, 0:2, :].rearrange("p a n -> p (a n)"), func=AF.Copy),
                         reads=[("pgu", 0), ("pgu", 1)], writes=[("ys7", s, 0)])
                    P.op("dve", lambda e, s=s: e.tensor_copy(out=ys7[s][:, 1024:2048], in_=pGU[:, 2:4, :].rearrange("p a n -> p (a n)")),
                         reads=[("pgu", 2), ("pgu", 3)], writes=[("ys7", s, 1)])
                    P.dma("sp", lambda e, s=s, b=b: e.dma_start(out=yslot[128 * b:128 * b + 128, :], in_=ys7[s][:]), f"o{s}",
                          reads=[("ys7", s, 0), ("ys7", s, 1)], writes=[("yslot", b)])
                P.barrier()

        def phase8(mode):
            full = (mode == 'full')
            with ExitStack() as ps:
                Gf = ps.enter_context(nc.sbuf_tensor("p8_G", [128, D], F32))
                gtf = ps.enter_context(nc.sbuf_tensor("p8_gtf", [128, D], F32))
                xt8 = [ps.enter_context(nc.sbuf_tensor(f"p8_x{i}", [128, D], F32)) for i in range(2)]
                y08 = [ps.enter_context(nc.sbuf_tensor(f"p8_y0{i}", [128, D], F32)) for i in range(2)]
                y18 = [ps.enter_context(nc.sbuf_tensor(f"p8_y1{i}", [128, D], F32)) for i in range(2)]
                ot8 = [ps.enter_context(nc.sbuf_tensor(f"p8_o{i}", [128, D], F32)) for i in range(2)]
                junk8 = ps.enter_context(nc.sbuf_tensor("p8_junk", [128, D], BF16))
                st8 = [ps.enter_context(nc.sbuf_tensor(f"p8_st{i}", [128, 4], F32)) for i in range(2)]
                P.dma("sp", lambda e: e.dma_start(out=Gf[:], in_=I["final_norm_g"].partition_broadcast(128)), "c0", writes=["Gf"])
                if full:
                    P.dma("sp", lambda e: e.dma_start(out=gtf[:], in_=modbc[:, 5 * D:6 * D]), "c1", writes=["gtf"])
                hsrc = I["x"] if mode == 'x' else h_s
                def loadx8(i):
                    s = i % 2
                    P.dma("sp", lambda e, s=s, i=i: e.dma_start(out=xt8[s][:], in_=hsrc[128 * i:128 * i + 128, :]), f"w{s}", writes=[("xt8", s)])
                loadx8(0)
                for i in range(16):
                    s = i % 2
                    if i + 1 < 16:
                        loadx8(i + 1)
                    if full:
                        P.dma("pool", lambda e, s=s, i=i: e.indirect_dma_start(out=y08[s][:], out_offset=None, in_=yslot,
                                                                              in_offset=bass.IndirectOffsetOnAxis(ap=desti[:, 2 * i:2 * i + 1], axis=0)),
                              f"g{s}", writes=[("y08", s)])
                        P.dma("pool", lambda e, s=s, i=i: e.indirect_dma_start(out=y18[s][:], out_offset=None, in_=yslot,
                                                                              in_offset=bass.IndirectOffsetOnAxis(ap=desti[:, 2 * i + 1:2 * i + 2], axis=0)),
                              f"d{s}", writes=[("y18", s)])
                        P.op("dve", lambda e, s=s, i=i: e.tensor_scalar(out=y08[s][:], in0=y08[s][:], scalar1=wts[:, 2 * i:2 * i + 1], scalar2=None, op0=ALU.mult),
                             reads=[("y08", s)], writes=[("y08", s)])
                        P.op("dve", lambda e, s=s, i=i: e.scalar_tensor_tensor(out=y18[s][:], in0=y18[s][:], scalar=wts[:, 2 * i + 1:2 * i + 2], in1=y08[s][:], op0=ALU.mult, op1=ALU.add),
                             reads=[("y08", s), ("y18", s)], writes=[("y18", s)])
                        P.op("pool", lambda e, s=s: e.tensor_tensor(out=y18[s][:], in0=y18[s][:], in1=gtf[:], op=ALU.mult), reads=[("y18", s), "gtf"], writes=[("y18", s)])
                        P.op("pool", lambda e, s=s: e.tensor_tensor(out=xt8[s][:], in0=xt8[s][:], in1=y18[s][:], op=ALU.add), reads=[("y18", s), ("xt8", s)], writes=[("xt8", s)])
                    P.op("act", lambda e, s=s: e.activation(out=junk8[:], in_=xt8[s][:], func=AF.Square, accum_out=st8[s][:, 0:1]),
                         reads=[("xt8", s)], writes=["junk8", ("st8", s)])
                    P.op("act", lambda e, s=s: e.activation(out=st8[s][:, 1:2], in_=st8[s][:, 0:1], func=AF.Sqrt, scale=1.0 / D, bias=EPS),
                         reads=[("st8", s)], writes=[("st8", s)])
                    P.op("dve", lambda e, s=s: e.reciprocal(out=st8[s][:, 2:3], in_=st8[s][:, 1:2]), reads=[("st8", s)], writes=[("st8", s)])
                    P.op("dve", lambda e, s=s: e.scalar_tensor_tensor(out=ot8[s][:], in0=xt8[s][:], scalar=st8[s][:, 2:3], in1=Gf[:], op0=ALU.mult, op1=ALU.mult),
                         reads=[("xt8", s), ("st8", s), "Gf"], writes=[("ot8", s)])
                    P.dma("sp", lambda e, s=s, i=i: e.dma_start(out=out[128 * i:128 * i + 128, :], in_=ot8[s][:]), f"o{s}", reads=[("ot8", s)], writes=[("out", i)])
                P.barrier()
        if stop_after >= 4 and 4 not in skip:
            phase4()
        if stop_after >= 5 and 5 not in skip:
            phase5a()
        if stop_after >= 5 and 55 not in skip:
            phase5b()
        if stop_after >= 6 and 6 not in skip:
            phase6()
        if stop_after >= 7 and 7 not in skip:
            phase7()
        phase8('full' if (stop_after >= 7 and 7 not in skip) else ('h' if (stop_after >= 5 and 55 not in skip) else 'x'))
        P.barrier(); P.emit(); return nc
        if debug and stop_after <= 1:
            uT_d = scratch('s_uT', [128, 16, S], BF16)
            P.dma('sp', lambda e: e.dma_start(out=uT_d, in_=uT[:]), 'c0', reads=['uT'], writes=['uT_d'])
            P.barrier()
        P.emit()
    return nc


def _prep(inp, b):
    m = {}
    m["x"] = np.ascontiguousarray(inp["x"][b])
    m["c_t"] = np.ascontiguousarray(np.asarray(inp["c"][b]).reshape(16, 128).T)
    m["pos"] = np.ascontiguousarray(inp["positions"][b])
    m["ada_w"] = inp["ada_w"][0]
    m["ada_b"] = inp["ada_b"][0]
    m["mix_norm_g"] = inp["mix_norm_g"][0]
    m["w_in"] = inp["w_in"][0]
    m["w_a"] = inp["w_attn_branch"][0]
    m["w_r"] = inp["w_rec_branch"][0]
    m["w_o"] = inp["w_mix_out"][0]
    m["rec_norm_g"] = np.ascontiguousarray(np.asarray(inp["rec_norm_g"][0]).reshape(128, 1))
    m["lb_logits_t"] = np.ascontiguousarray(np.asarray(inp["rec_lb_logits"]).reshape(2, 16, 128).transpose(2, 0, 1))
    m["ffn_norm_g"] = inp["ffn_norm_g"][0]
    m["w_router"] = np.ascontiguousarray(np.concatenate([inp["router_group_w"][0], inp["router_expert_w"][0]], axis=1))
    m["b_router"] = np.ascontiguousarray(np.concatenate([inp["router_group_b"][0], inp["router_expert_b"][0]]))
    m["w_gate"] = np.asarray(inp["expert_w_gate"][0]).reshape(32 * D, 1024)
    m["w_up"] = np.asarray(inp["expert_w_up"][0]).reshape(32 * D, 1024)
    m["w_down"] = np.asarray(inp["expert_w_down"][0]).reshape(32 * 1024, D)
    m["final_norm_g"] = inp["final_norm_g"]
    return m


STOP_AFTER = 7


def kernel(**inputs):
    inp = {k: np.asarray(v) for k, v in inputs.items()}
    consts = {"c_" + k: v for k, v in host_consts().items()}
    nc = build(stop_after=STOP_AFTER)
    in_maps = []
    for b in range(8):
        m = _prep(inp, b)
        m.update(consts)
        in_maps.append(m)
    res = run_bass_kernel_spmd(nc, in_maps, core_ids=list(range(8)))
    return np.stack([np.asarray(r["out"], dtype=np.float32) for r in res.results], axis=0)
```

```python
import numpy as np
from contextlib import ExitStack
import concourse.bass as bass
import concourse.mybir as mybir
from concourse.bass_utils import run_bass_kernel_spmd

F32 = mybir.dt.float32
BF16 = mybir.dt.bfloat16
I32 = mybir.dt.int32
U32 = mybir.dt.uint32
AF = mybir.ActivationFunctionType
ALU = mybir.AluOpType
AX = mybir.AxisListType

D = 2048
S = 2048
NH = 16
DH = 128
INW = 18432
EPS = 1e-6
NEG = -30000.0
ROPE_THETA = 500000.0


class Prog:
    COMPUTE = ("pe", "act", "dve", "pool")
    ENG = ("pe", "act", "dve", "pool", "sp")

    def __init__(self, nc, es):
        self.nc, self.es = nc, es
        self.streams = {e: [] for e in self.ENG}
        self.esem = {e: es.enter_context(nc.semaphore(f"sem_{e}")) for e in self.COMPUTE}
        self.ecount = {e: 0 for e in self.COMPUTE}
        self.known = {e: {} for e in self.ENG}
        self.state = {}
        self.dsem = {}

    def _need(self, eng, ev):
        if ev is None:
            return
        name, sem, val, owner = ev
        if owner == eng and eng == "pe":
            return
        if self.known[eng].get(name, 0) >= val:
            return
        self.known[eng][name] = val
        self.streams[eng].append(("wait", sem, val))

    def _deps(self, eng, reads, writes):
        for k in reads:
            st = self.state.get(k)
            if st:
                self._need(eng, st["w"])
        for k in writes:
            st = self.state.get(k)
            if st:
                self._need(eng, st["w"])
                for ev in st["r"]:
                    self._need(eng, ev)

    def _commit(self, ev, reads, writes):
        for k in reads:
            st = self.state.setdefault(k, {"w": None, "r": []})
            st["r"].append(ev)
            if len(st["r"]) > 24:
                st["r"] = st["r"][-24:] if False else st["r"]
        for k in writes:
            self.state[k] = {"w": ev, "r": []}

    def op(self, eng, fn, reads=(), writes=()):
        self._deps(eng, reads, writes)
        self.ecount[eng] += 1
        ev = (f"e_{eng}", self.esem[eng], self.ecount[eng], eng)
        self.streams[eng].append(("op", fn, self.esem[eng], 1))
        self._commit(ev, reads, writes)
        return ev

    def dma(self, q, fn, semname, reads=(), writes=()):
        self._deps(q, reads, writes)
        if semname not in self.dsem:
            self.dsem[semname] = [self.es.enter_context(self.nc.semaphore(f"d_{semname}")), 0]
        d = self.dsem[semname]
        d[1] += 16
        ev = (f"d_{semname}", d[0], d[1], "dma")
        self.streams[q].append(("op", fn, d[0], 16))
        self._commit(ev, reads, writes)
        return ev

    def barrier(self):
        evs = []
        for e in self.COMPUTE:
            if self.ecount[e]:
                evs.append((f"e_{e}", self.esem[e], self.ecount[e], e))
        for name, (sem, cnt) in self.dsem.items():
            if cnt:
                evs.append((f"d_{name}", sem, cnt, "dma"))
        for eng in self.ENG:
            for ev in evs:
                self._need(eng, ev)
        self.state = {}

    def emit(self):
        nc = self.nc
        engmap = {"pe": "tensor", "act": "scalar", "dve": "vector", "pool": "gpsimd", "sp": "sync"}
        with nc.Block() as block:
            for e in self.ENG:
                stream = self.streams[e]
                if not stream:
                    continue

                def body(eng, stream=stream):
                    for item in stream:
                        if item[0] == "wait":
                            eng.wait_ge(item[1], item[2])
                        else:
                            ins = item[1](eng)
                            ins.then_inc(item[2], item[3])

                getattr(block, engmap[e])(body)


def host_consts():
    c = {}
    c["ident_bf"] = np.eye(128, dtype=np.float32)
    c["ones_f"] = np.ones((128, 128), np.float32)
    inv = ROPE_THETA ** (-np.arange(0, 32, 2, dtype=np.float32) / np.float32(32))
    c["invf"] = np.concatenate([inv, inv]).astype(np.float32).reshape(32, 1)
    c["sinsign"] = np.concatenate([-np.ones(16), np.ones(16)]).astype(np.float32).reshape(32, 1)
    pm = np.eye(128, dtype=np.float32)
    pm[:32, :32] = 0.0
    for m in range(32):
        pm[(m + 16) % 32, m] = 1.0
    c["perm"] = pm
    kk = np.arange(128)[:, None]
    qq = np.arange(128)[None, :]
    mb = np.zeros((128, 256), np.float32)
    mb[:, :128] = np.where(kk <= qq, 0.0, NEG)
    mb[:, 128:] = np.where(kk >= qq, 0.0, NEG)
    c["maskb"] = mb
    rm = np.ones((128, S), np.float32)
    rm[:, ::64] = 0.0
    c["resetm"] = rm
    ss = np.arange(64)[:, None]
    tt = np.arange(64)[None, :]
    c["causal"] = np.tile((ss <= tt).astype(np.float32), (1, 8))
    c["ltri"] = (np.arange(128)[:, None] < np.arange(128)[None, :]).astype(np.float32)
    c["iota_p"] = np.arange(128, dtype=np.float32).reshape(128, 1)
    c["thrb"] = np.broadcast_to((np.arange(64, dtype=np.float32) * 128.0)[None, :], (128, 64)).copy()
    c["iota_e"] = np.broadcast_to(np.arange(32, dtype=np.float32)[None, :], (128, 32)).copy()
    return c


CONST_SHAPES = {k: v.shape for k, v in host_consts().items()}

IN_SHAPES = {
    "x": ([S, D], F32), "c_t": ([128, 16], F32), "pos": ([S], I32),
    "ada_w": ([D, 6 * D], F32), "ada_b": ([6 * D], F32), "mix_norm_g": ([D], F32),
    "w_in": ([D, INW], F32), "w_a": ([D, D], F32), "w_r": ([D, D], F32), "w_o": ([D, D], F32),
    "rec_norm_g": ([128, 1], F32), "lb_logits_t": ([128, 2, 16], F32), "ffn_norm_g": ([D], F32),
    "w_router": ([D, 36], F32), "b_router": ([36], F32),
    "w_gate": ([32 * D, 1024], F32), "w_up": ([32 * D, 1024], F32), "w_down": ([32 * 1024, D], F32),
    "final_norm_g": ([D], F32),
}


def build(stop_after=99, debug=False, rot=3, trig=True, p2_ct=72, skip=(), nh3=NH, nh4=NH, nb7=64, p3v=3, p3rs=(1, 4, 16)):
    nc = bass.Bass("TRN2", target_bir_lowering=False)
    I = {}
    for k, (shp, dt) in IN_SHAPES.items():
        I[k] = nc.dram_tensor(k, list(shp), dt, kind="ExternalInput").ap()
    C = {}
    for k, shp in CONST_SHAPES.items():
        C[k] = nc.dram_tensor("c_" + k, list(shp), F32, kind="ExternalInput").ap()
    out = nc.dram_tensor("out", [S, D], F32, kind="ExternalOutput").ap()
    skind = "ExternalOutput" if debug else "Internal"

    def scratch(name, shape, dt):
        return nc.dram_tensor(name, list(shape), dt, kind=skind).ap()

    modbc = scratch("s_modbc", [128, 6 * D], F32)
    qT_s = scratch("s_qT", [NH, 128, S], BF16)
    kT_s = scratch("s_kT", [NH, 128, S], BF16)
    v_s = scratch("s_v", [S, D], BF16)
    qr_s = scratch("s_qr", [NH, 128, S], BF16)
    f_s = scratch("s_f", [NH, 128, S], F32)
    i_s = scratch("s_i", [S, D], BF16)
    g_s = scratch("s_g", [NH, 128, S], BF16)
    ga_s = scratch("s_ga", [NH, 128, S], BF16)
    gr_s = scratch("s_gr", [NH, 128, S], BF16)
    ya_s = scratch("s_ya", [NH, 128, S], BF16)
    yr_s = scratch("s_yr", [NH, 128, S], BF16)
    m_s = scratch("s_m", [NH, 128, S], BF16)
    h_s = scratch("s_h", [S, D], F32)
    u2_s = scratch("s_u2", [S, D], BF16)
    lg_s = scratch("s_lg", [128, 16, 36], F32)
    xslot = scratch("s_xslot", [64 * 128, D], BF16)
    yslot = scratch("s_yslot", [64 * 128, D], F32)
    rt_s = scratch("s_rt", [128, 16, 8], F32)

    with ExitStack() as es:
        P = Prog(nc, es)
        ident_bf = es.enter_context(nc.sbuf_tensor("ident_bf", [128, 128], BF16))
        ident_f = es.enter_context(nc.sbuf_tensor("ident_f", [128, 128], F32))
        ones_f = es.enter_context(nc.sbuf_tensor("ones_f", [128, 128], F32))
        ones_bf = es.enter_context(nc.sbuf_tensor("ones_bf", [128, 128], BF16))
        P.dma("sp", lambda e: e.dma_start(out=ident_f[:], in_=C["ident_bf"]), "c0", writes=["ident_f"])
        P.dma("sp", lambda e: e.dma_start(out=ones_f[:], in_=C["ones_f"]), "c1", writes=["ones_f"])
        P.op("dve", lambda e: e.tensor_copy(out=ident_bf[:], in_=ident_f[:]), reads=["ident_f"], writes=["ident_bf"])
        P.op("dve", lambda e: e.tensor_copy(out=ones_bf[:], in_=ones_f[:]), reads=["ones_f"], writes=["ones_bf"])
        desti = es.enter_context(nc.sbuf_tensor("desti", [128, 32], I32))
        wts = es.enter_context(nc.sbuf_tensor("wts", [128, 32], F32))
        offs_gu = es.enter_context(nc.sbuf_tensor("offs_gu", [128, 64], I32))
        offs_d = es.enter_context(nc.sbuf_tensor("offs_d", [128, 64], I32))
        P.barrier()

        with ExitStack() as ps:
            c_sb = ps.enter_context(nc.sbuf_tensor("c_sb", [128, 16], F32))
            L = ps.enter_context(nc.sbuf_tensor("adaL", [128, 16, 128], BF16))
            modsb = ps.enter_context(nc.sbuf_tensor("modsb", [128, 6 * D], F32))
            wst = [ps.enter_context(nc.sbuf_tensor(f"a_wst{i}", [128, 16, 512], F32)) for i in range(2)]
            wbf = [ps.enter_context(nc.sbuf_tensor(f"a_wbf{i}", [128, 16, 512], BF16)) for i in range(2)]
            pp = [ps.enter_context(nc.psum_tensor(f"a_ps{i}", [128, 512], F32)) for i in range(2)]
            P.dma("sp", lambda e: e.dma_start(out=c_sb[:], in_=I["c_t"]), "c0", writes=["c_sb"])
            P.dma("sp", lambda e: e.dma_start(out=modsb[:], in_=I["ada_b"].partition_broadcast(128)), "c1", writes=["modsb"])
            P.op("act", lambda e: e.activation(out=c_sb[:], in_=c_sb[:], func=AF.Silu), reads=["c_sb"], writes=["c_sb"])

            def mkL(e):
                ins = None
                for j in range(16):
                    ins = e.tensor_scalar(out=L[:, j, :], in0=ones_f[:], scalar1=c_sb[:, j:j + 1], scalar2=None, op0=ALU.mult)
                return ins
            P.op("dve", mkL, reads=["c_sb"], writes=["L"])
            awv = I["ada_w"].rearrange("(kc p) n -> p kc n", p=128)
            for ct in range(0 if 0 in skip else 24):
                s = ct % 2
                P.dma("sp", lambda e, s=s, ct=ct: e.dma_start(out=wst[s][:], in_=awv[:, :, 512 * ct:512 * ct + 512]),
                      f"w{s}", writes=[("wst", s)])
                P.op("pool", lambda e, s=s: e.tensor_copy(out=wbf[s][:], in_=wst[s][:]), reads=[("wst", s)], writes=[("wbf", s)])

                def mm(e, s=s):
                    ins = None
                    for kc in range(16):
                        ins = e.matmul(pp[s][:], lhsT=L[:, kc, :], rhs=wbf[s][:, kc, :], start=(kc == 0), stop=(kc == 15))
                    return ins
                P.op("pe", mm, reads=["L", ("wbf", s)], writes=[("pp", s)])
                P.op("dve", lambda e, s=s, ct=ct: e.tensor_tensor(out=modsb[:, 512 * ct:512 * ct + 512], in0=pp[s][:],
                                                                 in1=modsb[:, 512 * ct:512 * ct + 512], op=ALU.add),
                     reads=[("pp", s), "modsb"], writes=["modsb"])
            P.dma("sp", lambda e: e.dma_start(out=modbc, in_=modsb[:]), "c0", reads=["modsb"], writes=["modbc"])
            P.barrier()
        if stop_after <= 0:
            P.barrier(); P.emit(); return nc

        ps12 = es.enter_context(ExitStack())
        uT = ps12.enter_context(nc.sbuf_tensor("uT", [128, 16, S], BF16))
        with ExitStack() as ps:
            G1 = ps.enter_context(nc.sbuf_tensor("G1", [128, D], F32))
            shm = ps.enter_context(nc.sbuf_tensor("shm", [128, D], F32))
            tmpg = ps.enter_context(nc.sbuf_tensor("tmpg", [128, D], F32))
            xt = [ps.enter_context(nc.sbuf_tensor(f"xt{i}", [128, D], F32)) for i in range(2)]
            junk = ps.enter_context(nc.sbuf_tensor("junk", [128, D], BF16))
            tf = ps.enter_context(nc.sbuf_tensor("tf", [128, D], F32))
            ub = [ps.enter_context(nc.sbuf_tensor(f"ub{i}", [128, D], BF16)) for i in range(2)]
            st = [ps.enter_context(nc.sbuf_tensor(f"st{i}", [128, 4], F32)) for i in range(2)]
            ptr = [ps.enter_context(nc.psum_tensor(f"ptr{i}", [128, 16, 128], BF16)) for i in range(2)]
            P.dma("sp", lambda e: e.dma_start(out=shm[:], in_=modbc[:, 0:D]), "c0", writes=["shm"])
            P.dma("sp", lambda e: e.dma_start(out=tmpg[:], in_=modbc[:, D:2 * D]), "c1", writes=["tmpg"])
            P.dma("sp", lambda e: e.dma_start(out=G1[:], in_=I["mix_norm_g"].partition_broadcast(128)), "c2", writes=["G1"])
            P.op("dve", lambda e: e.scalar_tensor_tensor(out=G1[:], in0=tmpg[:], scalar=1.0, in1=G1[:], op0=ALU.add, op1=ALU.mult),
                 reads=["tmpg", "G1"], writes=["G1"])
            for i in range(0 if 1 in skip else 16):
                s = i % 2
                P.dma("sp", lambda e, s=s, i=i: e.dma_start(out=xt[s][:], in_=I["x"][128 * i:128 * i + 128, :]), f"w{s}", writes=[("xt", s)])
                P.op("act", lambda e, s=s: e.activation(out=junk[:], in_=xt[s][:], func=AF.Square, accum_out=st[s][:, 0:1]),
                     reads=[("xt", s)], writes=["junk", ("st", s)])
                P.op("act", lambda e, s=s: e.activation(out=st[s][:, 1:2], in_=st[s][:, 0:1], func=AF.Sqrt, scale=1.0 / D, bias=EPS),
                     reads=[("st", s)], writes=[("st", s)])
                P.op("dve", lambda e, s=s: e.reciprocal(out=st[s][:, 2:3], in_=st[s][:, 1:2]), reads=[("st", s)], writes=[("st", s)])
                P.op("dve", lambda e, s=s: e.scalar_tensor_tensor(out=tf[:], in0=xt[s][:], scalar=st[s][:, 2:3], in1=G1[:], op0=ALU.mult, op1=ALU.mult),
                     reads=[("xt", s), ("st", s), "G1"], writes=["tf"])
                P.op("pool", lambda e, s=s: e.tensor_tensor(out=ub[s][:], in0=tf[:], in1=shm[:], op=ALU.add),
                     reads=["tf", "shm"], writes=[("ub", s)])

                def tr(e, s=s):
                    ins = None
                    for kc in range(16):
                        ins = e.transpose(out=ptr[s][:, kc, :], in_=ub[s][:, 128 * kc:128 * kc + 128], identity=ident_bf[:])
                    return ins
                P.op("pe", tr, reads=[("ub", s), "ident_bf"], writes=[("ptr", s)])
                P.op("act", lambda e, s=s, i=i: e.activation(out=uT[:, :, 128 * i:128 * i + 128], in_=ptr[s][:], func=AF.Copy),
                     reads=[("ptr", s)], writes=["uT"])
            P.barrier()

        if stop_after <= 1:
            P.barrier(); P.emit(); return nc
        PI = float(np.pi)
        with ExitStack() as ps:
            cosF = ps.enter_context(nc.sbuf_tensor("cosF", [128, S], F32))
            sinF = ps.enter_context(nc.sbuf_tensor("sinF", [128, S], F32))
            permb = ps.enter_context(nc.sbuf_tensor("permb", [128, 128], BF16))
            permf = ps.enter_context(nc.sbuf_tensor("permf", [128, 128], F32))
            with ExitStack() as ts:
                cosT = ts.enter_context(nc.sbuf_tensor("cosT", [32, S], F32))
                sinT = ts.enter_context(nc.sbuf_tensor("sinT", [32, S], F32))
                posi = ts.enter_context(nc.sbuf_tensor("posi", [32, S], I32))
                ang = ts.enter_context(nc.sbuf_tensor("ang", [32, S], F32))
                kq = ts.enter_context(nc.sbuf_tensor("kq", [32, S], F32))
                ki = ts.enter_context(nc.sbuf_tensor("ki", [32, S], I32))
                rr = ts.enter_context(nc.sbuf_tensor("rr", [32, S], F32))
                mm_ = ts.enter_context(nc.sbuf_tensor("mm_", [32, S], F32))
                sm = ts.enter_context(nc.sbuf_tensor("sm", [32, 40], F32))
                P.dma("sp", lambda e: e.dma_start(out=posi[:], in_=I["pos"].partition_broadcast(32)), "c0", writes=["posi"])
                P.dma("sp", lambda e: e.dma_start(out=sm[:, 0:1], in_=C["invf"]), "c1", writes=["sm0"])
                P.dma("sp", lambda e: e.dma_start(out=sm[:, 1:2], in_=C["sinsign"]), "c2", writes=["sm1"])
                P.dma("sp", lambda e: e.dma_start(out=permf[:], in_=C["perm"]), "c3", writes=["permf"])
                P.op("dve", lambda e: e.tensor_copy(out=permb[:], in_=permf[:]), reads=["permf"], writes=["permb"])
                P.op("dve", lambda e: e.tensor_copy(out=ang[:], in_=posi[:]), reads=["posi"], writes=["ang"])
                P.op("dve", lambda e: e.tensor_scalar(out=ang[:], in0=ang[:], scalar1=sm[:, 0:1], scalar2=None, op0=ALU.mult),
                     reads=["ang", "sm0"], writes=["ang"])

                def reduce_to(dst, shift, tag):
                    P.op("dve", lambda e: e.tensor_scalar(out=kq[:], in0=ang[:], scalar1=shift, scalar2=1.0 / (2 * PI), op0=ALU.add, op1=ALU.mult),
                         reads=["ang"], writes=["kq"])
                    P.op("dve", lambda e: e.tensor_copy(out=ki[:], in_=kq[:]), reads=["kq"], writes=["ki"])
                    P.op("dve", lambda e: e.tensor_copy(out=kq[:], in_=ki[:]), reads=["ki"], writes=["kq"])
                    P.op("dve", lambda e: e.scalar_tensor_tensor(out=rr[:], in0=kq[:], scalar=-2 * PI, in1=ang[:], op0=ALU.mult, op1=ALU.add),
                         reads=["kq", "ang"], writes=["rr"])
                    if shift != 0.0:
                        P.op("dve", lambda e: e.tensor_scalar(out=rr[:], in0=rr[:], scalar1=shift, scalar2=None, op0=ALU.add),
                             reads=["rr"], writes=["rr"])
                    P.op("dve", lambda e: e.tensor_scalar(out=mm_[:], in0=rr[:], scalar1=PI, scalar2=-2 * PI, op0=ALU.is_gt, op1=ALU.mult),
                         reads=["rr"], writes=["mm_"])
                    P.op("dve", lambda e: e.tensor_tensor(out=rr[:], in0=rr[:], in1=mm_[:], op=ALU.add), reads=["rr", "mm_"], writes=["rr"])
                    P.op("dve", lambda e: e.tensor_scalar(out=mm_[:], in0=rr[:], scalar1=-PI, scalar2=2 * PI, op0=ALU.is_lt, op1=ALU.mult),
                         reads=["rr"], writes=["mm_"])
                    P.op("dve", lambda e: e.tensor_tensor(out=rr[:], in0=rr[:], in1=mm_[:], op=ALU.add), reads=["rr", "mm_"], writes=["rr"])
                    P.op("dve", lambda e: e.tensor_scalar(out=rr[:], in0=rr[:], scalar1=3.1415925, scalar2=-3.1415925, op0=ALU.min, op1=ALU.max),
                         reads=["rr"], writes=["rr"])
                    P.op("act", lambda e: e.activation(out=dst[:], in_=rr[:], func=AF.Sin), reads=["rr"], writes=[tag])
                reduce_to(sinT, 0.0, "sinT")
                P.op("dve", lambda e: e.tensor_scalar(out=sinT[:], in0=sinT[:], scalar1=sm[:, 1:2], scalar2=None, op0=ALU.mult),
                     reads=["sinT", "sm1"], writes=["sinT"])
                reduce_to(cosT, PI / 2, "cosT")
                P.op("pool", lambda e: e.memset(cosF[:], 1.0), writes=["cosF"])
                P.op("pool", lambda e: e.memset(sinF[:], 0.0), writes=["sinF"])
                P.op("dve", lambda e: e.tensor_copy(out=cosF[0:32, :], in_=cosT[:]), reads=["cosT", "cosF"], writes=["cosF"])
                P.op("dve", lambda e: e.tensor_copy(out=sinF[0:32, :], in_=sinT[:]), reads=["sinT", "sinF"], writes=["sinF"])
                P.barrier()

            wst2 = [ps.enter_context(nc.sbuf_tensor(f"p2_wst{i}", [128, 16, 256], F32)) for i in range(2)]
            wbf2 = [ps.enter_context(nc.sbuf_tensor(f"p2_wbf{i}", [128, 16, 256], BF16)) for i in range(2)]
            stg = [ps.enter_context(nc.sbuf_tensor(f"p2_stg{i}", [128, S], BF16)) for i in range(2)]
            stgf = [ps.enter_context(nc.sbuf_tensor(f"p2_stgf{i}", [128, S], F32)) for i in range(2)]
            stgt = [ps.enter_context(nc.sbuf_tensor(f"p2_stgt{i}", [128, 256], BF16)) for i in range(2)]
            rt1 = ps.enter_context(nc.sbuf_tensor("p2_rt1", [128, 512], F32))
            rt2 = ps.enter_context(nc.sbuf_tensor("p2_rt2", [128, 512], F32))
            xb = ps.enter_context(nc.sbuf_tensor("p2_xb", [128, 512], BF16))
            xf = ps.enter_context(nc.sbuf_tensor("p2_xf", [128, 512], F32))
            pf = ps.enter_context(nc.sbuf_tensor("p2_pf", [128, 512], F32))
            pp2 = [ps.enter_context(nc.psum_tensor(f"p2_ps{i}", [128, 512], F32)) for i in range(4)]
            psw = [ps.enter_context(nc.psum_tensor(f"p2_psw{i}", [128, 512], F32)) for i in range(2)]
            wv = I["w_in"].rearrange("(kc p) n -> p kc n", p=128)
            SEC = [("rot", qT_s), ("rot", kT_s), ("tok", v_s), ("silu", qr_s), ("f32", f_s), ("tok", i_s), ("silu", g_s), ("sig", ga_s), ("sig", gr_s)]
            ppi = 0
            swi = 0
            sti = 0
            stt = 0
            for ct in range(0 if 2 in skip else p2_ct):
                s = ct % 2
                sec = ct // 8
                kind, dst = SEC[sec]
                if ct == 0:
                    P.dma("act", lambda e: e.dma_start(out=wst2[0][:], in_=wv[:, :, 0:256]), "w0", writes=[("wst", 0)])
                if ct + 1 < p2_ct:
                    P.dma("act", lambda e, s1=(ct + 1) % 2, c1=ct + 1: e.dma_start(out=wst2[s1][:], in_=wv[:, :, 256 * c1:256 * c1 + 256]), f"w{(ct + 1) % 2}", writes=[("wst", (ct + 1) % 2)])
                P.op("pool", lambda e, s=s: e.tensor_copy(out=wbf2[s][:], in_=wst2[s][:]), reads=[("wst", s)], writes=[("wbf", s)])
                if kind == "tok":
                    c0 = 256 * (ct % 8)
                    for tt in range(16):
                        b = ppi % 4; ppi += 1
                        def mm(e, s=s, tt=tt, b=b):
                            ins = None
                            for kc in range(16):
                                ins = e.matmul(pp2[b][:, 0:256], lhsT=uT[:, kc, 128 * tt:128 * tt + 128], rhs=wbf2[s][:, kc, :], start=(kc == 0), stop=(kc == 15))
                            return ins
                        P.op("pe", mm, reads=[("wbf", s)], writes=[("pp", b)])
                        q = stt % 2; stt += 1
                        P.op("act", lambda e, b=b, q=q: e.activation(out=stgt[q][:], in_=pp2[b][:, 0:256], func=AF.Copy), reads=[("pp", b)], writes=[("stgt", q)])
                        P.dma("sp", lambda e, q=q, tt=tt, c0=c0, dst=dst: e.dma_start(out=dst[128 * tt:128 * tt + 128, c0:c0 + 256], in_=stgt[q][:]),
                              f"o{q}", reads=[("stgt", q)], writes=[("dtok", ct, tt)])
                    continue
                for sub in range(2):
                    h = (ct % 8) * 2 + sub
                    q = sti % 2; sti += 1
                    sbuf_dst = stgf[q] if kind == "f32" else stg[q]
                    skey = ("stgf", q) if kind == "f32" else ("stg", q)
                    for tc in range(4):
                        b = ppi % 4; ppi += 1
                        def mm(e, s=s, sub=sub, tc=tc, b=b):
                            ins = None
                            for kc in range(16):
                                ins = e.matmul(pp2[b][:], lhsT=wbf2[s][:, kc, 128 * sub:128 * sub + 128], rhs=uT[:, kc, 512 * tc:512 * tc + 512], start=(kc == 0), stop=(kc == 15))
                            return ins
                        P.op("pe", mm, reads=[("wbf", s)], writes=[("pp", b)])
                        func = {"rot": AF.Copy, "silu": AF.Silu, "f32": AF.Copy, "sig": AF.Sigmoid}[kind]
                        cs = slice(512 * tc, 512 * tc + 512)
                        if kind == "rot" and rot:
                            w = swi % 2; swi += 1
                            P.op("act", lambda e, b=b: e.activation(out=xb[:], in_=pp2[b][:], func=AF.Copy), reads=[("pp", b)], writes=["xb"])
                            P.op("act", lambda e, b=b: e.activation(out=xf[:], in_=pp2[b][:], func=AF.Copy), reads=[("pp", b)], writes=["xf"])
                            P.op("pe", lambda e, w=w: e.matmul(psw[w][:], lhsT=permb[:], rhs=xb[:], start=True, stop=True),
                                 reads=["xb"], writes=[("psw", w)])
                            P.op("act", lambda e, w=w: e.activation(out=pf[:], in_=psw[w][:], func=AF.Copy), reads=[("psw", w)], writes=["pf"])
                            P.op("dve", lambda e, cs=cs: e.tensor_tensor(out=rt1[:], in0=xf[:], in1=cosF[:, cs], op=ALU.mult),
                                 reads=["xf"], writes=["rt1"])
                            P.op("dve", lambda e, cs=cs: e.tensor_tensor(out=rt2[:], in0=pf[:], in1=sinF[:, cs], op=ALU.mult),
                                 reads=["pf"], writes=["rt2"])
                            P.op("pool", lambda e, cs=cs, sbuf_dst=sbuf_dst: e.tensor_tensor(out=sbuf_dst[:, cs], in0=rt1[:], in1=rt2[:], op=ALU.add),
                                 reads=["rt1", "rt2"], writes=[skey])
                        else:
                            P.op("act", lambda e, b=b, cs=cs, sbuf_dst=sbuf_dst, func=func: e.activation(out=sbuf_dst[:, cs], in_=pp2[b][:], func=func),
                                 reads=[("pp", b)], writes=[skey])
                    P.dma("sp", lambda e, h=h, dst=dst, sbuf_dst=sbuf_dst: e.dma_start(out=dst[h], in_=sbuf_dst[:]), f"o{q}f" if kind == "f32" else f"o{q}",
                          reads=[skey], writes=[("dfm", sec, h)])
            P.barrier()

        ps12.close()
        if stop_after <= 2:
            P.barrier(); P.emit(); return nc

        def phase4():
            with ExitStack() as ps:
                A = lambda n, shp, dt: ps.enter_context(nc.sbuf_tensor("p4_" + n, shp, dt))
                lbl = A("lbl", [128, 2, 16], F32)
                lbT = A("lbT", [128, 16], F32)
                oml = A("oml", [128, 16], F32)
                noml = A("noml", [128, 16], F32)
                rng = A("rng", [128, 1], F32)
                resetm = A("resetm", [128, S], F32)
                causal = A("causal", [64, 512], F32)
                f4 = A("f", [128, S], F32)
                lf = A("lf", [128, S], F32)
                kk = A("kk", [128, S], F32)
                bcs = A("bcs", [128, S], F32)
                eb = A("eb", [128, S], F32)
                enb = A("enb", [128, S], F32)
                oT = A("oT", [128, S], F32)
                qr4 = A("qr", [128, S], BF16)
                g4 = A("g", [128, S], BF16)
                qe = A("qe", [128, S], BF16)
                ke = A("ke", [128, S], BF16)
                sq = A("sq", [128, S], BF16)
                yr = A("yr", [128, S], BF16)
                vch = A("vch", [64, 32, 128], BF16)
                keT = A("keT", [64, 32, 128], BF16)
                Vsb = A("Vsb", [128, 32, 128], F32)
                Sst = A("Sst", [128, 32, 128], F32)
                Sbf = A("Sbf", [128, 32, 128], BF16)
                scs = [A(f"scs{i}", [64, 512], BF16) for i in range(4)]
                psA = [ps.enter_context(nc.psum_tensor(f"p4_A{i}", [64, 512], F32)) for i in range(2)]
                psB = ps.enter_context(nc.psum_tensor("p4_B", [64, 8, 128], BF16))
                psC = [ps.enter_context(nc.psum_tensor(f"p4_C{i}", [128, 4, 128], F32)) for i in range(2)]
                psE = [ps.enter_context(nc.psum_tensor(f"p4_E{i}", [128, 512], F32)) for i in range(2)]
                psF = ps.enter_context(nc.psum_tensor("p4_F", [128, 512], F32))
                P.dma("sp", lambda e: e.dma_start(out=lbl[:], in_=I["lb_logits_t"]), "c0", writes=["lbl"])
                P.dma("sp", lambda e: e.dma_start(out=rng[:], in_=I["rec_norm_g"]), "c1", writes=["rng"])
                P.dma("sp", lambda e: e.dma_start(out=resetm[:], in_=C["resetm"]), "c2", writes=["resetm"])
                P.dma("sp", lambda e: e.dma_start(out=causal[:], in_=C["causal"]), "c3", writes=["causal"])
                P.op("dve", lambda e: e.tensor_tensor(out=lbT[:], in0=lbl[:, 0, :], in1=lbl[:, 1, :], op=ALU.subtract), reads=["lbl"], writes=["lbT"])
                P.op("act", lambda e: e.activation(out=lbT[:], in_=lbT[:], func=AF.Sigmoid), reads=["lbT"], writes=["lbT"])
                P.op("dve", lambda e: e.tensor_scalar(out=oml[:], in0=lbT[:], scalar1=-1.0, scalar2=1.0, op0=ALU.mult, op1=ALU.add), reads=["lbT"], writes=["oml"])
                P.op("dve", lambda e: e.tensor_scalar(out=noml[:], in0=oml[:], scalar1=-1.0, scalar2=None, op0=ALU.mult), reads=["oml"], writes=["noml"])
                P.op("pool", lambda e: e.memset(Sst[:, 0, :], 0.0), writes=["Sst0"])
                for h in range(nh4):
                    P.dma("sp", lambda e, h=h: e.dma_start(out=f4[:], in_=f_s[h]), "w0", writes=["f4"])
                    P.dma("sp", lambda e, h=h: e.dma_start(out=qr4[:], in_=qr_s[h]), "w1", writes=["qr4"])
                    P.dma("sp", lambda e, h=h: e.dma_start(out=g4[:], in_=g_s[h]), "o0", writes=["g4"])
                    P.dma("sp", lambda e, h=h: e.dma_start(out=vch[:], in_=i_s[:, 128 * h:128 * h + 128].rearrange("(c s) d -> s c d", s=64)), "o1", writes=["vch"])
                    hc = slice(h, h + 1)
                    P.op("act", lambda e: e.activation(out=f4[:], in_=f4[:], func=AF.Sigmoid), reads=["f4"], writes=["f4"])
                    P.op("act", lambda e, hc=hc: e.activation(out=lf[:], in_=f4[:], func=AF.Ln, scale=oml[:, hc], bias=lbT[:, hc]), reads=["f4", "oml", "lbT"], writes=["lf"])
                    P.op("dve", lambda e, hc=hc: e.tensor_scalar(out=kk[:], in0=f4[:], scalar1=noml[:, hc], scalar2=oml[:, hc], op0=ALU.mult, op1=ALU.add),
                         reads=["f4", "oml", "noml"], writes=["kk"])
                    P.op("dve", lambda e: e.tensor_tensor_scan(out=bcs[:], data0=resetm[:], data1=lf[:], initial=0.0, op0=ALU.mult, op1=ALU.add),
                         reads=["resetm", "lf"], writes=["bcs"])
                    P.op("act", lambda e: e.activation(out=eb[:], in_=bcs[:], func=AF.Exp), reads=["bcs"], writes=["eb"])
                    P.op("act", lambda e: e.activation(out=enb[:], in_=bcs[:], func=AF.Exp, scale=-1.0), reads=["bcs"], writes=["enb"])
                    P.op("dve", lambda e: e.tensor_tensor(out=qe[:], in0=qr4[:], in1=eb[:], op=ALU.mult), reads=["qr4", "eb"], writes=["qe"])
                    P.op("pool", lambda e: e.tensor_tensor(out=ke[:], in0=kk[:], in1=enb[:], op=ALU.mult), reads=["kk", "enb"], writes=["ke"])
                    for g in range(4):
                        a = g % 2
                        def mmA(e, g=g, a=a):
                            ins = None
                            for c8 in range(8):
                                c = 8 * g + c8
                                ins = e.matmul(psA[a][:, 64 * c8:64 * c8 + 64], lhsT=ke[:, 64 * c:64 * c + 64], rhs=qe[:, 64 * c:64 * c + 64], start=True, stop=True)
                            return ins
                        P.op("pe", mmA, reads=["ke", "qe"], writes=[("psA", a)])
                        P.op("dve", lambda e, g=g, a=a: e.tensor_tensor(out=scs[g][:], in0=psA[a][:], in1=causal[:], op=ALU.mult),
                             reads=[("psA", a), "causal"], writes=[("scs", g)])
                    for g in range(4):
                        def trB(e, g=g):
                            ins = None
                            for c8 in range(8):
                                c = 8 * g + c8
                                ins = e.transpose(out=psB[:, c8, :], in_=ke[:, 64 * c:64 * c + 64], identity=ident_bf[:])
                            return ins
                        P.op("pe", trB, reads=["ke"], writes=["psB"])
                        P.op("act", lambda e, g=g: e.activation(out=keT[:, 8 * g:8 * g + 8, :], in_=psB[:], func=AF.Copy), reads=["psB"], writes=["keT"])
                    for g in range(8):
                        a = g % 2
                        def mmC(e, g=g, a=a):
                            ins = None
                            for c4 in range(4):
                                c = 4 * g + c4
                                ins = e.matmul(psC[a][:, c4, :], lhsT=keT[:, c, :], rhs=vch[:, c, :], start=True, stop=True)
                            return ins
                        P.op("pe", mmC, reads=["keT", "vch"], writes=[("psC", a)])
                        def evC(e, g=g, a=a):
                            ins = None
                            for c4 in range(4):
                                c = 4 * g + c4
                                ins = e.activation(out=Vsb[:, c, :], in_=psC[a][:, c4, :], func=AF.Copy, scale=eb[:, 64 * c + 63:64 * c + 64])
                            return ins
                        P.op("act", evC, reads=[("psC", a), "eb"], writes=["Vsb"])
                    def scanD(e):
                        ins = None
                        for c in range(31):
                            ins = e.scalar_tensor_tensor(out=Sst[:, c + 1, :], in0=Sst[:, c, :], scalar=eb[:, 64 * c + 63:64 * c + 64], in1=Vsb[:, c, :], op0=ALU.mult, op1=ALU.add)
                        return ins
                    P.op("dve", scanD, reads=["Vsb", "eb", "Sst0"], writes=["Sst"])
                    P.op("pool", lambda e: e.tensor_copy(out=Sbf[:], in_=Sst[:]), reads=["Sst", "Sst0"], writes=["Sbf"])
                    for g in range(4):
                        a = g % 2
                        def mmE(e, g=g, a=a):
                            ins = None
                            for c8 in range(8):
                                c = 8 * g + c8
                                reg = psE[a][:, 64 * c8:64 * c8 + 64]
                                e.matmul(reg, lhsT=Sbf[:, c, :], rhs=qe[:, 64 * c:64 * c + 64], start=True, stop=False)
                                ins = e.matmul(reg, lhsT=vch[:, c, :], rhs=scs[g][:, 64 * c8:64 * c8 + 64], start=False, stop=True)
                            return ins
                        P.op("pe", mmE, reads=["Sbf", "qe", "vch", ("scs", g)], writes=[("psE", a)])
                        P.op("act", lambda e, g=g, a=a: e.activation(out=oT[:, 512 * g:512 * g + 512], in_=psE[a][:], func=AF.Copy), reads=[("psE", a)], writes=["oT"])
                    P.op("act", lambda e: e.activation(out=sq[:], in_=oT[:], func=AF.Square), reads=["oT"], writes=["sq"])
                    for g in range(4):
                        gs = slice(512 * g, 512 * g + 512)
                        P.op("pe", lambda e, gs=gs: e.matmul(psF[:], lhsT=ones_bf[:], rhs=sq[:, gs], start=True, stop=True), reads=["sq"], writes=["psF"])
                        P.op("act", lambda e, gs=gs: e.activation(out=bcs[:, gs], in_=psF[:], func=AF.Sqrt, scale=1.0 / 128, bias=EPS), reads=["psF"], writes=["bcs"])
                    P.op("dve", lambda e: e.reciprocal(out=bcs[:], in_=bcs[:]), reads=["bcs"], writes=["bcs"])
                    P.op("dve", lambda e: e.tensor_tensor(out=oT[:], in0=oT[:], in1=bcs[:], op=ALU.mult), reads=["oT", "bcs"], writes=["oT"])
                    P.op("dve", lambda e: e.scalar_tensor_tensor(out=yr[:], in0=oT[:], scalar=rng[:, 0:1], in1=g4[:], op0=ALU.mult, op1=ALU.mult),
                         reads=["oT", "rng", "g4"], writes=["yr"])
                    P.dma("sp", lambda e, h=h: e.dma_start(out=yr_s[h], in_=yr[:]), "y0", reads=["yr"], writes=[("yr_s", h)])
                P.barrier()

        def phase3():
            with ExitStack() as ps:
                mbf = ps.enter_context(nc.sbuf_tensor("p3_mbf", [128, 256], F32))
                maskb = ps.enter_context(nc.sbuf_tensor("p3_maskb", [128, 256], BF16))
                q3 = [ps.enter_context(nc.sbuf_tensor(f"p3_q{i}", [128, S], BF16)) for i in range(2)]
                k3 = [ps.enter_context(nc.sbuf_tensor(f"p3_k{i}", [128, S], BF16)) for i in range(2)]
                v3 = [[ps.enter_context(nc.sbuf_tensor(f"p3_v{i}_{r}", [128, 16, 128], BF16)) for r in range(3)] for i in range(2)]
                num = ps.enter_context(nc.sbuf_tensor("p3_num", [128, S], F32))
                den = ps.enter_context(nc.sbuf_tensor("p3_den", [128, S], F32))
                ya = [ps.enter_context(nc.sbuf_tensor(f"p3_ya{i}", [128, S], BF16)) for i in range(2)]
                pT = [ps.enter_context(nc.sbuf_tensor(f"p3_pT{i}", [128, 256], BF16)) for i in range(4)]
                pstb = [ps.enter_context(nc.psum_tensor(f"p3_pst{i}", [128, 512], F32)) for i in range(4)]
                pnum = [ps.enter_context(nc.psum_tensor(f"p3_pn{i}", [128, 512], F32)) for i in range(2)]
                pden = [ps.enter_context(nc.psum_tensor(f"p3_pd{i}", [128, 512], F32)) for i in range(2)]
                P.dma("sp", lambda e: e.dma_start(out=mbf[:], in_=C["maskb"]), "c0", writes=["mbf"])
                P.op("dve", lambda e: e.tensor_copy(out=maskb[:], in_=mbf[:]), reads=["mbf"], writes=["maskb"])
                SC = float(DH ** -0.5)
                RS = p3rs

                def pst(slot):
                    return pstb[slot][:, 0:256]

                def sview(t, r, j):
                    if r == 1:
                        return t[:]
                    return t[:].rearrange("p (n r) -> p r n", r=r)[:, j, :]

                def load3(h):
                    s = h % 2
                    P.dma("sp", lambda e, s=s, h=h: e.dma_start(out=q3[s][:], in_=qT_s[h]), f"w{s}", writes=[("q3", s)])
                    P.dma("sp", lambda e, s=s, h=h: e.dma_start(out=k3[s][:], in_=kT_s[h]), f"o{s}", writes=[("k3", s)])
                    for ri, r in enumerate(RS if p3v >= 0 else ()):
                        src = v_s[:, 128 * h:128 * h + 128].rearrange("(i n r) d -> n r i d", n=128, r=r)
                        dstv = v3[s][ri][:].rearrange("p (r i) d -> p r i d", r=r)
                        P.dma("sp", lambda e, src=src, dstv=dstv: e.dma_start(out=dstv, in_=src), f"v{s}{ri}", writes=[("v3", s, ri)])
                if nh3 > 0:
                    load3(0)
                for h in range(nh3):
                    s = h % 2
                    if h + 1 < nh3:
                        load3(h + 1)
                    P.op("pool", lambda e: e.memset(num[:], 0.0), writes=["num"])
                    P.op("pool", lambda e: e.memset(den[:], 0.0), writes=["den"])
                    tasks = []
                    for ri, r in enumerate(RS):
                        nb = 16 // r
                        for j in range(r):
                            for i in range(nb):
                                tasks.append((ri, r, j, i, nb))
                    if p3v <= 0:
                        tasks = []
                    T = len(tasks)

                    def emit_scores(t):
                        ri, r, j, i, nb = tasks[t]
                        slot = t % 4
                        nq = 256 if i < nb - 1 else 128
                        kv = sview(k3[s], r, j)
                        qv = sview(q3[s], r, j)

                        def mm(e, slot=slot, nq=nq, kv=kv, qv=qv, i=i):
                            e.matmul(pst(slot)[:, 0:nq], lhsT=kv[:, 128 * i:128 * i + 128], rhs=qv[:, 128 * i:128 * i + nq], start=True, stop=False)
                            return e.matmul(pst(slot)[:, 0:nq], lhsT=ident_bf[:], rhs=maskb[:, 0:nq], start=False, stop=True)
                        P.op("pe", mm, reads=[("q3", s), ("k3", s), "maskb"], writes=[("pst", slot)])
                        P.op("act", lambda e, slot=slot, nq=nq: e.activation(out=pT[slot][:, 0:nq], in_=pst(slot)[:, 0:nq], func=AF.Exp, scale=SC),
                             reads=[("pst", slot)], writes=[("pT", slot)])

                    def emit_pv(t):
                        ri, r, j, i, nb = tasks[t]
                        slot = t % 4
                        bank = (t // 4) % 2
                        qi = t % 4
                        vr = v3[s][ri]
                        bi = j * nb + i

                        def mm(e, slot=slot, bank=bank, qi=qi, vr=vr, bi=bi, i=i):
                            ins = None
                            for dst_, lget in ((pnum[bank], lambda b_: vr[:, b_, :]), (pden[bank], lambda b_: ones_bf[:])):
                                reg = dst_[:, 128 * qi:128 * qi + 128]
                                if i > 0:
                                    e.matmul(reg, lhsT=lget(bi - 1), rhs=pT[(slot - 1) % 4][:, 128:256], start=True, stop=False)
                                    ins = e.matmul(reg, lhsT=lget(bi), rhs=pT[slot][:, 0:128], start=False, stop=True)
                                else:
                                    ins = e.matmul(reg, lhsT=lget(bi), rhs=pT[slot][:, 0:128], start=True, stop=True)
                            return ins
                        rd = [("pT", slot), ("v3", s, ri)]
                        if i > 0:
                            rd.append(("pT", (slot - 1) % 4))
                        P.op("pe", mm, reads=rd, writes=[("pnum", bank), ("pden", bank)])
                        if qi == 3 and p3v >= 3:
                            g = t // 4
                            if r == 1:
                                tg = i // 4
                                nv = num[:, 512 * tg:512 * tg + 512]
                                dv = den[:, 512 * tg:512 * tg + 512]
                                pn, pd = pnum[bank][:], pden[bank][:]
                            elif r == 4:
                                nv = sview(num, 4, j)
                                dv = sview(den, 4, j)
                                pn, pd = pnum[bank][:], pden[bank][:]
                            else:
                                j0 = j - 3
                                nv = num[:].rearrange("p (n r) -> p r n", r=16)[:, j0:j0 + 4, :]
                                dv = den[:].rearrange("p (n r) -> p r n", r=16)[:, j0:j0 + 4, :]
                                pn = pnum[bank][:].rearrange("p (a n) -> p a n", a=4)
                                pd = pden[bank][:].rearrange("p (a n) -> p a n", a=4)
                            P.op("dve", lambda e, nv=nv, pn=pn: e.tensor_tensor(out=nv, in0=pn, in1=nv, op=ALU.add), reads=[("pnum", bank), "num"], writes=["num"])
                            P.op("dve", lambda e, dv=dv, pd=pd: e.tensor_tensor(out=dv, in0=pd, in1=dv, op=ALU.add), reads=[("pden", bank), "den"], writes=["den"])

                    for t in range(T + 1):
                        if t < T:
                            emit_scores(t)
                        if t >= 1 and p3v >= 2 and T > 0:
                            emit_pv(t - 1)
                    P.op("dve", lambda e: e.reciprocal(out=den[:], in_=den[:]), reads=["den"], writes=["den"])
                    P.op("dve", lambda e, s=s: e.tensor_tensor(out=ya[s][:], in0=num[:], in1=den[:], op=ALU.mult), reads=["num", "den"], writes=[("ya", s)])
                    P.dma("sp", lambda e, s=s, h=h: e.dma_start(out=ya_s[h], in_=ya[s][:]), f"y{s}", reads=[("ya", s)], writes=[("ya_s", h)])
                P.barrier()
        if 3 not in skip:
            phase3()


        def phase5a():
            with ExitStack() as ps:
                A = lambda n, shp, dt: ps.enter_context(nc.sbuf_tensor("p5a_" + n, shp, dt))
                yaT = A("yaT", [128, 16, 512], BF16)
                yrT = A("yrT", [128, 16, 512], BF16)
                wsta = [A(f"wsta{i}", [128, 16, 256], F32) for i in range(2)]
                wstr = [A(f"wstr{i}", [128, 16, 256], F32) for i in range(2)]
                wba = [A(f"wba{i}", [128, 16, 256], BF16) for i in range(2)]
                wbr = [A(f"wbr{i}", [128, 16, 256], BF16) for i in range(2)]
                gat = [A(f"gat{i}", [128, 512], BF16) for i in range(2)]
                grt = [A(f"grt{i}", [128, 512], BF16) for i in range(2)]
                t1 = A("t1", [128, 512], F32)
                t2 = A("t2", [128, 512], F32)
                mst = [A(f"mst{i}", [128, 512], BF16) for i in range(2)]
                pA = [ps.enter_context(nc.psum_tensor(f"p5a_A{i}", [128, 512], F32)) for i in range(2)]
                pR = [ps.enter_context(nc.psum_tensor(f"p5a_R{i}", [128, 512], F32)) for i in range(2)]
                wav = I["w_a"].rearrange("(kc p) n -> p kc n", p=128)
                wrv = I["w_r"].rearrange("(kc p) n -> p kc n", p=128)
                it = 0

                def loadw5(step):
                    ct_, s_ = step % 8, step % 2
                    P.dma("act", lambda e: e.dma_start(out=wsta[s_][:], in_=wav[:, :, 256 * ct_:256 * ct_ + 256]), f"w{s_}", writes=[("wsta", s_)])
                    P.dma("act", lambda e: e.dma_start(out=wstr[s_][:], in_=wrv[:, :, 256 * ct_:256 * ct_ + 256]), f"v{s_}0", writes=[("wstr", s_)])
                for tt in range(4):
                    tsl = slice(512 * tt, 512 * tt + 512)
                    P.dma("sp", lambda e, tsl=tsl: e.dma_start(out=yaT[:], in_=ya_s.rearrange("h p t -> p h t")[:, :, tsl]), "c0", writes=["yaT"])
                    P.dma("sp", lambda e, tsl=tsl: e.dma_start(out=yrT[:], in_=yr_s.rearrange("h p t -> p h t")[:, :, tsl]), "c1", writes=["yrT"])
                    for ct in range(8):
                        s = ct % 2
                        step = 8 * tt + ct
                        if step == 0:
                            loadw5(0)
                        if step + 1 < 32:
                            loadw5(step + 1)
                        P.op("pool", lambda e, s=s: e.tensor_copy(out=wba[s][:], in_=wsta[s][:]), reads=[("wsta", s)], writes=[("wba", s)])
                        P.op("pool", lambda e, s=s: e.tensor_copy(out=wbr[s][:], in_=wstr[s][:]), reads=[("wstr", s)], writes=[("wbr", s)])
                        for sub in range(2):
                            cc = 2 * ct + sub
                            q = it % 2; it += 1
                            P.dma("sp", lambda e, q=q, cc=cc, tsl=tsl: e.dma_start(out=gat[q][:], in_=ga_s[cc][:, tsl]), f"o{q}", writes=[("gat", q)])
                            P.dma("sp", lambda e, q=q, cc=cc, tsl=tsl: e.dma_start(out=grt[q][:], in_=gr_s[cc][:, tsl]), f"o{q}f", writes=[("grt", q)])
                            def mmA(e, s=s, sub=sub, q=q):
                                ins = None
                                for kc in range(16):
                                    ins = e.matmul(pA[q][:], lhsT=wba[s][:, kc, 128 * sub:128 * sub + 128], rhs=yaT[:, kc, :], start=(kc == 0), stop=(kc == 15))
                                return ins
                            def mmR(e, s=s, sub=sub, q=q):
                                ins = None
                                for kc in range(16):
                                    ins = e.matmul(pR[q][:], lhsT=wbr[s][:, kc, 128 * sub:128 * sub + 128], rhs=yrT[:, kc, :], start=(kc == 0), stop=(kc == 15))
                                return ins
                            P.op("pe", mmA, reads=[("wba", s), "yaT"], writes=[("pA", q)])
                            P.op("pe", mmR, reads=[("wbr", s), "yrT"], writes=[("pR", q)])
                            P.op("dve", lambda e, q=q: e.tensor_tensor(out=t1[:], in0=pA[q][:], in1=gat[q][:], op=ALU.mult), reads=[("pA", q), ("gat", q)], writes=["t1"])
                            P.op("dve", lambda e, q=q: e.tensor_tensor(out=t2[:], in0=pR[q][:], in1=grt[q][:], op=ALU.mult), reads=[("pR", q), ("grt", q)], writes=["t2"])
                            P.op("pool", lambda e, q=q: e.tensor_tensor(out=mst[q][:], in0=t1[:], in1=t2[:], op=ALU.add), reads=["t1", "t2"], writes=[("mst", q)])
                            P.dma("sp", lambda e, q=q, cc=cc, tsl=tsl: e.dma_start(out=m_s[cc][:, tsl], in_=mst[q][:]), f"y{q}", reads=[("mst", q)], writes=[("m_s", cc, tt)])
                P.barrier()

        def phase5b():
            with ExitStack() as ps:
                A = lambda n, shp, dt: ps.enter_context(nc.sbuf_tensor("p5b_" + n, shp, dt))
                wo = A("wo", [128, 16, D], BF16)
                wst5 = [A(f"wst{i}", [128, 16, 256], F32) for i in range(2)]
                gtm = A("gtm", [128, D], F32)
                G2 = A("G2", [128, D], F32)
                shf = A("shf", [128, D], F32)
                mT = [A(f"mT{i}", [128, 16, 128], BF16) for i in range(2)]
                xt5 = [A(f"xt{i}", [128, D], F32) for i in range(2)]
                ht = A("ht", [128, D], F32)
                tf5 = A("tf", [128, D], F32)
                u2f = A("u2f", [128, D], F32)
                u2b = [A(f"u2b{i}", [128, D], BF16) for i in range(2)]
                junk5 = A("junk", [128, D], BF16)
                st5 = [A(f"st{i}", [128, 4], F32) for i in range(2)]
                u2T = A("u2T", [128, 16, 128], F32)
                wr = A("wr", [128, 16, 36], F32)
                brt = A("brt", [128, 36], F32)
                lgall = A("lgall", [128, 16, 36], F32)
                pM = [ps.enter_context(nc.psum_tensor(f"p5b_M{i}", [128, 512], F32)) for i in range(4)]
                pT5 = ps.enter_context(nc.psum_tensor("p5b_T", [128, 8, 128], F32))
                pL = ps.enter_context(nc.psum_tensor("p5b_L", [128, 36], F32))
                wov = I["w_o"].rearrange("(kc p) n -> p kc n", p=128)
                for ct in range(8):
                    s = ct % 2
                    P.dma("sp", lambda e, s=s, ct=ct: e.dma_start(out=wst5[s][:], in_=wov[:, :, 256 * ct:256 * ct + 256]), f"w{s}", writes=[("wst5", s)])
                    P.op("pool", lambda e, s=s, ct=ct: e.tensor_copy(out=wo[:, :, 256 * ct:256 * ct + 256], in_=wst5[s][:]), reads=[("wst5", s)], writes=["wo"])
                P.dma("sp", lambda e: e.dma_start(out=gtm[:], in_=modbc[:, 2 * D:3 * D]), "c0", writes=["gtm"])
                P.dma("sp", lambda e: e.dma_start(out=shf[:], in_=modbc[:, 3 * D:4 * D]), "c1", writes=["shf"])
                P.dma("sp", lambda e: e.dma_start(out=tf5[:], in_=modbc[:, 4 * D:5 * D]), "c2", writes=["tf5"])
                P.dma("sp", lambda e: e.dma_start(out=G2[:], in_=I["ffn_norm_g"].partition_broadcast(128)), "c3", writes=["G2"])
                P.op("dve", lambda e: e.scalar_tensor_tensor(out=G2[:], in0=tf5[:], scalar=1.0, in1=G2[:], op0=ALU.add, op1=ALU.mult), reads=["tf5", "G2"], writes=["G2"])
                P.dma("sp", lambda e: e.dma_start(out=wr[:], in_=I["w_router"].rearrange("(kc p) n -> p kc n", p=128)), "o0", writes=["wr"])
                P.dma("sp", lambda e: e.dma_start(out=brt[:], in_=I["b_router"].partition_broadcast(128)), "o1", writes=["brt"])
                def load5b(i):
                    s = i % 2
                    tsl = slice(128 * i, 128 * i + 128)
                    P.dma("sp", lambda e, s=s, tsl=tsl: e.dma_start(out=mT[s][:], in_=m_s.rearrange("h p t -> p h t")[:, :, tsl]), f"v{s}0", writes=[("mT", s)])
                    P.dma("sp", lambda e, s=s, tsl=tsl: e.dma_start(out=xt5[s][:], in_=I["x"][tsl, :]), f"v{s}1", writes=[("xt5", s)])
                load5b(0)
                for i in range(16):
                    s = i % 2
                    tsl = slice(128 * i, 128 * i + 128)
                    if i + 1 < 16:
                        load5b(i + 1)
                    for cc in range(4):
                        csl = slice(512 * cc, 512 * cc + 512)
                        def mmM(e, s=s, cc=cc, csl=csl):
                            ins = None
                            for kc in range(16):
                                ins = e.matmul(pM[cc][:], lhsT=mT[s][:, kc, :], rhs=wo[:, kc, csl], start=(kc == 0), stop=(kc == 15))
                            return ins
                        P.op("pe", mmM, reads=[("mT", s), "wo"], writes=[("pM", cc)])
                        P.op("dve", lambda e, cc=cc, csl=csl: e.tensor_tensor(out=tf5[:, csl], in0=pM[cc][:], in1=gtm[:, csl], op=ALU.mult), reads=[("pM", cc), "gtm"], writes=[("tf5", cc)])
                    P.op("pool", lambda e, s=s: e.tensor_tensor(out=ht[:], in0=tf5[:], in1=xt5[s][:], op=ALU.add), reads=[("tf5", 0), ("tf5", 1), ("tf5", 2), ("tf5", 3), ("xt5", s)], writes=["ht"])
                    P.dma("sp", lambda e, tsl=tsl: e.dma_start(out=h_s[tsl, :], in_=ht[:]), "y0", reads=["ht"], writes=[("h_s", i)])
                    P.op("act", lambda e, s=s: e.activation(out=junk5[:], in_=ht[:], func=AF.Square, accum_out=st5[s][:, 0:1]), reads=["ht"], writes=["junk5", ("st5", s)])
                    P.op("act", lambda e, s=s: e.activation(out=st5[s][:, 1:2], in_=st5[s][:, 0:1], func=AF.Sqrt, scale=1.0 / D, bias=EPS), reads=[("st5", s)], writes=[("st5", s)])
                    P.op("dve", lambda e, s=s: e.reciprocal(out=st5[s][:, 2:3], in_=st5[s][:, 1:2]), reads=[("st5", s)], writes=[("st5", s)])
                    P.op("dve", lambda e, s=s: e.scalar_tensor_tensor(out=u2f[:], in0=ht[:], scalar=st5[s][:, 2:3], in1=G2[:], op0=ALU.mult, op1=ALU.mult),
                         reads=["ht", ("st5", s), "G2"], writes=["u2f"])
                    P.op("pool", lambda e: e.tensor_tensor(out=u2f[:], in0=u2f[:], in1=shf[:], op=ALU.add), reads=["u2f", "shf"], writes=["u2f"])
                    P.op("act", lambda e, s=s: e.activation(out=u2b[s][:], in_=u2f[:], func=AF.Copy), reads=["u2f"], writes=[("u2b", s)])
                    P.dma("sp", lambda e, s=s, tsl=tsl: e.dma_start(out=u2_s[tsl, :], in_=u2b[s][:]), f"y1{s}", reads=[("u2b", s)], writes=[("u2_s", i)])
                    for hf in range(2):
                        def tr5(e, hf=hf):
                            ins = None
                            for k8 in range(8):
                                kc = 8 * hf + k8
                                ins = e.transpose(out=pT5[:, k8, :], in_=u2f[:, 128 * kc:128 * kc + 128], identity=ident_f[:])
                            return ins
                        P.op("pe", tr5, reads=["u2f"], writes=["pT5"])
                        P.op("act", lambda e, hf=hf: e.activation(out=u2T[:, 8 * hf:8 * hf + 8, :], in_=pT5[:], func=AF.Copy), reads=["pT5"], writes=[("u2T", hf)])
                    def mmL(e):
                        ins = None
                        for kc in range(16):
                            ins = e.matmul(pL[:], lhsT=u2T[:, kc, :], rhs=wr[:, kc, :], start=(kc == 0), stop=(kc == 15))
                        return ins
                    P.op("pe", mmL, reads=[("u2T", 0), ("u2T", 1), "wr"], writes=["pL"])
                    P.op("dve", lambda e, i=i: e.tensor_tensor(out=lgall[:, i, :], in0=pL[:], in1=brt[:], op=ALU.add), reads=["pL", "brt"], writes=["lgall"])
                P.dma("sp", lambda e: e.dma_start(out=lg_s, in_=lgall[:]), "y2", reads=["lgall"], writes=["lg_s"])
                P.barrier()

        def phase6():
            with ExitStack() as ps:
                A = lambda n, shp, dt: ps.enter_context(nc.sbuf_tensor("p6_" + n, shp, dt))
                lg = A("lg", [128, 16, 36], F32)
                iota_e = A("iota_e", [128, 32], F32)
                ltf = A("ltf", [128, 128], F32)
                ltb = A("ltb", [128, 128], BF16)
                thrb = A("thrb", [128, 64], F32)
                iotap = A("iotap", [128, 1], F32)
                eidx = A("eidx", [128, 32], F32)
                E = A("E", [128, 32, 32], BF16)
                gm = A("gm", [128, 1], F32)
                sh = A("sh", [128, 4], F32)
                ex = A("ex", [128, 4], F32)
                se = A("se", [128, 1], F32)
                pg = A("pg", [128, 1], F32)
                pen = A("pen", [128, 4], F32)
                lem = A("lem", [128, 32], F32)
                mx8 = A("mx8", [128, 8], F32)
                ix8 = A("ix8", [128, 8], U32)
                dl = A("dl", [128, 4], F32)
                base = A("base", [128, 32, 32], F32)
                tmp = A("tmp", [128, 32, 32], F32)
                cnt = A("cnt", [128, 32], F32)
                ci = A("ci", [128, 32], I32)
                padded = A("padded", [128, 32], F32)
                pend = A("pend", [128, 32], F32)
                pstart = A("pstart", [128, 32], F32)
                dest = A("dest", [128, 32], F32)
                acc = A("acc", [128, 64], F32)
                of = A("of", [128, 64], F32)
                over = A("over", [128, 64], F32)
                pmk = A("pmk", [128, 1], F32)
                u2t = [A(f"u2t{i}", [128, D], BF16) for i in range(2)]
                pPre = ps.enter_context(nc.psum_tensor("p6_pre", [128, 1024], F32))
                pTot = ps.enter_context(nc.psum_tensor("p6_tot", [128, 1024], F32))
                P.dma("sp", lambda e: e.dma_start(out=lg[:], in_=lg_s), "c0", writes=["lg"])
                P.dma("sp", lambda e: e.dma_start(out=iota_e[:], in_=C["iota_e"]), "c1", writes=["iota_e"])
                P.dma("sp", lambda e: e.dma_start(out=ltf[:], in_=C["ltri"]), "c2", writes=["ltf"])
                P.dma("sp", lambda e: e.dma_start(out=thrb[:], in_=C["thrb"]), "c3", writes=["thrb"])
                P.dma("sp", lambda e: e.dma_start(out=iotap[:], in_=C["iota_p"]), "o0", writes=["iotap"])
                CH = ["p6chain"]

                def X(eng, fn):
                    P.op(eng, fn, reads=CH, writes=CH)
                P.op("dve", lambda e: e.tensor_copy(out=ltb[:], in_=ltf[:]), reads=["ltf", "lg", "iota_e", "thrb", "iotap"], writes=CH)
                for t in range(16):
                    lgt = lg[:, t, 0:4]
                    let = lg[:, t, 4:36]
                    X("dve", lambda e, lgt=lgt: e.tensor_reduce(out=gm[:], in_=lgt, axis=AX.X, op=ALU.max))
                    X("dve", lambda e, lgt=lgt: e.tensor_scalar(out=sh[:], in0=lgt, scalar1=gm[:, 0:1], scalar2=None, op0=ALU.subtract))
                    X("dve", lambda e: e.tensor_scalar(out=pen[:], in0=sh[:], scalar1=0.0, scalar2=None, op0=ALU.is_equal))
                    X("dve", lambda e: e.tensor_scalar(out=pen[:], in0=pen[:], scalar1=-1.0, scalar2=1e30, op0=ALU.add, op1=ALU.mult))
                    for g in range(4):
                        X("dve", lambda e, g=g, let=let: e.tensor_scalar(out=lem[:, 8 * g:8 * g + 8], in0=let[:, 8 * g:8 * g + 8], scalar1=pen[:, g:g + 1], scalar2=None, op0=ALU.add))
                    X("act", lambda e: e.activation(out=ex[:], in_=sh[:], func=AF.Exp, accum_out=se[:, 0:1]))
                    X("dve", lambda e: e.max(out=mx8[:], in_=lem[:]))
                    X("dve", lambda e: e.max_index(out=ix8[:], in_max=mx8[:], in_values=lem[:]))
                    X("dve", lambda e, t=t: e.tensor_copy(out=eidx[:, 2 * t:2 * t + 2], in_=ix8[:, 0:2]))
                    X("dve", lambda e: e.tensor_tensor(out=dl[:, 0:1], in0=mx8[:, 1:2], in1=mx8[:, 0:1], op=ALU.subtract))
                    X("act", lambda e: e.activation(out=dl[:, 1:2], in_=dl[:, 0:1], func=AF.Exp))
                    X("dve", lambda e: e.reciprocal(out=pg[:], in_=se[:]))
                    X("dve", lambda e: e.tensor_scalar(out=dl[:, 2:3], in0=dl[:, 1:2], scalar1=1.0, scalar2=None, op0=ALU.add))
                    X("dve", lambda e: e.reciprocal(out=dl[:, 3:4], in_=dl[:, 2:3]))
                    X("dve", lambda e, t=t: e.tensor_tensor(out=wts[:, 2 * t:2 * t + 1], in0=pg[:], in1=dl[:, 3:4], op=ALU.mult))
                    X("dve", lambda e, t=t: e.tensor_tensor(out=wts[:, 2 * t + 1:2 * t + 2], in0=wts[:, 2 * t:2 * t + 1], in1=dl[:, 1:2], op=ALU.mult))
                for g in range(32):
                    X("dve", lambda e, g=g: e.tensor_scalar(out=E[:, g, :], in0=iota_e[:], scalar1=eidx[:, g:g + 1], scalar2=None, op0=ALU.is_equal))
                Ef = E[:].rearrange("p g e -> p (g e)")
                X("pe", lambda e: e.matmul(pPre[:, 0:512], lhsT=ltb[:], rhs=Ef[:, 0:512], start=True, stop=True))
                X("pe", lambda e: e.matmul(pPre[:, 512:1024], lhsT=ltb[:], rhs=Ef[:, 512:1024], start=True, stop=True))
                X("pe", lambda e: e.matmul(pTot[:, 0:512], lhsT=ones_bf[:], rhs=Ef[:, 0:512], start=True, stop=True))
                X("pe", lambda e: e.matmul(pTot[:, 512:1024], lhsT=ones_bf[:], rhs=Ef[:, 512:1024], start=True, stop=True))
                pTv = pTot[:].rearrange("p (g e) -> p g e", e=32)
                pPv = pPre[:].rearrange("p (g e) -> p g e", e=32)
                X("dve", lambda e: e.memset(base[:, 0, :], 0.0))
                for g in range(1, 32):
                    X("dve", lambda e, g=g: e.tensor_tensor(out=base[:, g, :], in0=pTv[:, g - 1, :], in1=base[:, g - 1, :], op=ALU.add))
                X("dve", lambda e: e.tensor_tensor(out=cnt[:], in0=pTv[:, 31, :], in1=base[:, 31, :], op=ALU.add))
                X("dve", lambda e: e.memset(padded[:], 0.0))
                for jj in range(32):
                    X("dve", lambda e, jj=jj: e.scalar_tensor_tensor(out=padded[:], in0=cnt[:], scalar=128.0 * jj, in1=padded[:], op0=ALU.is_gt, op1=ALU.add))
                X("dve", lambda e: e.tensor_scalar(out=padded[:], in0=padded[:], scalar1=128.0, scalar2=None, op0=ALU.mult))
                X("dve", lambda e: e.tensor_tensor_scan(out=pend[:], data0=ones_f[:, 0:32], data1=padded[:], initial=0.0, op0=ALU.mult, op1=ALU.add))
                X("dve", lambda e: e.tensor_tensor(out=pstart[:], in0=pend[:], in1=padded[:], op=ALU.subtract))
                X("dve", lambda e: e.tensor_tensor(out=tmp[:, 0:16, :], in0=pPv[:, 0:16, :], in1=base[:, 0:16, :], op=ALU.add))
                X("dve", lambda e: e.tensor_tensor(out=tmp[:, 16:32, :], in0=pPv[:, 16:32, :], in1=base[:, 16:32, :], op=ALU.add))
                for g in range(32):
                    X("dve", lambda e, g=g: e.tensor_tensor(out=tmp[:, g, :], in0=tmp[:, g, :], in1=pstart[:], op=ALU.add))
                X("dve", lambda e: e.tensor_tensor(out=tmp[:], in0=tmp[:], in1=E[:], op=ALU.mult))
                X("dve", lambda e: e.tensor_reduce(out=dest[:], in_=tmp[:], axis=AX.X, op=ALU.add))
                X("dve", lambda e: e.tensor_copy(out=desti[:], in_=dest[:]))
                X("dve", lambda e: e.memset(acc[:], 0.0))
                for ee in range(32):
                    X("dve", lambda e, ee=ee: e.scalar_tensor_tensor(out=acc[:], in0=thrb[:], scalar=pend[:, ee:ee + 1], in1=acc[:], op0=ALU.is_ge, op1=ALU.add))
                X("dve", lambda e: e.tensor_scalar(out=pmk[:], in0=iotap[:], scalar1=1.0, scalar2=None, op0=ALU.min))
                X("dve", lambda e: e.tensor_scalar(out=over[:], in0=acc[:], scalar1=32.0, scalar2=None, op0=ALU.is_ge))
                X("dve", lambda e: e.tensor_scalar(out=over[:], in0=over[:], scalar1=pmk[:, 0:1], scalar2=100000.0, op0=ALU.mult, op1=ALU.mult))
                X("dve", lambda e: e.tensor_scalar(out=acc[:], in0=acc[:], scalar1=31.0, scalar2=None, op0=ALU.min))
                X("dve", lambda e: e.tensor_scalar(out=of[:], in0=acc[:], scalar1=2048.0, scalar2=iotap[:, 0:1], op0=ALU.mult, op1=ALU.add))
                X("dve", lambda e: e.tensor_copy(out=offs_gu[:], in_=of[:]))
                X("dve", lambda e: e.tensor_scalar(out=of[:], in0=acc[:], scalar1=1024.0, scalar2=iotap[:, 0:1], op0=ALU.mult, op1=ALU.add))
                P.op("dve", lambda e: e.tensor_copy(out=offs_d[:], in_=of[:]), reads=CH, writes=CH + ["desti", "offs"])
                if debug:
                    rt = A("rt", [128, 16, 8], F32)
                    X("dve", lambda e: e.memset(rt[:], 0.0))
                    X("dve", lambda e: e.tensor_copy(out=rt[:, :, 0:2], in_=eidx[:].rearrange("p (t k) -> p t k", k=2)))
                    X("dve", lambda e: e.tensor_copy(out=rt[:, :, 2:4], in_=wts[:].rearrange("p (t k) -> p t k", k=2)))
                    X("dve", lambda e: e.tensor_copy(out=rt[:, :, 4:6], in_=dest[:].rearrange("p (t k) -> p t k", k=2)))
                    P.dma("sp", lambda e: e.dma_start(out=rt_s, in_=rt[:]), "o1", reads=CH, writes=["rt_s"])
                for t in range(16):
                    s = t % 2
                    P.dma("sp", lambda e, s=s, t=t: e.dma_start(out=u2t[s][:], in_=u2_s[128 * t:128 * t + 128, :]), f"w{s}", writes=[("u2t", s)])
                    for k in range(2):
                        g = 2 * t + k
                        P.dma("pool", lambda e, s=s, g=g: e.indirect_dma_start(out=xslot, out_offset=bass.IndirectOffsetOnAxis(ap=desti[:, g:g + 1], axis=0),
                                                                              in_=u2t[s][:], in_offset=None),
                              f"v{s}{k}", reads=[("u2t", s), "desti"], writes=[("xslot", g)])
                P.barrier()

        def phase7():
            with ExitStack() as ps:
                A = lambda n, shp, dt: ps.enter_context(nc.sbuf_tensor("p7_" + n, shp, dt))
                xt7 = [A(f"xt{i}", [128, D], BF16) for i in range(2)]
                xT7 = [A(f"xT{i}", [128, 16, 128], BF16) for i in range(2)]
                guf = [A(f"guf{i}", [128, 1024], F32) for i in range(10)]
                gub = [A(f"gub{i}", [128, 1024], BF16) for i in range(10)]
                dnf = [A(f"dnf{i}", [128, D], F32) for i in range(4)]
                dnb = [A(f"dnb{i}", [128, D], BF16) for i in range(4)]
                sg7 = A("sg", [128, 1024], F32)
                hd7 = A("hd", [128, 1024], BF16)
                hT7 = A("hT", [128, 8, 128], BF16)
                ys7 = [A(f"ys{i}", [128, D], F32) for i in range(2)]
                pGU = ps.enter_context(nc.psum_tensor("p7_GU", [128, 4, 512], F32))
                pTr = [ps.enter_context(nc.psum_tensor(f"p7_Tr{i}", [128, 8, 128], BF16)) for i in range(2)]
                gi = 0
                di = 0
                tri = 0
                def loadx7(b):
                    s = b % 2
                    P.dma("sp", lambda e, s=s, b=b: e.dma_start(out=xt7[s][:], in_=xslot[128 * b:128 * b + 128, :]), f"w{s}", writes=[("xt7", s)])
                if nb7 > 0:
                    loadx7(0)
                for b in range(nb7):
                    s = b % 2
                    if b + 1 < nb7:
                        loadx7(b + 1)
                    for hf in range(2):
                        w = tri % 2; tri += 1
                        def trx(e, s=s, hf=hf, w=w):
                            ins = None
                            for k8 in range(8):
                                kc = 8 * hf + k8
                                ins = e.transpose(out=pTr[w][:, k8, :], in_=xt7[s][:, 128 * kc:128 * kc + 128], identity=ident_bf[:])
                            return ins
                        P.op("pe", trx, reads=[("xt7", s)], writes=[("pTr", w)])
                        P.op("act", lambda e, s=s, hf=hf, w=w: e.activation(out=xT7[s][:, 8 * hf:8 * hf + 8, :], in_=pTr[w][:], func=AF.Copy),
                             reads=[("pTr", w)], writes=[("xT7", s, hf)])
                    for kc in range(16):
                        for which in range(2):
                            r = gi % 10; gi += 1
                            src = I["w_gate"] if which == 0 else I["w_up"]
                            P.dma("pool", lambda e, r=r, b=b, kc=kc, src=src: e.indirect_dma_start(
                                out=guf[r][:], out_offset=None, in_=src, in_offset=bass.IndirectOffsetOnAxis(ap=offs_gu[:, b:b + 1], axis=0),
                                element_offset=kc * 128 * 1024), f"g{r}", reads=["offs"], writes=[("guf", r)])
                            ceng = "act" if (gi % 2 == 0) else "dve"
                            if ceng == "act":
                                P.op("act", lambda e, r=r: e.activation(out=gub[r][:], in_=guf[r][:], func=AF.Copy), reads=[("guf", r)], writes=[("gub", r)])
                            else:
                                P.op("dve", lambda e, r=r: e.tensor_copy(out=gub[r][:], in_=guf[r][:]), reads=[("guf", r)], writes=[("gub", r)])
                            def mmg(e, s=s, r=r, kc=kc, which=which):
                                e.matmul(pGU[:, 2 * which, :], lhsT=xT7[s][:, kc, :], rhs=gub[r][:, 0:512], start=(kc == 0), stop=(kc == 15))
                                return e.matmul(pGU[:, 2 * which + 1, :], lhsT=xT7[s][:, kc, :], rhs=gub[r][:, 512:1024], start=(kc == 0), stop=(kc == 15))
                            P.op("pe", mmg, reads=[("gub", r), ("xT7", s, 0), ("xT7", s, 1)], writes=[("pgu", 2 * which), ("pgu", 2 * which + 1)])
                    P.op("act", lambda e: e.activation(out=sg7[:], in_=pGU[:, 0:2, :].rearrange("p a n -> p (a n)"), func=AF.Silu), reads=[("pgu", 0), ("pgu", 1)], writes=["sg7"])
                    P.op("dve", lambda e: e.tensor_tensor(out=hd7[:], in0=pGU[:, 2:4, :].rearrange("p a n -> p (a n)"), in1=sg7[:], op=ALU.mult),
                         reads=[("pgu", 2), ("pgu", 3), "sg7"], writes=["hd7"])
                    w = tri % 2; tri += 1
                    def trh(e, w=w):
                        ins = None
                        for k8 in range(8):
                            ins = e.transpose(out=pTr[w][:, k8, :], in_=hd7[:, 128 * k8:128 * k8 + 128], identity=ident_bf[:])
                        return ins
                    P.op("pe", trh, reads=["hd7"], writes=[("pTr", w)])
                    P.op("act", lambda e, w=w: e.activation(out=hT7[:], in_=pTr[w][:], func=AF.Copy), reads=[("pTr", w)], writes=["hT7"])
                    for k2 in range(8):
                        r = di % 4; di += 1
                        P.dma("pool", lambda e, r=r, b=b, k2=k2: e.indirect_dma_start(
                            out=dnf[r][:], out_offset=None, in_=I["w_down"], in_offset=bass.IndirectOffsetOnAxis(ap=offs_d[:, b:b + 1], axis=0),
                            element_offset=k2 * 128 * D), f"d{r}", reads=["offs"], writes=[("dnf", r)])
                        P.op("act", lambda e, r=r: e.activation(out=dnb[r][:, 0:1024], in_=dnf[r][:, 0:1024], func=AF.Copy), reads=[("dnf", r)], writes=[("dnb", r, 0)])
                        P.op("dve", lambda e, r=r: e.tensor_copy(out=dnb[r][:, 1024:2048], in_=dnf[r][:, 1024:2048]), reads=[("dnf", r)], writes=[("dnb", r, 1)])
                        def mmd(e, r=r, k2=k2):
                            ins = None
                            for cc in range(4):
                                ins = e.matmul(pGU[:, cc, :], lhsT=hT7[:, k2, :], rhs=dnb[r][:, 512 * cc:512 * cc + 512], start=(k2 == 0), stop=(k2 == 7))
                            return ins
                        P.op("pe", mmd, reads=[("dnb", r, 0), ("dnb", r, 1), "hT7"], writes=[("pgu", 0), ("pgu", 1), ("pgu", 2), ("pgu", 3)])
                    P.op("act", lambda e, s=s: e.activation(out=ys7[s][:, 0:1024], in_=pGU[:, 0:2, :].rearrange("p a n -> p (a n)"), func=AF.Copy),
                         reads=[("pgu", 0), ("pgu", 1)], writes=[("ys7", s, 0)])
                    P.op("dve", lambda e, s=s: e.tensor_copy(out=ys7[s][:, 1024:2048], in_=pGU[:, 2:4, :].rearrange("p a n -> p (a n)")),
                         reads=[("pgu", 2), ("pgu", 3)], writes=[("ys7", s, 1)])
                    P.dma("sp", lambda e, s=s, b=b: e.dma_start(out=yslot[128 * b:128 * b + 128, :], in_=ys7[s][:]), f"o{s}",
                          reads=[("ys7", s, 0), ("ys7", s, 1)], writes=[("yslot", b)])
                P.barrier()

        def phase8(mode):
            full = (mode == 'full')
            with ExitStack() as ps:
                Gf = ps.enter_context(nc.sbuf_tensor("p8_G", [128, D], F32))
                gtf = ps.enter_context(nc.sbuf_tensor("p8_gtf", [128, D], F32))
                xt8 = [ps.enter_context(nc.sbuf_tensor(f"p8_x{i}", [128, D], F32)) for i in range(2)]
                y08 = [ps.enter_context(nc.sbuf_tensor(f"p8_y0{i}", [128, D], F32)) for i in range(2)]
                y18 = [ps.enter_context(nc.sbuf_tensor(f"p8_y1{i}", [128, D], F32)) for i in range(2)]
                ot8 = [ps.enter_context(nc.sbuf_tensor(f"p8_o{i}", [128, D], F32)) for i in range(2)]
                junk8 = ps.enter_context(nc.sbuf_tensor("p8_junk", [128, D], BF16))
                st8 = [ps.enter_context(nc.sbuf_tensor(f"p8_st{i}", [128, 4], F32)) for i in range(2)]
                P.dma("sp", lambda e: e.dma_start(out=Gf[:], in_=I["final_norm_g"].partition_broadcast(128)), "c0", writes=["Gf"])
                if full:
                    P.dma("sp", lambda e: e.dma_start(out=gtf[:], in_=modbc[:, 5 * D:6 * D]), "c1", writes=["gtf"])
                hsrc = I["x"] if mode == 'x' else h_s
                def loadx8(i):
                    s = i % 2
                    P.dma("sp", lambda e, s=s, i=i: e.dma_start(out=xt8[s][:], in_=hsrc[128 * i:128 * i + 128, :]), f"w{s}", writes=[("xt8", s)])
                loadx8(0)
                for i in range(16):
                    s = i % 2
                    if i + 1 < 16:
                        loadx8(i + 1)
                    if full:
                        P.dma("pool", lambda e, s=s, i=i: e.indirect_dma_start(out=y08[s][:], out_offset=None, in_=yslot,
                                                                              in_offset=bass.IndirectOffsetOnAxis(ap=desti[:, 2 * i:2 * i + 1], axis=0)),
                              f"g{s}", writes=[("y08", s)])
                        P.dma("pool", lambda e, s=s, i=i: e.indirect_dma_start(out=y18[s][:], out_offset=None, in_=yslot,
                                                                              in_offset=bass.IndirectOffsetOnAxis(ap=desti[:, 2 * i + 1:2 * i + 2], axis=0)),
                              f"d{s}", writes=[("y18", s)])
                        P.op("dve", lambda e, s=s, i=i: e.tensor_scalar(out=y08[s][:], in0=y08[s][:], scalar1=wts[:, 2 * i:2 * i + 1], scalar2=None, op0=ALU.mult),
                             reads=[("y08", s)], writes=[("y08", s)])
                        P.op("dve", lambda e, s=s, i=i: e.scalar_tensor_tensor(out=y18[s][:], in0=y18[s][:], scalar=wts[:, 2 * i + 1:2 * i + 2], in1=y08[s][:], op0=ALU.mult, op1=ALU.add),
                             reads=[("y08", s), ("y18", s)], writes=[("y18", s)])
                        P.op("pool", lambda e, s=s: e.tensor_tensor(out=y18[s][:], in0=y18[s][:], in1=gtf[:], op=ALU.mult), reads=[("y18", s), "gtf"], writes=[("y18", s)])
                        P.op("pool", lambda e, s=s: e.tensor_tensor(out=xt8[s][:], in0=xt8[s][:], in1=y18[s][:], op=ALU.add), reads=[("y18", s), ("xt8", s)], writes=[("xt8", s)])
                    P.op("act", lambda e, s=s: e.activation(out=junk8[:], in_=xt8[s][:], func=AF.Square, accum_out=st8[s][:, 0:1]),
                         reads=[("xt8", s)], writes=["junk8", ("st8", s)])
                    P.op("act", lambda e, s=s: e.activation(out=st8[s][:, 1:2], in_=st8[s][:, 0:1], func=AF.Sqrt, scale=1.0 / D, bias=EPS),
                         reads=[("st8", s)], writes=[("st8", s)])
                    P.op("dve", lambda e, s=s: e.reciprocal(out=st8[s][:, 2:3], in_=st8[s][:, 1:2]), reads=[("st8", s)], writes=[("st8", s)])
                    P.op("dve", lambda e, s=s: e.scalar_tensor_tensor(out=ot8[s][:], in0=xt8[s][:], scalar=st8[s][:, 2:3], in1=Gf[:], op0=ALU.mult, op1=ALU.mult),
                         reads=[("xt8", s), ("st8", s), "Gf"], writes=[("ot8", s)])
                    P.dma("sp", lambda e, s=s, i=i: e.dma_start(out=out[128 * i:128 * i + 128, :], in_=ot8[s][:]), f"o{s}", reads=[("ot8", s)], writes=[("out", i)])
                P.barrier()
        if stop_after >= 4 and 4 not in skip:
            phase4()
        if stop_after >= 5 and 5 not in skip:
            phase5a()
        if stop_after >= 5 and 55 not in skip:
            phase5b()
        if stop_after >= 6 and 6 not in skip:
            phase6()
        if stop_after >= 7 and 7 not in skip:
            phase7()
        phase8('full' if (stop_after >= 7 and 7 not in skip) else ('h' if (stop_after >= 5 and 55 not in skip) else 'x'))
        P.barrier(); P.emit(); return nc
        if debug and stop_after <= 1:
            uT_d = scratch('s_uT', [128, 16, S], BF16)
            P.dma('sp', lambda e: e.dma_start(out=uT_d, in_=uT[:]), 'c0', reads=['uT'], writes=['uT_d'])
            P.barrier()
        P.emit()
    return nc


def _prep(inp, b):
    m = {}
    m["x"] = np.ascontiguousarray(inp["x"][b])
    m["c_t"] = np.ascontiguousarray(np.asarray(inp["c"][b]).reshape(16, 128).T)
    m["pos"] = np.ascontiguousarray(inp["positions"][b])
    m["ada_w"] = inp["ada_w"][0]
    m["ada_b"] = inp["ada_b"][0]
    m["mix_norm_g"] = inp["mix_norm_g"][0]
    m["w_in"] = inp["w_in"][0]
    m["w_a"] = inp["w_attn_branch"][0]
    m["w_r"] = inp["w_rec_branch"][0]
    m["w_o"] = inp["w_mix_out"][0]
    m["rec_norm_g"] = np.ascontiguousarray(np.asarray(inp["rec_norm_g"][0]).reshape(128, 1))
    m["lb_logits_t"] = np.ascontiguousarray(np.asarray(inp["rec_lb_logits"]).reshape(2, 16, 128).transpose(2, 0, 1))
    m["ffn_norm_g"] = inp["ffn_norm_g"][0]
    m["w_router"] = np.ascontiguousarray(np.concatenate([inp["router_group_w"][0], inp["router_expert_w"][0]], axis=1))
    m["b_router"] = np.ascontiguousarray(np.concatenate([inp["router_group_b"][0], inp["router_expert_b"][0]]))
    m["w_gate"] = np.asarray(inp["expert_w_gate"][0]).reshape(32 * D, 1024)
    m["w_up"] = np.asarray(inp["expert_w_up"][0]).reshape(32 * D, 1024)
    m["w_down"] = np.asarray(inp["expert_w_down"][0]).reshape(32 * 1024, D)
    m["final_norm_g"] = inp["final_norm_g"]
    return m


STOP_AFTER = 7


def kernel(**inputs):
    inp = {k: np.asarray(v) for k, v in inputs.items()}
    consts = {"c_" + k: v for k, v in host_consts().items()}
    nc = build(stop_after=STOP_AFTER)
    in_maps = []
    for b in range(8):
        m = _prep(inp, b)
        m.update(consts)
        in_maps.append(m)
    res = run_bass_kernel_spmd(nc, in_maps, core_ids=list(range(8)))
    return np.stack([np.asarray(r["out"], dtype=np.float32) for r in res.results], axis=0)
```

```python
import numpy as np
from contextlib import ExitStack
import concourse.bass as bass
import concourse.mybir as mybir
from concourse.bass_utils import run_bass_kernel_spmd

F32 = mybir.dt.float32
BF16 = mybir.dt.bfloat16
I32 = mybir.dt.int32
U32 = mybir.dt.uint32
AF = mybir.ActivationFunctionType
ALU = mybir.AluOpType
AX = mybir.AxisListType

D = 2048
S = 2048
NH = 16
DH = 128
INW = 18432
EPS = 1e-6
NEG = -30000.0
ROPE_THETA = 500000.0


class Prog:
    COMPUTE = ("pe", "act", "dve", "pool")
    ENG = ("pe", "act", "dve", "pool", "sp")

    def __init__(self, nc, es):
        self.nc, self.es = nc, es
        self.streams = {e: [] for e in self.ENG}
        self.esem = {e: es.enter_context(nc.semaphore(f"sem_{e}")) for e in self.COMPUTE}
        self.ecount = {e: 0 for e in self.COMPUTE}
        self.known = {e: {} for e in self.ENG}
        self.state = {}
        self.dsem = {}

    def _need(self, eng, ev):
        if ev is None:
            return
        name, sem, val, owner = ev
        if owner == eng and eng == "pe":
            return
        if self.known[eng].get(name, 0) >= val:
            return
        self.known[eng][name] = val
        self.streams[eng].append(("wait", sem, val))

    def _deps(self, eng, reads, writes):
        for k in reads:
            st = self.state.get(k)
            if st:
                self._need(eng, st["w"])
        for k in writes:
            st = self.state.get(k)
            if st:
                self._need(eng, st["w"])
                for ev in st["r"]:
                    self._need(eng, ev)

    def _commit(self, ev, reads, writes):
        for k in reads:
            st = self.state.setdefault(k, {"w": None, "r": []})
            st["r"].append(ev)
            if len(st["r"]) > 24:
                st["r"] = st["r"][-24:] if False else st["r"]
        for k in writes:
            self.state[k] = {"w": ev, "r": []}

    def op(self, eng, fn, reads=(), writes=()):
        self._deps(eng, reads, writes)
        self.ecount[eng] += 1
        ev = (f"e_{eng}", self.esem[eng], self.ecount[eng], eng)
        self.streams[eng].append(("op", fn, self.esem[eng], 1))
        self._commit(ev, reads, writes)
        return ev

    def dma(self, q, fn, semname, reads=(), writes=()):
        self._deps(q, reads, writes)
        if semname not in self.dsem:
            self.dsem[semname] = [self.es.enter_context(self.nc.semaphore(f"d_{semname}")), 0]
        d = self.dsem[semname]
        d[1] += 16
        ev = (f"d_{semname}", d[0], d[1], "dma")
        self.streams[q].append(("op", fn, d[0], 16))
        self._commit(ev, reads, writes)
        return ev

    def barrier(self):
        evs = []
        for e in self.COMPUTE:
            if self.ecount[e]:
                evs.append((f"e_{e}", self.esem[e], self.ecount[e], e))
        for name, (sem, cnt) in self.dsem.items():
            if cnt:
                evs.append((f"d_{name}", sem, cnt, "dma"))
        for eng in self.ENG:
            for ev in evs:
                self._need(eng, ev)
        self.state = {}

    def emit(self):
        nc = self.nc
        engmap = {"pe": "tensor", "act": "scalar", "dve": "vector", "pool": "gpsimd", "sp": "sync"}
        with nc.Block() as block:
            for e in self.ENG:
                stream = self.streams[e]
                if not stream:
                    continue

                def body(eng, stream=stream):
                    for item in stream:
                        if item[0] == "wait":
                            eng.wait_ge(item[1], item[2])
                        else:
                            ins = item[1](eng)
                            ins.then_inc(item[2], item[3])

                getattr(block, engmap[e])(body)


def host_consts():
    c = {}
    c["ident_bf"] = np.eye(128, dtype=np.float32)
    c["ones_f"] = np.ones((128, 128), np.float32)
    inv = ROPE_THETA ** (-np.arange(0, 32, 2, dtype=np.float32) / np.float32(32))
    c["invf"] = np.concatenate([inv, inv]).astype(np.float32).reshape(32, 1)
    c["sinsign"] = np.concatenate([-np.ones(16), np.ones(16)]).astype(np.float32).reshape(32, 1)
    pm = np.eye(128, dtype=np.float32)
    pm[:32, :32] = 0.0
    for m in range(32):
        pm[(m + 16) % 32, m] = 1.0
    c["perm"] = pm
    kk = np.arange(128)[:, None]
    qq = np.arange(128)[None, :]
    mb = np.zeros((128, 256), np.float32)
    mb[:, :128] = np.where(kk <= qq, 0.0, NEG)
    mb[:, 128:] = np.where(kk >= qq, 0.0, NEG)
    c["maskb"] = mb
    rm = np.ones((128, S), np.float32)
    rm[:, ::64] = 0.0
    c["resetm"] = rm
    ss = np.arange(64)[:, None]
    tt = np.arange(64)[None, :]
    c["causal"] = np.tile((ss <= tt).astype(np.float32), (1, 8))
    c["ltri"] = (np.arange(128)[:, None] < np.arange(128)[None, :]).astype(np.float32)
    c["iota_p"] = np.arange(128, dtype=np.float32).reshape(128, 1)
    c["thrb"] = np.broadcast_to((np.arange(64, dtype=np.float32) * 128.0)[None, :], (128, 64)).copy()
    c["iota_e"] = np.broadcast_to(np.arange(32, dtype=np.float32)[None, :], (128, 32)).copy()
    return c


CONST_SHAPES = {k: v.shape for k, v in host_consts().items()}

IN_SHAPES = {
    "x": ([S, D], F32), "c_t": ([128, 16], F32), "pos": ([S], I32),
    "ada_w": ([D, 6 * D], F32), "ada_b": ([6 * D], F32), "mix_norm_g": ([D], F32),
    "w_in": ([D, INW], F32), "w_a": ([D, D], F32), "w_r": ([D, D], F32), "w_o": ([D, D], F32),
    "rec_norm_g": ([128, 1], F32), "lb_logits_t": ([128, 2, 16], F32), "ffn_norm_g": ([D], F32),
    "w_router": ([D, 36], F32), "b_router": ([36], F32),
    "w_gate": ([32 * D, 1024], F32), "w_up": ([32 * D, 1024], F32), "w_down": ([32 * 1024, D], F32),
    "final_norm_g": ([D], F32),
}


def build(stop_after=99, debug=False, rot=3, trig=True, p2_ct=72, skip=(), nh3=NH, nh4=NH, nb7=64, p3v=3, p3rs=(1, 4, 16)):
    nc = bass.Bass("TRN2", target_bir_lowering=False)
    I = {}
    for k, (shp, dt) in IN_SHAPES.items():
        I[k] = nc.dram_tensor(k, list(shp), dt, kind="ExternalInput").ap()
    C = {}
    for k, shp in CONST_SHAPES.items():
        C[k] = nc.dram_tensor("c_" + k, list(shp), F32, kind="ExternalInput").ap()
    out = nc.dram_tensor("out", [S, D], F32, kind="ExternalOutput").ap()
    skind = "ExternalOutput" if debug else "Internal"

    def scratch(name, shape, dt):
        return nc.dram_tensor(name, list(shape), dt, kind=skind).ap()

    modbc = scratch("s_modbc", [128, 6 * D], F32)
    qT_s = scratch("s_qT", [NH, 128, S], BF16)
    kT_s = scratch("s_kT", [NH, 128, S], BF16)
    v_s = scratch("s_v", [S, D], BF16)
    qr_s = scratch("s_qr", [NH, 128, S], BF16)
    f_s = scratch("s_f", [NH, 128, S], F32)
    i_s = scratch("s_i", [S, D], BF16)
    g_s = scratch("s_g", [NH, 128, S], BF16)
    ga_s = scratch("s_ga", [NH, 128, S], BF16)
    gr_s = scratch("s_gr", [NH, 128, S], BF16)
    ya_s = scratch("s_ya", [NH, 128, S], BF16)
    yr_s = scratch("s_yr", [NH, 128, S], BF16)
    m_s = scratch("s_m", [NH, 128, S], BF16)
    h_s = scratch("s_h", [S, D], F32)
    u2_s = scratch("s_u2", [S, D], BF16)
    lg_s = scratch("s_lg", [128, 16, 36], F32)
    xslot = scratch("s_xslot", [64 * 128, D], BF16)
    yslot = scratch("s_yslot", [64 * 128, D], F32)
    rt_s = scratch("s_rt", [128, 16, 8], F32)

    with ExitStack() as es:
        P = Prog(nc, es)
        ident_bf = es.enter_context(nc.sbuf_tensor("ident_bf", [128, 128], BF16))
        ident_f = es.enter_context(nc.sbuf_tensor("ident_f", [128, 128], F32))
        ones_f = es.enter_context(nc.sbuf_tensor("ones_f", [128, 128], F32))
        ones_bf = es.enter_context(nc.sbuf_tensor("ones_bf", [128, 128], BF16))
        P.dma("sp", lambda e: e.dma_start(out=ident_f[:], in_=C["ident_bf"]), "c0", writes=["ident_f"])
        P.dma("sp", lambda e: e.dma_start(out=ones_f[:], in_=C["ones_f"]), "c1", writes=["ones_f"])
        P.op("dve", lambda e: e.tensor_copy(out=ident_bf[:], in_=ident_f[:]), reads=["ident_f"], writes=["ident_bf"])
        P.op("dve", lambda e: e.tensor_copy(out=ones_bf[:], in_=ones_f[:]), reads=["ones_f"], writes=["ones_bf"])
        desti = es.enter_context(nc.sbuf_tensor("desti", [128, 32], I32))
        wts = es.enter_context(nc.sbuf_tensor("wts", [128, 32], F32))
        offs_gu = es.enter_context(nc.sbuf_tensor("offs_gu", [128, 64], I32))
        offs_d = es.enter_context(nc.sbuf_tensor("offs_d", [128, 64], I32))
        P.barrier()

        with ExitStack() as ps:
            c_sb = ps.enter_context(nc.sbuf_tensor("c_sb", [128, 16], F32))
            L = ps.enter_context(nc.sbuf_tensor("adaL", [128, 16, 128], BF16))
            modsb = ps.enter_context(nc.sbuf_tensor("modsb", [128, 6 * D], F32))
            wst = [ps.enter_context(nc.sbuf_tensor(f"a_wst{i}", [128, 16, 512], F32)) for i in range(2)]
            wbf = [ps.enter_context(nc.sbuf_tensor(f"a_wbf{i}", [128, 16, 512], BF16)) for i in range(2)]
            pp = [ps.enter_context(nc.psum_tensor(f"a_ps{i}", [128, 512], F32)) for i in range(2)]
            P.dma("sp", lambda e: e.dma_start(out=c_sb[:], in_=I["c_t"]), "c0", writes=["c_sb"])
            P.dma("sp", lambda e: e.dma_start(out=modsb[:], in_=I["ada_b"].partition_broadcast(128)), "c1", writes=["modsb"])
            P.op("act", lambda e: e.activation(out=c_sb[:], in_=c_sb[:], func=AF.Silu), reads=["c_sb"], writes=["c_sb"])

            def mkL(e):
                ins = None
                for j in range(16):
                    ins = e.tensor_scalar(out=L[:, j, :], in0=ones_f[:], scalar1=c_sb[:, j:j + 1], scalar2=None, op0=ALU.mult)
                return ins
            P.op("dve", mkL, reads=["c_sb"], writes=["L"])
            awv = I["ada_w"].rearrange("(kc p) n -> p kc n", p=128)
            for ct in range(0 if 0 in skip else 24):
                s = ct % 2
                P.dma("sp", lambda e, s=s, ct=ct: e.dma_start(out=wst[s][:], in_=awv[:, :, 512 * ct:512 * ct + 512]),
                      f"w{s}", writes=[("wst", s)])
                P.op("pool", lambda e, s=s: e.tensor_copy(out=wbf[s][:], in_=wst[s][:]), reads=[("wst", s)], writes=[("wbf", s)])

                def mm(e, s=s):
                    ins = None
                    for kc in range(16):
                        ins = e.matmul(pp[s][:], lhsT=L[:, kc, :], rhs=wbf[s][:, kc, :], start=(kc == 0), stop=(kc == 15))
                    return ins
                P.op("pe", mm, reads=["L", ("wbf", s)], writes=[("pp", s)])
                P.op("dve", lambda e, s=s, ct=ct: e.tensor_tensor(out=modsb[:, 512 * ct:512 * ct + 512], in0=pp[s][:],
                                                                 in1=modsb[:, 512 * ct:512 * ct + 512], op=ALU.add),
                     reads=[("pp", s), "modsb"], writes=["modsb"])
            P.dma("sp", lambda e: e.dma_start(out=modbc, in_=modsb[:]), "c0", reads=["modsb"], writes=["modbc"])
            P.barrier()
        if stop_after <= 0:
            P.barrier(); P.emit(); return nc

        ps12 = es.enter_context(ExitStack())
        uT = ps12.enter_context(nc.sbuf_tensor("uT", [128, 16, S], BF16))
        with ExitStack() as ps:
            G1 = ps.enter_context(nc.sbuf_tensor("G1", [128, D], F32))
            shm = ps.enter_context(nc.sbuf_tensor("shm", [128, D], F32))
            tmpg = ps.enter_context(nc.sbuf_tensor("tmpg", [128, D], F32))
            xt = [ps.enter_context(nc.sbuf_tensor(f"xt{i}", [128, D], F32)) for i in range(2)]
            junk = ps.enter_context(nc.sbuf_tensor("junk", [128, D], BF16))
            tf = ps.enter_context(nc.sbuf_tensor("tf", [128, D], F32))
            ub = [ps.enter_context(nc.sbuf_tensor(f"ub{i}", [128, D], BF16)) for i in range(2)]
            st = [ps.enter_context(nc.sbuf_tensor(f"st{i}", [128, 4], F32)) for i in range(2)]
            ptr = [ps.enter_context(nc.psum_tensor(f"ptr{i}", [128, 16, 128], BF16)) for i in range(2)]
            P.dma("sp", lambda e: e.dma_start(out=shm[:], in_=modbc[:, 0:D]), "c0", writes=["shm"])
            P.dma("sp", lambda e: e.dma_start(out=tmpg[:], in_=modbc[:, D:2 * D]), "c1", writes=["tmpg"])
            P.dma("sp", lambda e: e.dma_start(out=G1[:], in_=I["mix_norm_g"].partition_broadcast(128)), "c2", writes=["G1"])
            P.op("dve", lambda e: e.scalar_tensor_tensor(out=G1[:], in0=tmpg[:], scalar=1.0, in1=G1[:], op0=ALU.add, op1=ALU.mult),
                 reads=["tmpg", "G1"], writes=["G1"])
            for i in range(0 if 1 in skip else 16):
                s = i % 2
                P.dma("sp", lambda e, s=s, i=i: e.dma_start(out=xt[s][:], in_=I["x"][128 * i:128 * i + 128, :]), f"w{s}", writes=[("xt", s)])
                P.op("act", lambda e, s=s: e.activation(out=junk[:], in_=xt[s][:], func=AF.Square, accum_out=st[s][:, 0:1]),
                     reads=[("xt", s)], writes=["junk", ("st", s)])
                P.op("act", lambda e, s=s: e.activation(out=st[s][:, 1:2], in_=st[s][:, 0:1], func=AF.Sqrt, scale=1.0 / D, bias=EPS),
                     reads=[("st", s)], writes=[("st", s)])
                P.op("dve", lambda e, s=s: e.reciprocal(out=st[s][:, 2:3], in_=st[s][:, 1:2]), reads=[("st", s)], writes=[("st", s)])
                P.op("dve", lambda e, s=s: e.scalar_tensor_tensor(out=tf[:], in0=xt[s][:], scalar=st[s][:, 2:3], in1=G1[:], op0=ALU.mult, op1=ALU.mult),
                     reads=[("xt", s), ("st", s), "G1"], writes=["tf"])
                P.op("pool", lambda e, s=s: e.tensor_tensor(out=ub[s][:], in0=tf[:], in1=shm[:], op=ALU.add),
                     reads=["tf", "shm"], writes=[("ub", s)])

                def tr(e, s=s):
                    ins = None
                    for kc in range(16):
                        ins = e.transpose(out=ptr[s][:, kc, :], in_=ub[s][:, 128 * kc:128 * kc + 128], identity=ident_bf[:])
                    return ins
                P.op("pe", tr, reads=[("ub", s), "ident_bf"], writes=[("ptr", s)])
                P.op("act", lambda e, s=s, i=i: e.activation(out=uT[:, :, 128 * i:128 * i + 128], in_=ptr[s][:], func=AF.Copy),
                     reads=[("ptr", s)], writes=["uT"])
            P.barrier()

        if stop_after <= 1:
            P.barrier(); P.emit(); return nc
        PI = float(np.pi)
        with ExitStack() as ps:
            cosF = ps.enter_context(nc.sbuf_tensor("cosF", [128, S], F32))
            sinF = ps.enter_context(nc.sbuf_tensor("sinF", [128, S], F32))
            permb = ps.enter_context(nc.sbuf_tensor("permb", [128, 128], BF16))
            permf = ps.enter_context(nc.sbuf_tensor("permf", [128, 128], F32))
            with ExitStack() as ts:
                cosT = ts.enter_context(nc.sbuf_tensor("cosT", [32, S], F32))
                sinT = ts.enter_context(nc.sbuf_tensor("sinT", [32, S], F32))
                posi = ts.enter_context(nc.sbuf_tensor("posi", [32, S], I32))
                ang = ts.enter_context(nc.sbuf_tensor("ang", [32, S], F32))
                kq = ts.enter_context(nc.sbuf_tensor("kq", [32, S], F32))
                ki = ts.enter_context(nc.sbuf_tensor("ki", [32, S], I32))
                rr = ts.enter_context(nc.sbuf_tensor("rr", [32, S], F32))
                mm_ = ts.enter_context(nc.sbuf_tensor("mm_", [32, S], F32))
                sm = ts.enter_context(nc.sbuf_tensor("sm", [32, 40], F32))
                P.dma("sp", lambda e: e.dma_start(out=posi[:], in_=I["pos"].partition_broadcast(32)), "c0", writes=["posi"])
                P.dma("sp", lambda e: e.dma_start(out=sm[:, 0:1], in_=C["invf"]), "c1", writes=["sm0"])
                P.dma("sp", lambda e: e.dma_start(out=sm[:, 1:2], in_=C["sinsign"]), "c2", writes=["sm1"])
                P.dma("sp", lambda e: e.dma_start(out=permf[:], in_=C["perm"]), "c3", writes=["permf"])
                P.op("dve", lambda e: e.tensor_copy(out=permb[:], in_=permf[:]), reads=["permf"], writes=["permb"])
                P.op("dve", lambda e: e.tensor_copy(out=ang[:], in_=posi[:]), reads=["posi"], writes=["ang"])
                P.op("dve", lambda e: e.tensor_scalar(out=ang[:], in0=ang[:], scalar1=sm[:, 0:1], scalar2=None, op0=ALU.mult),
                     reads=["ang", "sm0"], writes=["ang"])

                def reduce_to(dst, shift, tag):
                    P.op("dve", lambda e: e.tensor_scalar(out=kq[:], in0=ang[:], scalar1=shift, scalar2=1.0 / (2 * PI), op0=ALU.add, op1=ALU.mult),
                         reads=["ang"], writes=["kq"])
                    P.op("dve", lambda e: e.tensor_copy(out=ki[:], in_=kq[:]), reads=["kq"], writes=["ki"])
                    P.op("dve", lambda e: e.tensor_copy(out=kq[:], in_=ki[:]), reads=["ki"], writes=["kq"])
                    P.op("dve", lambda e: e.scalar_tensor_tensor(out=rr[:], in0=kq[:], scalar=-2 * PI, in1=ang[:], op0=ALU.mult, op1=ALU.add),
                         reads=["kq", "ang"], writes=["rr"])
                    if shift != 0.0:
                        P.op("dve", lambda e: e.tensor_scalar(out=rr[:], in0=rr[:], scalar1=shift, scalar2=None, op0=ALU.add),
                             reads=["rr"], writes=["rr"])
                    P.op("dve", lambda e: e.tensor_scalar(out=mm_[:], in0=rr[:], scalar1=PI, scalar2=-2 * PI, op0=ALU.is_gt, op1=ALU.mult),
                         reads=["rr"], writes=["mm_"])
                    P.op("dve", lambda e: e.tensor_tensor(out=rr[:], in0=rr[:], in1=mm_[:], op=ALU.add), reads=["rr", "mm_"], writes=["rr"])
                    P.op("dve", lambda e: e.tensor_scalar(out=mm_[:], in0=rr[:], scalar1=-PI, scalar2=2 * PI, op0=ALU.is_lt, op1=ALU.mult),
                         reads=["rr"], writes=["mm_"])
                    P.op("dve", lambda e: e.tensor_tensor(out=rr[:], in0=rr[:], in1=mm_[:], op=ALU.add), reads=["rr", "mm_"], writes=["rr"])
                    P.op("dve", lambda e: e.tensor_scalar(out=rr[:], in0=rr[:], scalar1=3.1415925, scalar2=-3.1415925, op0=ALU.min, op1=ALU.max),
                         reads=["rr"], writes=["rr"])
                    P.op("act", lambda e: e.activation(out=dst[:], in_=rr[:], func=AF.Sin), reads=["rr"], writes=[tag])
                reduce_to(sinT, 0.0, "sinT")
                P.op("dve", lambda e: e.tensor_scalar(out=sinT[:], in0=sinT[:], scalar1=sm[:, 1:2], scalar2=None, op0=ALU.mult),
                     reads=["sinT", "sm1"], writes=["sinT"])
                reduce_to(cosT, PI / 2, "cosT")
                P.op("pool", lambda e: e.memset(cosF[:], 1.0), writes=["cosF"])
                P.op("pool", lambda e: e.memset(sinF[:], 0.0), writes=["sinF"])
                P.op("dve", lambda e: e.tensor_copy(out=cosF[0:32, :], in_=cosT[:]), reads=["cosT", "cosF"], writes=["cosF"])
                P.op("dve", lambda e: e.tensor_copy(out=sinF[0:32, :], in_=sinT[:]), reads=["sinT", "sinF"], writes=["sinF"])
                P.barrier()

            wst2 = [ps.enter_context(nc.sbuf_tensor(f"p2_wst{i}", [128, 16, 256], F32)) for i in range(2)]
            wbf2 = [ps.enter_context(nc.sbuf_tensor(f"p2_wbf{i}", [128, 16, 256], BF16)) for i in range(2)]
            stg = [ps.enter_context(nc.sbuf_tensor(f"p2_stg{i}", [128, S], BF16)) for i in range(2)]
            stgf = [ps.enter_context(nc.sbuf_tensor(f"p2_stgf{i}", [128, S], F32)) for i in range(2)]
            stgt = [ps.enter_context(nc.sbuf_tensor(f"p2_stgt{i}", [128, 256], BF16)) for i in range(2)]
            rt1 = ps.enter_context(nc.sbuf_tensor("p2_rt1", [128, 512], F32))
            rt2 = ps.enter_context(nc.sbuf_tensor("p2_rt2", [128, 512], F32))
            xb = ps.enter_context(nc.sbuf_tensor("p2_xb", [128, 512], BF16))
            xf = ps.enter_context(nc.sbuf_tensor("p2_xf", [128, 512], F32))
            pf = ps.enter_context(nc.sbuf_tensor("p2_pf", [128, 512], F32))
            pp2 = [ps.enter_context(nc.psum_tensor(f"p2_ps{i}", [128, 512], F32)) for i in range(4)]
            psw = [ps.enter_context(nc.psum_tensor(f"p2_psw{i}", [128, 512], F32)) for i in range(2)]
            wv = I["w_in"].rearrange("(kc p) n -> p kc n", p=128)
            SEC = [("rot", qT_s), ("rot", kT_s), ("tok", v_s), ("silu", qr_s), ("f32", f_s), ("tok", i_s), ("silu", g_s), ("sig", ga_s), ("sig", gr_s)]
            ppi = 0
            swi = 0
            sti = 0
            stt = 0
            for ct in range(0 if 2 in skip else p2_ct):
                s = ct % 2
                sec = ct // 8
                kind, dst = SEC[sec]
                if ct == 0:
                    P.dma("act", lambda e: e.dma_start(out=wst2[0][:], in_=wv[:, :, 0:256]), "w0", writes=[("wst", 0)])
                if ct + 1 < p2_ct:
                    P.dma("act", lambda e, s1=(ct + 1) % 2, c1=ct + 1: e.dma_start(out=wst2[s1][:], in_=wv[:, :, 256 * c1:256 * c1 + 256]), f"w{(ct + 1) % 2}", writes=[("wst", (ct + 1) % 2)])
                P.op("pool", lambda e, s=s: e.tensor_copy(out=wbf2[s][:], in_=wst2[s][:]), reads=[("wst", s)], writes=[("wbf", s)])
                if kind == "tok":
                    c0 = 256 * (ct % 8)
                    for tt in range(16):
                        b = ppi % 4; ppi += 1
                        def mm(e, s=s, tt=tt, b=b):
                            ins = None
                            for kc in range(16):
                                ins = e.matmul(pp2[b][:, 0:256], lhsT=uT[:, kc, 128 * tt:128 * tt + 128], rhs=wbf2[s][:, kc, :], start=(kc == 0), stop=(kc == 15))
                            return ins
                        P.op("pe", mm, reads=[("wbf", s)], writes=[("pp", b)])
                        q = stt % 2; stt += 1
                        P.op("act", lambda e, b=b, q=q: e.activation(out=stgt[q][:], in_=pp2[b][:, 0:256], func=AF.Copy), reads=[("pp", b)], writes=[("stgt", q)])
                        P.dma("sp", lambda e, q=q, tt=tt, c0=c0, dst=dst: e.dma_start(out=dst[128 * tt:128 * tt + 128, c0:c0 + 256], in_=stgt[q][:]),
                              f"o{q}", reads=[("stgt", q)], writes=[("dtok", ct, tt)])
                    continue
                for sub in range(2):
                    h = (ct % 8) * 2 + sub
                    q = sti % 2; sti += 1
                    sbuf_dst = stgf[q] if kind == "f32" else stg[q]
                    skey = ("stgf", q) if kind == "f32" else ("stg", q)
                    for tc in range(4):
                        b = ppi % 4; ppi += 1
                        def mm(e, s=s, sub=sub, tc=tc, b=b):
                            ins = None
                            for kc in range(16):
                                ins = e.matmul(pp2[b][:], lhsT=wbf2[s][:, kc, 128 * sub:128 * sub + 128], rhs=uT[:, kc, 512 * tc:512 * tc + 512], start=(kc == 0), stop=(kc == 15))
                            return ins
                        P.op("pe", mm, reads=[("wbf", s)], writes=[("pp", b)])
                        func = {"rot": AF.Copy, "silu": AF.Silu, "f32": AF.Copy, "sig": AF.Sigmoid}[kind]
                        cs = slice(512 * tc, 512 * tc + 512)
                        if kind == "rot" and rot:
                            w = swi % 2; swi += 1
                            P.op("act", lambda e, b=b: e.activation(out=xb[:], in_=pp2[b][:], func=AF.Copy), reads=[("pp", b)], writes=["xb"])
                            P.op("act", lambda e, b=b: e.activation(out=xf[:], in_=pp2[b][:], func=AF.Copy), reads=[("pp", b)], writes=["xf"])
                            P.op("pe", lambda e, w=w: e.matmul(psw[w][:], lhsT=permb[:], rhs=xb[:], start=True, stop=True),
                                 reads=["xb"], writes=[("psw", w)])
                            P.op("act", lambda e, w=w: e.activation(out=pf[:], in_=psw[w][:], func=AF.Copy), reads=[("psw", w)], writes=["pf"])
                            P.op("dve", lambda e, cs=cs: e.tensor_tensor(out=rt1[:], in0=xf[:], in1=cosF[:, cs], op=ALU.mult),
                                 reads=["xf"], writes=["rt1"])
                            P.op("dve", lambda e, cs=cs: e.tensor_tensor(out=rt2[:], in0=pf[:], in1=sinF[:, cs], op=ALU.mult),
                                 reads=["pf"], writes=["rt2"])
                            P.op("pool", lambda e, cs=cs, sbuf_dst=sbuf_dst: e.tensor_tensor(out=sbuf_dst[:, cs], in0=rt1[:], in1=rt2[:], op=ALU.add),
                                 reads=["rt1", "rt2"], writes=[skey])
                        else:
                            P.op("act", lambda e, b=b, cs=cs, sbuf_dst=sbuf_dst, func=func: e.activation(out=sbuf_dst[:, cs], in_=pp2[b][:], func=func),
                                 reads=[("pp", b)], writes=[skey])
                    P.dma("sp", lambda e, h=h, dst=dst, sbuf_dst=sbuf_dst: e.dma_start(out=dst[h], in_=sbuf_dst[:]), f"o{q}f" if kind == "f32" else f"o{q}",
                          reads=[skey], writes=[("dfm", sec, h)])
            P.barrier()

        ps12.close()
        if stop_after <= 2:
            P.barrier(); P.emit(); return nc

        def phase4():
            with ExitStack() as ps:
                A = lambda n, shp, dt: ps.enter_context(nc.sbuf_tensor("p4_" + n, shp, dt))
                lbl = A("lbl", [128, 2, 16], F32)
                lbT = A("lbT", [128, 16], F32)
                oml = A("oml", [128, 16], F32)
                noml = A("noml", [128, 16], F32)
                rng = A("rng", [128, 1], F32)
                resetm = A("resetm", [128, S], F32)
                causal = A("causal", [64, 512], F32)
                f4 = A("f", [128, S], F32)
                lf = A("lf", [128, S], F32)
                kk = A("kk", [128, S], F32)
                bcs = A("bcs", [128, S], F32)
                eb = A("eb", [128, S], F32)
                enb = A("enb", [128, S], F32)
                oT = A("oT", [128, S], F32)
                qr4 = A("qr", [128, S], BF16)
                g4 = A("g", [128, S], BF16)
                qe = A("qe", [128, S], BF16)
                ke = A("ke", [128, S], BF16)
                sq = A("sq", [128, S], BF16)
                yr = A("yr", [128, S], BF16)
                vch = A("vch", [64, 32, 128], BF16)
                keT = A("keT", [64, 32, 128], BF16)
                Vsb = A("Vsb", [128, 32, 128], F32)
                Sst = A("Sst", [128, 32, 128], F32)
                Sbf = A("Sbf", [128, 32, 128], BF16)
                scs = [A(f"scs{i}", [64, 512], BF16) for i in range(4)]
                psA = [ps.enter_context(nc.psum_tensor(f"p4_A{i}", [64, 512], F32)) for i in range(2)]
                psB = ps.enter_context(nc.psum_tensor("p4_B", [64, 8, 128], BF16))
                psC = [ps.enter_context(nc.psum_tensor(f"p4_C{i}", [128, 4, 128], F32)) for i in range(2)]
                psE = [ps.enter_context(nc.psum_tensor(f"p4_E{i}", [128, 512], F32)) for i in range(2)]
                psF = ps.enter_context(nc.psum_tensor("p4_F", [128, 512], F32))
                P.dma("sp", lambda e: e.dma_start(out=lbl[:], in_=I["lb_logits_t"]), "c0", writes=["lbl"])
                P.dma("sp", lambda e: e.dma_start(out=rng[:], in_=I["rec_norm_g"]), "c1", writes=["rng"])
                P.dma("sp", lambda e: e.dma_start(out=resetm[:], in_=C["resetm"]), "c2", writes=["resetm"])
                P.dma("sp", lambda e: e.dma_start(out=causal[:], in_=C["causal"]), "c3", writes=["causal"])
                P.op("dve", lambda e: e.tensor_tensor(out=lbT[:], in0=lbl[:, 0, :], in1=lbl[:, 1, :], op=ALU.subtract), reads=["lbl"], writes=["lbT"])
                P.op("act", lambda e: e.activation(out=lbT[:], in_=lbT[:], func=AF.Sigmoid), reads=["lbT"], writes=["lbT"])
                P.op("dve", lambda e: e.tensor_scalar(out=oml[:], in0=lbT[:], scalar1=-1.0, scalar2=1.0, op0=ALU.mult, op1=ALU.add), reads=["lbT"], writes=["oml"])
                P.op("dve", lambda e: e.tensor_scalar(out=noml[:], in0=oml[:], scalar1=-1.0, scalar2=None, op0=ALU.mult), reads=["oml"], writes=["noml"])
                P.op("pool", lambda e: e.memset(Sst[:, 0, :], 0.0), writes=["Sst0"])
                for h in range(nh4):
                    P.dma("sp", lambda e, h=h: e.dma_start(out=f4[:], in_=f_s[h]), "w0", writes=["f4"])
                    P.dma("sp", lambda e, h=h: e.dma_start(out=qr4[:], in_=qr_s[h]), "w1", writes=["qr4"])
                    P.dma("sp", lambda e, h=h: e.dma_start(out=g4[:], in_=g_s[h]), "o0", writes=["g4"])
                    P.dma("sp", lambda e, h=h: e.dma_start(out=vch[:], in_=i_s[:, 128 * h:128 * h + 128].rearrange("(c s) d -> s c d", s=64)), "o1", writes=["vch"])
                    hc = slice(h, h + 1)
                    P.op("act", lambda e: e.activation(out=f4[:], in_=f4[:], func=AF.Sigmoid), reads=["f4"], writes=["f4"])
                    P.op("act", lambda e, hc=hc: e.activation(out=lf[:], in_=f4[:], func=AF.Ln, scale=oml[:, hc], bias=lbT[:, hc]), reads=["f4", "oml", "lbT"], writes=["lf"])
                    P.op("dve", lambda e, hc=hc: e.tensor_scalar(out=kk[:], in0=f4[:], scalar1=noml[:, hc], scalar2=oml[:, hc], op0=ALU.mult, op1=ALU.add),
                         reads=["f4", "oml", "noml"], writes=["kk"])
                    P.op("dve", lambda e: e.tensor_tensor_scan(out=bcs[:], data0=resetm[:], data1=lf[:], initial=0.0, op0=ALU.mult, op1=ALU.add),
                         reads=["resetm", "lf"], writes=["bcs"])
                    P.op("act", lambda e: e.activation(out=eb[:], in_=bcs[:], func=AF.Exp), reads=["bcs"], writes=["eb"])
                    P.op("act", lambda e: e.activation(out=enb[:], in_=bcs[:], func=AF.Exp, scale=-1.0), reads=["bcs"], writes=["enb"])
                    P.op("dve", lambda e: e.tensor_tensor(out=qe[:], in0=qr4[:], in1=eb[:], op=ALU.mult), reads=["qr4", "eb"], writes=["qe"])
                    P.op("pool", lambda e: e.tensor_tensor(out=ke[:], in0=kk[:], in1=enb[:], op=ALU.mult), reads=["kk", "enb"], writes=["ke"])
                    for g in range(4):
                        a = g % 2
                        def mmA(e, g=g, a=a):
                            ins = None
                            for c8 in range(8):
                                c = 8 * g + c8
                                ins = e.matmul(psA[a][:, 64 * c8:64 * c8 + 64], lhsT=ke[:, 64 * c:64 * c + 64], rhs=qe[:, 64 * c:64 * c + 64], start=True, stop=True)
                            return ins
                        P.op("pe", mmA, reads=["ke", "qe"], writes=[("psA", a)])
                        P.op("dve", lambda e, g=g, a=a: e.tensor_tensor(out=scs[g][:], in0=psA[a][:], in1=causal[:], op=ALU.mult),
                             reads=[("psA", a), "causal"], writes=[("scs", g)])
                    for g in range(4):
                        def trB(e, g=g):
                            ins = None
                            for c8 in range(8):
                                c = 8 * g + c8
                                ins = e.transpose(out=psB[:, c8, :], in_=ke[:, 64 * c:64 * c + 64], identity=ident_bf[:])
                            return ins
                        P.op("pe", trB, reads=["ke"], writes=["psB"])
                        P.op("act", lambda e, g=g: e.activation(out=keT[:, 8 * g:8 * g + 8, :], in_=psB[:], func=AF.Copy), reads=["psB"], writes=["keT"])
                    for g in range(8):
                        a = g % 2
                        def mmC(e, g=g, a=a):
                            ins = None
                            for c4 in range(4):
                                c = 4 * g + c4
                                ins = e.matmul(psC[a][:, c4, :], lhsT=keT[:, c, :], rhs=vch[:, c, :], start=True, stop=True)
                            return ins
                        P.op("pe", mmC, reads=["keT", "vch"], writes=[("psC", a)])
                        def evC(e, g=g, a=a):
                            ins = None
                            for c4 in range(4):
                                c = 4 * g + c4
                                ins = e.activation(out=Vsb[:, c, :], in_=psC[a][:, c4, :], func=AF.Copy, scale=eb[:, 64 * c + 63:64 * c + 64])
                            return ins
                        P.op("act", evC, reads=[("psC", a), "eb"], writes=["Vsb"])
                    def scanD(e):
                        ins = None
                        for c in range(31):
                            ins = e.scalar_tensor_tensor(out=Sst[:, c + 1, :], in0=Sst[:, c, :], scalar=eb[:, 64 * c + 63:64 * c + 64], in1=Vsb[:, c, :], op0=ALU.mult, op1=ALU.add)
                        return ins
                    P.op("dve", scanD, reads=["Vsb", "eb", "Sst0"], writes=["Sst"])
                    P.op("pool", lambda e: e.tensor_copy(out=Sbf[:], in_=Sst[:]), reads=["Sst", "Sst0"], writes=["Sbf"])
                    for g in range(4):
                        a = g % 2
                        def mmE(e, g=g, a=a):
                            ins = None
                            for c8 in range(8):
                                c = 8 * g + c8
                                reg = psE[a][:, 64 * c8:64 * c8 + 64]
                                e.matmul(reg, lhsT=Sbf[:, c, :], rhs=qe[:, 64 * c:64 * c + 64], start=True, stop=False)
                                ins = e.matmul(reg, lhsT=vch[:, c, :], rhs=scs[g][:, 64 * c8:64 * c8 + 64], start=False, stop=True)
                            return ins
                        P.op("pe", mmE, reads=["Sbf", "qe", "vch", ("scs", g)], writes=[("psE", a)])
                        P.op("act", lambda e, g=g, a=a: e.activation(out=oT[:, 512 * g:512 * g + 512], in_=psE[a][:], func=AF.Copy), reads=[("psE", a)], writes=["oT"])
                    P.op("act", lambda e: e.activation(out=sq[:], in_=oT[:], func=AF.Square), reads=["oT"], writes=["sq"])
                    for g in range(4):
                        gs = slice(512 * g, 512 * g + 512)
                        P.op("pe", lambda e, gs=gs: e.matmul(psF[:], lhsT=ones_bf[:], rhs=sq[:, gs], start=True, stop=True), reads=["sq"], writes=["psF"])
                        P.op("act", lambda e, gs=gs: e.activation(out=bcs[:, gs], in_=psF[:], func=AF.Sqrt, scale=1.0 / 128, bias=EPS), reads=["psF"], writes=["bcs"])
                    P.op("dve", lambda e: e.reciprocal(out=bcs[:], in_=bcs[:]), reads=["bcs"], writes=["bcs"])
                    P.op("dve", lambda e: e.tensor_tensor(out=oT[:], in0=oT[:], in1=bcs[:], op=ALU.mult), reads=["oT", "bcs"], writes=["oT"])
                    P.op("dve", lambda e: e.scalar_tensor_tensor(out=yr[:], in0=oT[:], scalar=rng[:, 0:1], in1=g4[:], op0=ALU.mult, op1=ALU.mult),
                         reads=["oT", "rng", "g4"], writes=["yr"])
                    P.dma("sp", lambda e, h=h: e.dma_start(out=yr_s[h], in_=yr[:]), "y0", reads=["yr"], writes=[("yr_s", h)])
                P.barrier()

        def phase3():
            with ExitStack() as ps:
                mbf = ps.enter_context(nc.sbuf_tensor("p3_mbf", [128, 256], F32))
                maskb = ps.enter_context(nc.sbuf_tensor("p3_maskb", [128, 256], BF16))
                q3 = [ps.enter_context(nc.sbuf_tensor(f"p3_q{i}", [128, S], BF16)) for i in range(2)]
                k3 = [ps.enter_context(nc.sbuf_tensor(f"p3_k{i}", [128, S], BF16)) for i in range(2)]
                v3 = [[ps.enter_context(nc.sbuf_tensor(f"p3_v{i}_{r}", [128, 16, 128], BF16)) for r in range(3)] for i in range(2)]
                num = ps.enter_context(nc.sbuf_tensor("p3_num", [128, S], F32))
                den = ps.enter_context(nc.sbuf_tensor("p3_den", [128, S], F32))
                ya = [ps.enter_context(nc.sbuf_tensor(f"p3_ya{i}", [128, S], BF16)) for i in range(2)]
                pT = [ps.enter_context(nc.sbuf_tensor(f"p3_pT{i}", [128, 256], BF16)) for i in range(4)]
                pstb = [ps.enter_context(nc.psum_tensor(f"p3_pst{i}", [128, 512], F32)) for i in range(4)]
                pnum = [ps.enter_context(nc.psum_tensor(f"p3_pn{i}", [128, 512], F32)) for i in range(2)]
                pden = [ps.enter_context(nc.psum_tensor(f"p3_pd{i}", [128, 512], F32)) for i in range(2)]
                P.dma("sp", lambda e: e.dma_start(out=mbf[:], in_=C["maskb"]), "c0", writes=["mbf"])
                P.op("dve", lambda e: e.tensor_copy(out=maskb[:], in_=mbf[:]), reads=["mbf"], writes=["maskb"])
                SC = float(DH ** -0.5)
                RS = p3rs

                def pst(slot):
                    return pstb[slot][:, 0:256]

                def sview(t, r, j):
                    if r == 1:
                        return t[:]
                    return t[:].rearrange("p (n r) -> p r n", r=r)[:, j, :]

                def load3(h):
                    s = h % 2
                    P.dma("sp", lambda e, s=s, h=h: e.dma_start(out=q3[s][:], in_=qT_s[h]), f"w{s}", writes=[("q3", s)])
                    P.dma("sp", lambda e, s=s, h=h: e.dma_start(out=k3[s][:], in_=kT_s[h]), f"o{s}", writes=[("k3", s)])
                    for ri, r in enumerate(RS if p3v >= 0 else ()):
                        src = v_s[:, 128 * h:128 * h + 128].rearrange("(i n r) d -> n r i d", n=128, r=r)
                        dstv = v3[s][ri][:].rearrange("p (r i) d -> p r i d", r=r)
                        P.dma("sp", lambda e, src=src, dstv=dstv: e.dma_start(out=dstv, in_=src), f"v{s}{ri}", writes=[("v3", s, ri)])
                if nh3 > 0:
                    load3(0)
                for h in range(nh3):
                    s = h % 2
                    if h + 1 < nh3:
                        load3(h + 1)
                    P.op("pool", lambda e: e.memset(num[:], 0.0), writes=["num"])
                    P.op("pool", lambda e: e.memset(den[:], 0.0), writes=["den"])
                    tasks = []
                    for ri, r in enumerate(RS):
                        nb = 16 // r
                        for j in range(r):
                            for i in range(nb):
                                tasks.append((ri, r, j, i, nb))
                    if p3v <= 0:
                        tasks = []
                    T = len(tasks)

                    def emit_scores(t):
                        ri, r, j, i, nb = tasks[t]
                        slot = t % 4
                        nq = 256 if i < nb - 1 else 128
                        kv = sview(k3[s], r, j)
                        qv = sview(q3[s], r, j)

                        def mm(e, slot=slot, nq=nq, kv=kv, qv=qv, i=i):
                            e.matmul(pst(slot)[:, 0:nq], lhsT=kv[:, 128 * i:128 * i + 128], rhs=qv[:, 128 * i:128 * i + nq], start=True, stop=False)
                            return e.matmul(pst(slot)[:, 0:nq], lhsT=ident_bf[:], rhs=maskb[:, 0:nq], start=False, stop=True)
                        P.op("pe", mm, reads=[("q3", s), ("k3", s), "maskb"], writes=[("pst", slot)])
                        P.op("act", lambda e, slot=slot, nq=nq: e.activation(out=pT[slot][:, 0:nq], in_=pst(slot)[:, 0:nq], func=AF.Exp, scale=SC),
                             reads=[("pst", slot)], writes=[("pT", slot)])

                    def emit_pv(t):
                        ri, r, j, i, nb = tasks[t]
                        slot = t % 4
                        bank = (t // 4) % 2
                        qi = t % 4
                        vr = v3[s][ri]
                        bi = j * nb + i

                        def mm(e, slot=slot, bank=bank, qi=qi, vr=vr, bi=bi, i=i):
                            ins = None
                            for dst_, lget in ((pnum[bank], lambda b_: vr[:, b_, :]), (pden[bank], lambda b_: ones_bf[:])):
                                reg = dst_[:, 128 * qi:128 * qi + 128]
                                if i > 0:
                                    e.matmul(reg, lhsT=lget(bi - 1), rhs=pT[(slot - 1) % 4][:, 128:256], start=True, stop=False)
                                    ins = e.matmul(reg, lhsT=lget(bi), rhs=pT[slot][:, 0:128], start=False, stop=True)
                                else:
                                    ins = e.matmul(reg, lhsT=lget(bi), rhs=pT[slot][:, 0:128], start=True, stop=True)
                            return ins
                        rd = [("pT", slot), ("v3", s, ri)]
                        if i > 0:
                            rd.append(("pT", (slot - 1) % 4))
                        P.op("pe", mm, reads=rd, writes=[("pnum", bank), ("pden", bank)])
                        if qi == 3 and p3v >= 3:
                            g = t // 4
                            if r == 1:
                                tg = i // 4
                                nv = num[:, 512 * tg:512 * tg + 512]
                                dv = den[:, 512 * tg:512 * tg + 512]
                                pn, pd = pnum[bank][:], pden[bank][:]
                            elif r == 4:
                                nv = sview(num, 4, j)
                                dv = sview(den, 4, j)
                                pn, pd = pnum[bank][:], pden[bank][:]
                            else:
                                j0 = j - 3
                                nv = num[:].rearrange("p (n r) -> p r n", r=16)[:, j0:j0 + 4, :]
                                dv = den[:].rearrange("p (n r) -> p r n", r=16)[:, j0:j0 + 4, :]
                                pn = pnum[bank][:].rearrange("p (a n) -> p a n", a=4)
                                pd = pden[bank][:].rearrange("p (a n) -> p a n", a=4)
                            P.op("dve", lambda e, nv=nv, pn=pn: e.tensor_tensor(out=nv, in0=pn, in1=nv, op=ALU.add), reads=[("pnum", bank), "num"], writes=["num"])
                            P.op("dve", lambda e, dv=dv, pd=pd: e.tensor_tensor(out=dv, in0=pd, in1=dv, op=ALU.add), reads=[("pden", bank), "den"], writes=["den"])

                    for t in range(T + 1):
                        if t < T:
                            emit_scores(t)
                        if t >= 1 and p3v >= 2 and T > 0:
                            emit_pv(t - 1)
                    P.op("dve", lambda e: e.reciprocal(out=den[:], in_=den[:]), reads=["den"], writes=["den"])
                    P.op("dve", lambda e, s=s: e.tensor_tensor(out=ya[s][:], in0=num[:], in1=den[:], op=ALU.mult), reads=["num", "den"], writes=[("ya", s)])
                    P.dma("sp", lambda e, s=s, h=h: e.dma_start(out=ya_s[h], in_=ya[s][:]), f"y{s}", reads=[("ya", s)], writes=[("ya_s", h)])
                P.barrier()
        if 3 not in skip:
            phase3()


        def phase5a():
            with ExitStack() as ps:
                A = lambda n, shp, dt: ps.enter_context(nc.sbuf_tensor("p5a_" + n, shp, dt))
                yaT = A("yaT", [128, 16, 512], BF16)
                yrT = A("yrT", [128, 16, 512], BF16)
                wsta = [A(f"wsta{i}", [128, 16, 256], F32) for i in range(2)]
                wstr = [A(f"wstr{i}", [128, 16, 256], F32) for i in range(2)]
                wba = [A(f"wba{i}", [128, 16, 256], BF16) for i in range(2)]
                wbr = [A(f"wbr{i}", [128, 16, 256], BF16) for i in range(2)]
                gat = [A(f"gat{i}", [128, 512], BF16) for i in range(2)]
                grt = [A(f"grt{i}", [128, 512], BF16) for i in range(2)]
                t1 = A("t1", [128, 512], F32)
                t2 = A("t2", [128, 512], F32)
                mst = [A(f"mst{i}", [128, 512], BF16) for i in range(2)]
                pA = [ps.enter_context(nc.psum_tensor(f"p5a_A{i}", [128, 512], F32)) for i in range(2)]
                pR = [ps.enter_context(nc.psum_tensor(f"p5a_R{i}", [128, 512], F32)) for i in range(2)]
                wav = I["w_a"].rearrange("(kc p) n -> p kc n", p=128)
                wrv = I["w_r"].rearrange("(kc p) n -> p kc n", p=128)
                it = 0

                def loadw5(step):
                    ct_, s_ = step % 8, step % 2
                    P.dma("act", lambda e: e.dma_start(out=wsta[s_][:], in_=wav[:, :, 256 * ct_:256 * ct_ + 256]), f"w{s_}", writes=[("wsta", s_)])
                    P.dma("act", lambda e: e.dma_start(out=wstr[s_][:], in_=wrv[:, :, 256 * ct_:256 * ct_ + 256]), f"v{s_}0", writes=[("wstr", s_)])
                for tt in range(4):
                    tsl = slice(512 * tt, 512 * tt + 512)
                    P.dma("sp", lambda e, tsl=tsl: e.dma_start(out=yaT[:], in_=ya_s.rearrange("h p t -> p h t")[:, :, tsl]), "c0", writes=["yaT"])
                    P.dma("sp", lambda e, tsl=tsl: e.dma_start(out=yrT[:], in_=yr_s.rearrange("h p t -> p h t")[:, :, tsl]), "c1", writes=["yrT"])
                    for ct in range(8):
                        s = ct % 2
                        step = 8 * tt + ct
                        if step == 0:
                            loadw5(0)
                        if step + 1 < 32:
                            loadw5(step + 1)
                        P.op("pool", lambda e, s=s: e.tensor_copy(out=wba[s][:], in_=wsta[s][:]), reads=[("wsta", s)], writes=[("wba", s)])
                        P.op("pool", lambda e, s=s: e.tensor_copy(out=wbr[s][:], in_=wstr[s][:]), reads=[("wstr", s)], writes=[("wbr", s)])
                        for sub in range(2):
                            cc = 2 * ct + sub
                            q = it % 2; it += 1
                            P.dma("sp", lambda e, q=q, cc=cc, tsl=tsl: e.dma_start(out=gat[q][:], in_=ga_s[cc][:, tsl]), f"o{q}", writes=[("gat", q)])
                            P.dma("sp", lambda e, q=q, cc=cc, tsl=tsl: e.dma_start(out=grt[q][:], in_=gr_s[cc][:, tsl]), f"o{q}f", writes=[("grt", q)])
                            def mmA(e, s=s, sub=sub, q=q):
                                ins = None
                                for kc in range(16):
                                    ins = e.matmul(pA[q][:], lhsT=wba[s][:, kc, 128 * sub:128 * sub + 128], rhs=yaT[:, kc, :], start=(kc == 0), stop=(kc == 15))
                                return ins
                            def mmR(e, s=s, sub=sub, q=q):
                                ins = None
                                for kc in range(16):
                                    ins = e.matmul(pR[q][:], lhsT=wbr[s][:, kc, 128 * sub:128 * sub + 128], rhs=yrT[:, kc, :], start=(kc == 0), stop=(kc == 15))
                                return ins
                            P.op("pe", mmA, reads=[("wba", s), "yaT"], writes=[("pA", q)])
                            P.op("pe", mmR, reads=[("wbr", s), "yrT"], writes=[("pR", q)])
                            P.op("dve", lambda e, q=q: e.tensor_tensor(out=t1[:], in0=pA[q][:], in1=gat[q][:], op=ALU.mult), reads=[("pA", q), ("gat", q)], writes=["t1"])
                            P.op("dve", lambda e, q=q: e.tensor_tensor(out=t2[:], in0=pR[q][:], in1=grt[q][:], op=ALU.mult), reads=[("pR", q), ("grt", q)], writes=["t2"])
                            P.op("pool", lambda e, q=q: e.tensor_tensor(out=mst[q][:], in0=t1[:], in1=t2[:], op=ALU.add), reads=["t1", "t2"], writes=[("mst", q)])
                            P.dma("sp", lambda e, q=q, cc=cc, tsl=tsl: e.dma_start(out=m_s[cc][:, tsl], in_=mst[q][:]), f"y{q}", reads=[("mst", q)], writes=[("m_s", cc, tt)])
                P.barrier()

        def phase5b():
            with ExitStack() as ps:
                A = lambda n, shp, dt: ps.enter_context(nc.sbuf_tensor("p5b_" + n, shp, dt))
                wo = A("wo", [128, 16, D], BF16)
                wst5 = [A(f"wst{i}", [128, 16, 256], F32) for i in range(2)]
                gtm = A("gtm", [128, D], F32)
                G2 = A("G2", [128, D], F32)
                shf = A("shf", [128, D], F32)
                mT = [A(f"mT{i}", [128, 16, 128], BF16) for i in range(2)]
                xt5 = [A(f"xt{i}", [128, D], F32) for i in range(2)]
                ht = A("ht", [128, D], F32)
                tf5 = A("tf", [128, D], F32)
                u2f = A("u2f", [128, D], F32)
                u2b = [A(f"u2b{i}", [128, D], BF16) for i in range(2)]
                junk5 = A("junk", [128, D], BF16)
                st5 = [A(f"st{i}", [128, 4], F32) for i in range(2)]
                u2T = A("u2T", [128, 16, 128], F32)
                wr = A("wr", [128, 16, 36], F32)
                brt = A("brt", [128, 36], F32)
                lgall = A("lgall", [128, 16, 36], F32)
                pM = [ps.enter_context(nc.psum_tensor(f"p5b_M{i}", [128, 512], F32)) for i in range(4)]
                pT5 = ps.enter_context(nc.psum_tensor("p5b_T", [128, 8, 128], F32))
                pL = ps.enter_context(nc.psum_tensor("p5b_L", [128, 36], F32))
                wov = I["w_o"].rearrange("(kc p) n -> p kc n", p=128)
                for ct in range(8):
                    s = ct % 2
                    P.dma("sp", lambda e, s=s, ct=ct: e.dma_start(out=wst5[s][:], in_=wov[:, :, 256 * ct:256 * ct + 256]), f"w{s}", writes=[("wst5", s)])
                    P.op("pool", lambda e, s=s, ct=ct: e.tensor_copy(out=wo[:, :, 256 * ct:256 * ct + 256], in_=wst5[s][:]), reads=[("wst5", s)], writes=["wo"])
                P.dma("sp", lambda e: e.dma_start(out=gtm[:], in_=modbc[:, 2 * D:3 * D]), "c0", writes=["gtm"])
                P.dma("sp", lambda e: e.dma_start(out=shf[:], in_=modbc[:, 3 * D:4 * D]), "c1", writes=["shf"])
                P.dma("sp", lambda e: e.dma_start(out=tf5[:], in_=modbc[:, 4 * D:5 * D]), "c2", writes=["tf5"])
                P.dma("sp", lambda e: e.dma_start(out=G2[:], in_=I["ffn_norm_g"].partition_broadcast(128)), "c3", writes=["G2"])
                P.op("dve", lambda e: e.scalar_tensor_tensor(out=G2[:], in0=tf5[:], scalar=1.0, in1=G2[:], op0=ALU.add, op1=ALU.mult), reads=["tf5", "G2"], writes=["G2"])
                P.dma("sp", lambda e: e.dma_start(out=wr[:], in_=I["w_router"].rearrange("(kc p) n -> p kc n", p=128)), "o0", writes=["wr"])
                P.dma("sp", lambda e: e.dma_start(out=brt[:], in_=I["b_router"].partition_broadcast(128)), "o1", writes=["brt"])
                def load5b(i):
                    s = i % 2
                    tsl = slice(128 * i, 128 * i + 128)
                    P.dma("sp", lambda e, s=s, tsl=tsl: e.dma_start(out=mT[s][:], in_=m_s.rearrange("h p t -> p h t")[:, :, tsl]), f"v{s}0", writes=[("mT", s)])
                    P.dma("sp", lambda e, s=s, tsl=tsl: e.dma_start(out=xt5[s][:], in_=I["x"][tsl, :]), f"v{s}1", writes=[("xt5", s)])
                load5b(0)
                for i in range(16):
                    s = i % 2
                    tsl = slice(128 * i, 128 * i + 128)
                    if i + 1 < 16:
                        load5b(i + 1)
                    for cc in range(4):
                        csl = slice(512 * cc, 512 * cc + 512)
                        def mmM(e, s=s, cc=cc, csl=csl):
                            ins = None
                            for kc in range(16):
                                ins = e.matmul(pM[cc][:], lhsT=mT[s][:, kc, :], rhs=wo[:, kc, csl], start=(kc == 0), stop=(kc == 15))
                            return ins
                        P.op("pe", mmM, reads=[("mT", s), "wo"], writes=[("pM", cc)])
                        P.op("dve", lambda e, cc=cc, csl=csl: e.tensor_tensor(out=tf5[:, csl], in0=pM[cc][:], in1=gtm[:, csl], op=ALU.mult), reads=[("pM", cc), "gtm"], writes=[("tf5", cc)])
                    P.op("pool", lambda e, s=s: e.tensor_tensor(out=ht[:], in0=tf5[:], in1=xt5[s][:], op=ALU.add), reads=[("tf5", 0), ("tf5", 1), ("tf5", 2), ("tf5", 3), ("xt5", s)], writes=["ht"])
                    P.dma("sp", lambda e, tsl=tsl: e.dma_start(out=h_s[tsl, :], in_=ht[:]), "y0", reads=["ht"], writes=[("h_s", i)])
                    P.op("act", lambda e, s=s: e.activation(out=junk5[:], in_=ht[:], func=AF.Square, accum_out=st5[s][:, 0:1]), reads=["ht"], writes=["junk5", ("st5", s)])
                    P.op("act", lambda e, s=s: e.activation(out=st5[s][:, 1:2], in_=st5[s][:, 0:1], func=AF.Sqrt, scale=1.0 / D, bias=EPS), reads=[("st5", s)], writes=[("st5", s)])
                    P.op("dve", lambda e, s=s: e.reciprocal(out=st5[s][:, 2:3], in_=st5[s][:, 1:2]), reads=[("st5", s)], writes=[("st5", s)])
                    P.op("dve", lambda e, s=s: e.scalar_tensor_tensor(out=u2f[:], in0=ht[:], scalar=st5[s][:, 2:3], in1=G2[:], op0=ALU.mult, op1=ALU.mult),
                         reads=["ht", ("st5", s), "G2"], writes=["u2f"])
                    P.op("pool", lambda e: e.tensor_tensor(out=u2f[:], in0=u2f[:], in1=shf[:], op=ALU.add), reads=["u2f", "shf"], writes=["u2f"])
                    P.op("act", lambda e, s=s: e.activation(out=u2b[s][:], in_=u2f[:], func=AF.Copy), reads=["u2f"], writes=[("u2b", s)])
                    P.dma("sp", lambda e, s=s, tsl=tsl: e.dma_start(out=u2_s[tsl, :], in_=u2b[s][:]), f"y1{s}", reads=[("u2b", s)], writes=[("u2_s", i)])
                    for hf in range(2):
                        def tr5(e, hf=hf):
                            ins = None
                            for k8 in range(8):
                                kc = 8 * hf + k8
                                ins = e.transpose(out=pT5[:, k8, :], in_=u2f[:, 128 * kc:128 * kc + 128], identity=ident_f[:])
                            return ins
                        P.op("pe", tr5, reads=["u2f"], writes=["pT5"])
                        P.op("act", lambda e, hf=hf: e.activation(out=u2T[:, 8 * hf:8 * hf + 8, :], in_=pT5[:], func=AF.Copy), reads=["pT5"], writes=[("u2T", hf)])
                    def mmL(e):
                        ins = None
                        for kc in range(16):
                            ins = e.matmul(pL[:], lhsT=u2T[:, kc, :], rhs=wr[:, kc, :], start=(kc == 0), stop=(kc == 15))
                        return ins
                    P.op("pe", mmL, reads=[("u2T", 0), ("u2T", 1), "wr"], writes=["pL"])
                    P.op("dve", lambda e, i=i: e.tensor_tensor(out=lgall[:, i, :], in0=pL[:], in1=brt[:], op=ALU.add), reads=["pL", "brt"], writes=["lgall"])
                P.dma("sp", lambda e: e.dma_start(out=lg_s, in_=lgall[:]), "y2", reads=["lgall"], writes=["lg_s"])
                P.barrier()

        def phase6():
            with ExitStack() as ps:
                A = lambda n, shp, dt: ps.enter_context(nc.sbuf_tensor("p6_" + n, shp, dt))
                lg = A("lg", [128, 16, 36], F32)
                iota_e = A("iota_e", [128, 32], F32)
                ltf = A("ltf", [128, 128], F32)
                ltb = A("ltb", [128, 128], BF16)
                thrb = A("thrb", [128, 64], F32)
                iotap = A("iotap", [128, 1], F32)
                eidx = A("eidx", [128, 32], F32)
                E = A("E", [128, 32, 32], BF16)
                gm = A("gm", [128, 1], F32)
                sh = A("sh", [128, 4], F32)
                ex = A("ex", [128, 4], F32)
                se = A("se", [128, 1], F32)
                pg = A("pg", [128, 1], F32)
                pen = A("pen", [128, 4], F32)
                lem = A("lem", [128, 32], F32)
                mx8 = A("mx8", [128, 8], F32)
                ix8 = A("ix8", [128, 8], U32)
                dl = A("dl", [128, 4], F32)
                base = A("base", [128, 32, 32], F32)
                tmp = A("tmp", [128, 32, 32], F32)
                cnt = A("cnt", [128, 32], F32)
                ci = A("ci", [128, 32], I32)
                padded = A("padded", [128, 32], F32)
                pend = A("pend", [128, 32], F32)
                pstart = A("pstart", [128, 32], F32)
                dest = A("dest", [128, 32], F32)
                acc = A("acc", [128, 64], F32)
                of = A("of", [128, 64], F32)
                over = A("over", [128, 64], F32)
                pmk = A("pmk", [128, 1], F32)
                u2t = [A(f"u2t{i}", [128, D], BF16) for i in range(2)]
                pPre = ps.enter_context(nc.psum_tensor("p6_pre", [128, 1024], F32))
                pTot = ps.enter_context(nc.psum_tensor("p6_tot", [128, 1024], F32))
                P.dma("sp", lambda e: e.dma_start(out=lg[:], in_=lg_s), "c0", writes=["lg"])
                P.dma("sp", lambda e: e.dma_start(out=iota_e[:], in_=C["iota_e"]), "c1", writes=["iota_e"])
                P.dma("sp", lambda e: e.dma_start(out=ltf[:], in_=C["ltri"]), "c2", writes=["ltf"])
                P.dma("sp", lambda e: e.dma_start(out=thrb[:], in_=C["thrb"]), "c3", writes=["thrb"])
                P.dma("sp", lambda e: e.dma_start(out=iotap[:], in_=C["iota_p"]), "o0", writes=["iotap"])
                CH = ["p6chain"]

                def X(eng, fn):
                    P.op(eng, fn, reads=CH, writes=CH)
                P.op("dve", lambda e: e.tensor_copy(out=ltb[:], in_=ltf[:]), reads=["ltf", "lg", "iota_e", "thrb", "iotap"], writes=CH)
                for t in range(16):
                    lgt = lg[:, t, 0:4]
                    let = lg[:, t, 4:36]
                    X("dve", lambda e, lgt=lgt: e.tensor_reduce(out=gm[:], in_=lgt, axis=AX.X, op=ALU.max))
                    X("dve", lambda e, lgt=lgt: e.tensor_scalar(out=sh[:], in0=lgt, scalar1=gm[:, 0:1], scalar2=None, op0=ALU.subtract))
                    X("dve", lambda e: e.tensor_scalar(out=pen[:], in0=sh[:], scalar1=0.0, scalar2=None, op0=ALU.is_equal))
                    X("dve", lambda e: e.tensor_scalar(out=pen[:], in0=pen[:], scalar1=-1.0, scalar2=1e30, op0=ALU.add, op1=ALU.mult))
                    for g in range(4):
                        X("dve", lambda e, g=g, let=let: e.tensor_scalar(out=lem[:, 8 * g:8 * g + 8], in0=let[:, 8 * g:8 * g + 8], scalar1=pen[:, g:g + 1], scalar2=None, op0=ALU.add))
                    X("act", lambda e: e.activation(out=ex[:], in_=sh[:], func=AF.Exp, accum_out=se[:, 0:1]))
                    X("dve", lambda e: e.max(out=mx8[:], in_=lem[:]))
                    X("dve", lambda e: e.max_index(out=ix8[:], in_max=mx8[:], in_values=lem[:]))
                    X("dve", lambda e, t=t: e.tensor_copy(out=eidx[:, 2 * t:2 * t + 2], in_=ix8[:, 0:2]))
                    X("dve", lambda e: e.tensor_tensor(out=dl[:, 0:1], in0=mx8[:, 1:2], in1=mx8[:, 0:1], op=ALU.subtract))
                    X("act", lambda e: e.activation(out=dl[:, 1:2], in_=dl[:, 0:1], func=AF.Exp))
                    X("dve", lambda e: e.reciprocal(out=pg[:], in_=se[:]))
                    X("dve", lambda e: e.tensor_scalar(out=dl[:, 2:3], in0=dl[:, 1:2], scalar1=1.0, scalar2=None, op0=ALU.add))
                    X("dve", lambda e: e.reciprocal(out=dl[:, 3:4], in_=dl[:, 2:3]))
                    X("dve", lambda e, t=t: e.tensor_tensor(out=wts[:, 2 * t:2 * t + 1], in0=pg[:], in1=dl[:, 3:4], op=ALU.mult))
                    X("dve", lambda e, t=t: e.tensor_tensor(out=wts[:, 2 * t + 1:2 * t + 2], in0=wts[:, 2 * t:2 * t + 1], in1=dl[:, 1:2], op=ALU.mult))
                for g in range(32):
                    X("dve", lambda e, g=g: e.tensor_scalar(out=E[:, g, :], in0=iota_e[:], scalar1=eidx[:, g:g + 1], scalar2=None, op0=ALU.is_equal))
                Ef = E[:].rearrange("p g e -> p (g e)")
                X("pe", lambda e: e.matmul(pPre[:, 0:512], lhsT=ltb[:], rhs=Ef[:, 0:512], start=True, stop=True))
                X("pe", lambda e: e.matmul(pPre[:, 512:1024], lhsT=ltb[:], rhs=Ef[:, 512:1024], start=True, stop=True))
                X("pe", lambda e: e.matmul(pTot[:, 0:512], lhsT=ones_bf[:], rhs=Ef[:, 0:512], start=True, stop=True))
                X("pe", lambda e: e.matmul(pTot[:, 512:1024], lhsT=ones_bf[:], rhs=Ef[:, 512:1024], start=True, stop=True))
                pTv = pTot[:].rearrange("p (g e) -> p g e", e=32)
                pPv = pPre[:].rearrange("p (g e) -> p g e", e=32)
                X("dve", lambda e: e.memset(base[:, 0, :], 0.0))
                for g in range(1, 32):
                    X("dve", lambda e, g=g: e.tensor_tensor(out=base[:, g, :], in0=pTv[:, g - 1, :], in1=base[:, g - 1, :], op=ALU.add))
                X("dve", lambda e: e.tensor_tensor(out=cnt[:], in0=pTv[:, 31, :], in1=base[:, 31, :], op=ALU.add))
                X("dve", lambda e: e.memset(padded[:], 0.0))
                for jj in range(32):
                    X("dve", lambda e, jj=jj: e.scalar_tensor_tensor(out=padded[:], in0=cnt[:], scalar=128.0 * jj, in1=padded[:], op0=ALU.is_gt, op1=ALU.add))
                X("dve", lambda e: e.tensor_scalar(out=padded[:], in0=padded[:], scalar1=128.0, scalar2=None, op0=ALU.mult))
                X("dve", lambda e: e.tensor_tensor_scan(out=pend[:], data0=ones_f[:, 0:32], data1=padded[:], initial=0.0, op0=ALU.mult, op1=ALU.add))
                X("dve", lambda e: e.tensor_tensor(out=pstart[:], in0=pend[:], in1=padded[:], op=ALU.subtract))
                X("dve", lambda e: e.tensor_tensor(out=tmp[:, 0:16, :], in0=pPv[:, 0:16, :], in1=base[:, 0:16, :], op=ALU.add))
                X("dve", lambda e: e.tensor_tensor(out=tmp[:, 16:32, :], in0=pPv[:, 16:32, :], in1=base[:, 16:32, :], op=ALU.add))
                for g in range(32):
                    X("dve", lambda e, g=g: e.tensor_tensor(out=tmp[:, g, :], in0=tmp[:, g, :], in1=pstart[:], op=ALU.add))
                X("dve", lambda e: e.tensor_tensor(out=tmp[:], in0=tmp[:], in1=E[:], op=ALU.mult))
                X("dve", lambda e: e.tensor_reduce(out=dest[:], in_=tmp[:], axis=AX.X, op=ALU.add))
                X("dve", lambda e: e.tensor_copy(out=desti[:], in_=dest[:]))
                X("dve", lambda e: e.memset(acc[:], 0.0))
                for ee in range(32):
                    X("dve", lambda e, ee=ee: e.scalar_tensor_tensor(out=acc[:], in0=thrb[:], scalar=pend[:, ee:ee + 1], in1=acc[:], op0=ALU.is_ge, op1=ALU.add))
                X("dve", lambda e: e.tensor_scalar(out=pmk[:], in0=iotap[:], scalar1=1.0, scalar2=None, op0=ALU.min))
                X("dve", lambda e: e.tensor_scalar(out=over[:], in0=acc[:], scalar1=32.0, scalar2=None, op0=ALU.is_ge))
                X("dve", lambda e: e.tensor_scalar(out=over[:], in0=over[:], scalar1=pmk[:, 0:1], scalar2=100000.0, op0=ALU.mult, op1=ALU.mult))
                X("dve", lambda e: e.tensor_scalar(out=acc[:], in0=acc[:], scalar1=31.0, scalar2=None, op0=ALU.min))
                X("dve", lambda e: e.tensor_scalar(out=of[:], in0=acc[:], scalar1=2048.0, scalar2=iotap[:, 0:1], op0=ALU.mult, op1=ALU.add))
                X("dve", lambda e: e.tensor_copy(out=offs_gu[:], in_=of[:]))
                X("dve", lambda e: e.tensor_scalar(out=of[:], in0=acc[:], scalar1=1024.0, scalar2=iotap[:, 0:1], op0=ALU.mult, op1=ALU.add))
                P.op("dve", lambda e: e.tensor_copy(out=offs_d[:], in_=of[:]), reads=CH, writes=CH + ["desti", "offs"])
                if debug:
                    rt = A("rt", [128, 16, 8], F32)
                    X("dve", lambda e: e.memset(rt[:], 0.0))
                    X("dve", lambda e: e.tensor_copy(out=rt[:, :, 0:2], in_=eidx[:].rearrange("p (t k) -> p t k", k=2)))
                    X("dve", lambda e: e.tensor_copy(out=rt[:, :, 2:4], in_=wts[:].rearrange("p (t k) -> p t k", k=2)))
                    X("dve", lambda e: e.tensor_copy(out=rt[:, :, 4:6], in_=dest[:].rearrange("p (t k) -> p t k", k=2)))
                    P.dma("sp", lambda e: e.dma_start(out=rt_s, in_=rt[:]), "o1", reads=CH, writes=["rt_s"])
                for t in range(16):
                    s = t % 2
                    P.dma("sp", lambda e, s=s, t=t: e.dma_start(out=u2t[s][:], in_=u2_s[128 * t:128 * t + 128, :]), f"w{s}", writes=[("u2t", s)])
                    for k in range(2):
                        g = 2 * t + k
                        P.dma("pool", lambda e, s=s, g=g: e.indirect_dma_start(out=xslot, out_offset=bass.IndirectOffsetOnAxis(ap=desti[:, g:g + 1], axis=0),
                                                                              in_=u2t[s][:], in_offset=None),
                              f"v{s}{k}", reads=[("u2t", s), "desti"], writes=[("xslot", g)])
                P.barrier()

        def phase7():
            with ExitStack() as ps:
                A = lambda n, shp, dt: ps.enter_context(nc.sbuf_tensor("p7_" + n, shp, dt))
                xt7 = [A(f"xt{i}", [128, D], BF16) for i in range(2)]
                xT7 = [A(f"xT{i}", [128, 16, 128], BF16) for i in range(2)]
                guf = [A(f"guf{i}", [128, 1024], F32) for i in range(12)]
                gub = [A(f"gub{i}", [128, 1024], BF16) for i in range(12)]
                dnf = [A(f"dnf{i}", [128, D], F32) for i in range(5)]
                dnb = [A(f"dnb{i}", [128, D], BF16) for i in range(5)]
                sg7 = A("sg", [128, 1024], F32)
                hd7 = A("hd", [128, 1024], BF16)
                hT7 = A("hT", [128, 8, 128], BF16)
                ys7 = [A(f"ys{i}", [128, D], F32) for i in range(2)]
                pGU = ps.enter_context(nc.psum_tensor("p7_GU", [128, 4, 512], F32))
                pTr = [ps.enter_context(nc.psum_tensor(f"p7_Tr{i}", [128, 8, 128], BF16)) for i in range(2)]
                gi = 0
                di = 0
                tri = 0
                def loadx7(b):
                    s = b % 2
                    P.dma("sp", lambda e, s=s, b=b: e.dma_start(out=xt7[s][:], in_=xslot[128 * b:128 * b + 128, :]), f"w{s}", writes=[("xt7", s)])
                if nb7 > 0:
                    loadx7(0)
                for b in range(nb7):
                    s = b % 2
                    if b + 1 < nb7:
                        loadx7(b + 1)
                    for hf in range(2):
                        w = tri % 2; tri += 1
                        def trx(e, s=s, hf=hf, w=w):
                            ins = None
                            for k8 in range(8):
                                kc = 8 * hf + k8
                                ins = e.transpose(out=pTr[w][:, k8, :], in_=xt7[s][:, 128 * kc:128 * kc + 128], identity=ident_bf[:])
                            return ins
                        P.op("pe", trx, reads=[("xt7", s)], writes=[("pTr", w)])
                        P.op("act", lambda e, s=s, hf=hf, w=w: e.activation(out=xT7[s][:, 8 * hf:8 * hf + 8, :], in_=pTr[w][:], func=AF.Copy),
                             reads=[("pTr", w)], writes=[("xT7", s, hf)])
                    for kc in range(16):
                        for which in range(2):
                            r = gi % 12; gi += 1
                            src = I["w_gate"] if which == 0 else I["w_up"]
                            P.dma("pool", lambda e, r=r, b=b, kc=kc, src=src: e.indirect_dma_start(
                                out=guf[r][:], out_offset=None, in_=src, in_offset=bass.IndirectOffsetOnAxis(ap=offs_gu[:, b:b + 1], axis=0),
                                element_offset=kc * 128 * 1024), f"g{r}", reads=["offs"], writes=[("guf", r)])
                            ceng = "act" if (gi % 2 == 0) else "dve"
                            if ceng == "act":
                                P.op("act", lambda e, r=r: e.activation(out=gub[r][:], in_=guf[r][:], func=AF.Copy), reads=[("guf", r)], writes=[("gub", r)])
                            else:
                                P.op("dve", lambda e, r=r: e.tensor_copy(out=gub[r][:], in_=guf[r][:]), reads=[("guf", r)], writes=[("gub", r)])
                            def mmg(e, s=s, r=r, kc=kc, which=which):
                                e.matmul(pGU[:, 2 * which, :], lhsT=xT7[s][:, kc, :], rhs=gub[r][:, 0:512], start=(kc == 0), stop=(kc == 15))
                                return e.matmul(pGU[:, 2 * which + 1, :], lhsT=xT7[s][:, kc, :], rhs=gub[r][:, 512:1024], start=(kc == 0), stop=(kc == 15))
                            P.op("pe", mmg, reads=[("gub", r), ("xT7", s, 0), ("xT7", s, 1)], writes=[("pgu", 2 * which), ("pgu", 2 * which + 1)])
                    P.op("act", lambda e: e.activation(out=sg7[:], in_=pGU[:, 0:2, :].rearrange("p a n -> p (a n)"), func=AF.Silu), reads=[("pgu", 0), ("pgu", 1)], writes=["sg7"])
                    P.op("dve", lambda e: e.tensor_tensor(out=hd7[:], in0=pGU[:, 2:4, :].rearrange("p a n -> p (a n)"), in1=sg7[:], op=ALU.mult),
                         reads=[("pgu", 2), ("pgu", 3), "sg7"], writes=["hd7"])
                    w = tri % 2; tri += 1
                    def trh(e, w=w):
                        ins = None
                        for k8 in range(8):
                            ins = e.transpose(out=pTr[w][:, k8, :], in_=hd7[:, 128 * k8:128 * k8 + 128], identity=ident_bf[:])
                        return ins
                    P.op("pe", trh, reads=["hd7"], writes=[("pTr", w)])
                    P.op("act", lambda e, w=w: e.activation(out=hT7[:], in_=pTr[w][:], func=AF.Copy), reads=[("pTr", w)], writes=["hT7"])
                    for k2 in range(8):
                        r = di % 5; di += 1
                        P.dma("pool", lambda e, r=r, b=b, k2=k2: e.indirect_dma_start(
                            out=dnf[r][:], out_offset=None, in_=I["w_down"], in_offset=bass.IndirectOffsetOnAxis(ap=offs_d[:, b:b + 1], axis=0),
                            element_offset=k2 * 128 * D), f"d{r}", reads=["offs"], writes=[("dnf", r)])
                        P.op("act", lambda e, r=r: e.activation(out=dnb[r][:, 0:1024], in_=dnf[r][:, 0:1024], func=AF.Copy), reads=[("dnf", r)], writes=[("dnb", r, 0)])
                        P.op("dve", lambda e, r=r: e.tensor_copy(out=dnb[r][:, 1024:2048], in_=dnf[r][:, 1024:2048]), reads=[("dnf", r)], writes=[("dnb", r, 1)])
                        def mmd(e, r=r, k2=k2):
                            ins = None
                            for cc in range(4):
                                ins = e.matmul(pGU[:, cc, :], lhsT=hT7[:, k2, :], rhs=dnb[r][:, 512 * cc:512 * cc + 512], start=(k2 == 0), stop=(k2 == 7))
                            return ins
                        P.op("pe", mmd, reads=[("dnb", r, 0), ("dnb", r, 1), "hT7"], writes=[("pgu", 0), ("pgu", 1), ("pgu", 2), ("pgu", 3)])
                    P.op("act", lambda e, s=s: e.activation(out=ys7[s][:, 0:1024], in_=pGU[:, 0:2, :].rearrange("p a n -> p (a n)"), func=AF.Copy),
                         reads=[("pgu", 0), ("pgu", 1)], writes=[("ys7", s, 0)])
                    P.op("dve", lambda e, s=s: e.tensor_copy(out=ys7[s][:, 1024:2048], in_=pGU[:, 2:4, :].rearrange("p a n -> p (a n)")),
                         reads=[("pgu", 2), ("pgu", 3)], writes=[("ys7", s, 1)])
                    P.dma("sp", lambda e, s=s, b=b: e.dma_start(out=yslot[128 * b:128 * b + 128, :], in_=ys7[s][:]), f"o{s}",
                          reads=[("ys7", s, 0), ("ys7", s, 1)], writes=[("yslot", b)])
                P.barrier()

        def phase8(mode):
            full = (mode == 'full')
            with ExitStack() as ps:
                Gf = ps.enter_context(nc.sbuf_tensor("p8_G", [128, D], F32))
                gtf = ps.enter_context(nc.sbuf_tensor("p8_gtf", [128, D], F32))
                xt8 = [ps.enter_context(nc.sbuf_tensor(f"p8_x{i}", [128, D], F32)) for i in range(2)]
                y08 = [ps.enter_context(nc.sbuf_tensor(f"p8_y0{i}", [128, D], F32)) for i in range(2)]
                y18 = [ps.enter_context(nc.sbuf_tensor(f"p8_y1{i}", [128, D], F32)) for i in range(2)]
                ot8 = [ps.enter_context(nc.sbuf_tensor(f"p8_o{i}", [128, D], F32)) for i in range(2)]
                junk8 = ps.enter_context(nc.sbuf_tensor("p8_junk", [128, D], BF16))
                st8 = [ps.enter_context(nc.sbuf_tensor(f"p8_st{i}", [128, 4], F32)) for i in range(2)]
                P.dma("sp", lambda e: e.dma_start(out=Gf[:], in_=I["final_norm_g"].partition_broadcast(128)), "c0", writes=["Gf"])
                if full:
                    P.dma("sp", lambda e: e.dma_start(out=gtf[:], in_=modbc[:, 5 * D:6 * D]), "c1", writes=["gtf"])
                hsrc = I["x"] if mode == 'x' else h_s
                def loadx8(i):
                    s = i % 2
                    P.dma("sp", lambda e, s=s, i=i: e.dma_start(out=xt8[s][:], in_=hsrc[128 * i:128 * i + 128, :]), f"w{s}", writes=[("xt8", s)])
                loadx8(0)
                for i in range(16):
                    s = i % 2
                    if i + 1 < 16:
                        loadx8(i + 1)
                    if full:
                        P.dma("pool", lambda e, s=s, i=i: e.indirect_dma_start(out=y08[s][:], out_offset=None, in_=yslot,
                                                                              in_offset=bass.IndirectOffsetOnAxis(ap=desti[:, 2 * i:2 * i + 1], axis=0)),
                              f"g{s}", writes=[("y08", s)])
                        P.dma("pool", lambda e, s=s, i=i: e.indirect_dma_start(out=y18[s][:], out_offset=None, in_=yslot,
                                                                              in_offset=bass.IndirectOffsetOnAxis(ap=desti[:, 2 * i + 1:2 * i + 2], axis=0)),
                              f"d{s}", writes=[("y18", s)])
                        P.op("dve", lambda e, s=s, i=i: e.tensor_scalar(out=y08[s][:], in0=y08[s][:], scalar1=wts[:, 2 * i:2 * i + 1], scalar2=None, op0=ALU.mult),
                             reads=[("y08", s)], writes=[("y08", s)])
                        P.op("dve", lambda e, s=s, i=i: e.scalar_tensor_tensor(out=y18[s][:], in0=y18[s][:], scalar=wts[:, 2 * i + 1:2 * i + 2], in1=y08[s][:], op0=ALU.mult, op1=ALU.add),
                             reads=[("y08", s), ("y18", s)], writes=[("y18", s)])
                        P.op("pool", lambda e, s=s: e.tensor_tensor(out=y18[s][:], in0=y18[s][:], in1=gtf[:], op=ALU.mult), reads=[("y18", s), "gtf"], writes=[("y18", s)])
                        P.op("pool", lambda e, s=s: e.tensor_tensor(out=xt8[s][:], in0=xt8[s][:], in1=y18[s][:], op=ALU.add), reads=[("y18", s), ("xt8", s)], writes=[("xt8", s)])
                    P.op("act", lambda e, s=s: e.activation(out=junk8[:], in_=xt8[s][:], func=AF.Square, accum_out=st8[s][:, 0:1]),
                         reads=[("xt8", s)], writes=["junk8", ("st8", s)])
                    P.op("act", lambda e, s=s: e.activation(out=st8[s][:, 1:2], in_=st8[s][:, 0:1], func=AF.Sqrt, scale=1.0 / D, bias=EPS),
                         reads=[("st8", s)], writes=[("st8", s)])
                    P.op("dve", lambda e, s=s: e.reciprocal(out=st8[s][:, 2:3], in_=st8[s][:, 1:2]), reads=[("st8", s)], writes=[("st8", s)])
                    P.op("dve", lambda e, s=s: e.scalar_tensor_tensor(out=ot8[s][:], in0=xt8[s][:], scalar=st8[s][:, 2:3], in1=Gf[:], op0=ALU.mult, op1=ALU.mult),
                         reads=[("xt8", s), ("st8", s), "Gf"], writes=[("ot8", s)])
                    P.dma("sp", lambda e, s=s, i=i: e.dma_start(out=out[128 * i:128 * i + 128, :], in_=ot8[s][:]), f"o{s}", reads=[("ot8", s)], writes=[("out", i)])
                P.barrier()
        if stop_after >= 4 and 4 not in skip:
            phase4()
        if stop_after >= 5 and 5 not in skip:
            phase5a()
        if stop_after >= 5 and 55 not in skip:
            phase5b()
        if stop_after >= 6 and 6 not in skip:
            phase6()
        if stop_after >= 7 and 7 not in skip:
            phase7()
        phase8('full' if (stop_after >= 7 and 7 not in skip) else ('h' if (stop_after >= 5 and 55 not in skip) else 'x'))
        P.barrier(); P.emit(); return nc
        if debug and stop_after <= 1:
            uT_d = scratch('s_uT', [128, 16, S], BF16)
            P.dma('sp', lambda e: e.dma_start(out=uT_d, in_=uT[:]), 'c0', reads=['uT'], writes=['uT_d'])
            P.barrier()
        P.emit()
    return nc


def _prep(inp, b):
    m = {}
    m["x"] = np.ascontiguousarray(inp["x"][b])
    m["c_t"] = np.ascontiguousarray(np.asarray(inp["c"][b]).reshape(16, 128).T)
    m["pos"] = np.ascontiguousarray(inp["positions"][b])
    m["ada_w"] = inp["ada_w"][0]
    m["ada_b"] = inp["ada_b"][0]
    m["mix_norm_g"] = inp["mix_norm_g"][0]
    m["w_in"] = inp["w_in"][0]
    m["w_a"] = inp["w_attn_branch"][0]
    m["w_r"] = inp["w_rec_branch"][0]
    m["w_o"] = inp["w_mix_out"][0]
    m["rec_norm_g"] = np.ascontiguousarray(np.asarray(inp["rec_norm_g"][0]).reshape(128, 1))
    m["lb_logits_t"] = np.ascontiguousarray(np.asarray(inp["rec_lb_logits"]).reshape(2, 16, 128).transpose(2, 0, 1))
    m["ffn_norm_g"] = inp["ffn_norm_g"][0]
    m["w_router"] = np.ascontiguousarray(np.concatenate([inp["router_group_w"][0], inp["router_expert_w"][0]], axis=1))
    m["b_router"] = np.ascontiguousarray(np.concatenate([inp["router_group_b"][0], inp["router_expert_b"][0]]))
    m["w_gate"] = np.asarray(inp["expert_w_gate"][0]).reshape(32 * D, 1024)
    m["w_up"] = np.asarray(inp["expert_w_up"][0]).reshape(32 * D, 1024)
    m["w_down"] = np.asarray(inp["expert_w_down"][0]).reshape(32 * 1024, D)
    m["final_norm_g"] = inp["final_norm_g"]
    return m


STOP_AFTER = 7


def kernel(**inputs):
    inp = {k: np.asarray(v) for k, v in inputs.items()}
    consts = {"c_" + k: v for k, v in host_consts().items()}
    nc = build(stop_after=STOP_AFTER)
    in_maps = []
    for b in range(8):
        m = _prep(inp, b)
        m.update(consts)
        in_maps.append(m)
    res = run_bass_kernel_spmd(nc, in_maps, core_ids=list(range(8)))
    return np.stack([np.asarray(r["out"], dtype=np.float32) for r in res.results], axis=0)
```
